# Optimizing a Trainium2 kernel written in Bass

```python
import math
import jax, jax.numpy as jnp
from jax import lax
import numpy as np

D_MODEL = 1024
BATCH = 8
SEQ = 4096
DEPTH = 4

N_META = 16
D_FF = 2816
EPS = 1e-6
Q_BLOCK = 128
A_HEADS = 8
A_HEAD_DIM = 64
A_WIDTH = A_HEADS * A_HEAD_DIM
IDX_HEADS = 8
IDX_DIM = 64
TOPK_MAX = 256
B_HEADS = 4
B_QK_DIM = 64
B_V_DIM = 2 * B_QK_DIM
B_WIDTH = B_HEADS * B_V_DIM
MIX_WIDTH = A_WIDTH + B_WIDTH
ATTN_SPLIT_SIZES = (A_WIDTH, A_WIDTH, A_WIDTH, IDX_HEADS * IDX_DIM, IDX_DIM, IDX_HEADS,
                    B_HEADS * 2 * B_QK_DIM, B_HEADS * 2 * B_QK_DIM, B_WIDTH)
ATTN_IN = 3 * A_WIDTH + IDX_HEADS * IDX_DIM + IDX_DIM + IDX_HEADS + 2 * B_HEADS * 2 * B_QK_DIM + B_WIDTH
LRU_WIDTH = D_MODEL
LRU_BLOCKS = 4
LRU_BLOCK_W = LRU_WIDTH // LRU_BLOCKS
CONV_W = 4
RG_C = 8.0
N_EVEN = (DEPTH + 1) // 2
N_ODD = DEPTH // 2

kernel_name = 'hybrid_dsa_diff_rglru_macaron'


def rms_norm(x, g):
    xf = x.astype(jnp.float32)
    y = xf * lax.rsqrt(jnp.mean(xf * xf, axis=-1, keepdims=True) + EPS)
    return (y * g.astype(jnp.float32)).astype(x.dtype)


def layer_norm(x, g, b):
    xf = x.astype(jnp.float32)
    mu = jnp.mean(xf, axis=-1, keepdims=True)
    xc = xf - mu
    y = xc * lax.rsqrt(jnp.mean(xc * xc, axis=-1, keepdims=True) + EPS)
    return (y * g.astype(jnp.float32) + b.astype(jnp.float32)).astype(x.dtype)


def swiglu(x, w_gu, w_down):
    g, u = jnp.split(x @ w_gu, 2, axis=-1)
    return (jax.nn.silu(g) * u) @ w_down


def to_blocks(a):
    b, s = a.shape[:2]
    a = a.reshape((b, s // Q_BLOCK, Q_BLOCK) + a.shape[2:])
    return jnp.moveaxis(a, 1, 0)


def from_blocks(a):
    a = jnp.moveaxis(a, 0, 1)
    return a.reshape((a.shape[0], a.shape[1] * a.shape[2]) + a.shape[3:])


def dense_causal(q, k, v, mask, scale):
    s = jnp.einsum('bqhd,bkhd->bhqk', q, k).astype(jnp.float32) * scale
    s = jnp.where(mask[None, None], s, -jnp.inf)
    p = jax.nn.softmax(s, axis=-1).astype(v.dtype)
    return jnp.einsum('bhqk,bkhd->bqhd', p, v)


def dsa_attention(q, k, v, q_idx, k_idx, w_idx):
    t_len = q.shape[1]
    s_len = t_len - N_META
    top_k = min(TOPK_MAX, s_len // 4)
    scale = A_HEAD_DIM ** -0.5
    k_meta, v_meta = k[:, :N_META], v[:, :N_META]
    kv_real = jnp.concatenate([k[:, N_META:], v[:, N_META:]], axis=-1)
    k_idx_real = k_idx[:, N_META:]
    meta_mask = jnp.tril(jnp.ones((N_META, N_META), dtype=bool))
    out_meta = dense_causal(q[:, :N_META], k_meta, v_meta, meta_mask, scale)
    key_pos = jnp.arange(s_len)

    def block(args):
        qb, qib, wib, start = args
        logits = jnp.einsum('bqhd,bsd->bqhs', qib, k_idx_real)
        score = jnp.einsum('bqh,bqhs->bqs', wib, jax.nn.relu(logits)).astype(jnp.float32)
        q_pos = start + jnp.arange(Q_BLOCK)
        causal = key_pos[None, :] <= q_pos[:, None]
        score = jnp.where(causal[None], score, -jnp.inf)
        sel_score, sel_idx = lax.top_k(score, top_k)
        valid = jnp.isfinite(sel_score)
        kv_sel = jax.vmap(lambda kv_b, ix_b: kv_b[ix_b])(kv_real, sel_idx)
        k_sel, v_sel = jnp.split(kv_sel, 2, axis=-1)
        s_meta = jnp.einsum('bqhd,bmhd->bhqm', qb, k_meta).astype(jnp.float32)
        s_sel = jnp.einsum('bqhd,bqkhd->bhqk', qb, k_sel).astype(jnp.float32)
        s_sel = jnp.where(valid[:, None], s_sel, -jnp.inf)
        p = jax.nn.softmax(jnp.concatenate([s_meta, s_sel], axis=-1) * scale, axis=-1).astype(v.dtype)
        return (jnp.einsum('bhqm,bmhd->bqhd', p[..., :N_META], v_meta)
                + jnp.einsum('bhqk,bqkhd->bqhd', p[..., N_META:], v_sel))

    starts = jnp.arange(s_len // Q_BLOCK, dtype=jnp.int32) * Q_BLOCK
    out_real = from_blocks(lax.map(block, (to_blocks(q[:, N_META:]), to_blocks(q_idx[:, N_META:]),
                                           to_blocks(w_idx[:, N_META:]), starts)))
    return jnp.concatenate([out_meta, out_real], axis=1)


def diff_attention(q, k, v, lam, lam_init, subln_g):
    t_len = q.shape[1]
    s_len = t_len - N_META
    scale = B_QK_DIM ** -0.5

    def attend(qb, mask):
        n_keys = mask.shape[1]
        s = jnp.einsum('bqhcd,bkhcd->bchqk', qb, k[:, :n_keys]).astype(jnp.float32) * scale
        s = jnp.where(mask, s, -jnp.inf)
        p = jax.nn.softmax(s, axis=-1)
        a = p[:, 0] - lam * p[:, 1]
        return jnp.einsum('bhqk,bkhd->bqhd', a.astype(v.dtype), v[:, :n_keys])

    out_meta = attend(q[:, :N_META], jnp.tril(jnp.ones((N_META, N_META), dtype=bool)))
    key_pos = jnp.arange(t_len)

    def block(args):
        qb, start = args
        q_pos = N_META + start + jnp.arange(Q_BLOCK)
        return attend(qb, key_pos[None, :] <= q_pos[:, None])

    starts = jnp.arange(s_len // Q_BLOCK, dtype=jnp.int32) * Q_BLOCK
    out_real = from_blocks(lax.map(block, (to_blocks(q[:, N_META:]), starts)))
    o = jnp.concatenate([out_meta, out_real], axis=1)
    return rms_norm(o, subln_g) * (1.0 - lam_init)


def attn_mixer(h, w_in, idx_ln_g, idx_ln_b, lam_params, subln_g, w_out, lam_init):
    b, t, _ = h.shape
    split_at = [int(c) for c in np.cumsum(ATTN_SPLIT_SIZES)[:-1]]
    qa, ka, va, qi, ki, wi, qb, kb, vb = jnp.split(h @ w_in, split_at, axis=-1)
    qa = qa.reshape(b, t, A_HEADS, A_HEAD_DIM)
    ka = ka.reshape(b, t, A_HEADS, A_HEAD_DIM)
    va = va.reshape(b, t, A_HEADS, A_HEAD_DIM)
    qi = qi.reshape(b, t, IDX_HEADS, IDX_DIM)
    ki = layer_norm(ki, idx_ln_g, idx_ln_b)
    wi = wi * (IDX_HEADS ** -0.5 * IDX_DIM ** -0.5)
    out_a = dsa_attention(qa, ka, va, qi, ki, wi).reshape(b, t, A_WIDTH)
    lp = lam_params.astype(jnp.float32)
    lam = jnp.exp(jnp.sum(lp[0] * lp[1])) - jnp.exp(jnp.sum(lp[2] * lp[3])) + lam_init
    qb = qb.reshape(b, t, B_HEADS, 2, B_QK_DIM)
    kb = kb.reshape(b, t, B_HEADS, 2, B_QK_DIM)
    vb = vb.reshape(b, t, B_HEADS, B_V_DIM)
    out_b = diff_attention(qb, kb, vb, lam, lam_init, subln_g).reshape(b, t, B_WIDTH)
    return jnp.concatenate([out_a, out_b], axis=-1) @ w_out


def causal_conv(x, w, bias):
    c = x.shape[-1]
    y = lax.conv_general_dilated(x, w[:, None, :], window_strides=(1,), padding=[(CONV_W - 1, 0)],
                                 dimension_numbers=('NWC', 'WIO', 'NWC'), feature_group_count=c)
    return y + bias


def rglru_mixer(h, w_in, conv_w, conv_b, gate_w, gate_b, lru_lambda, w_out):
    b, t, _ = h.shape
    y_br, x_br = jnp.split(h @ w_in, 2, axis=-1)
    y_br = jax.nn.gelu(y_br, approximate=True)
    xc = causal_conv(x_br, conv_w, conv_b)
    xb = xc.reshape(b, t, LRU_BLOCKS, LRU_BLOCK_W)
    gates = jnp.einsum('btni,gnij->gbtnj', xb, gate_w).reshape(2, b, t, LRU_WIDTH)
    gates = (gates + gate_b[:, None, None, :]).astype(jnp.float32)
    gate_x = jax.nn.sigmoid(gates[0])
    gate_a = jax.nn.sigmoid(gates[1])
    log_a = RG_C * gate_a * jax.nn.log_sigmoid(lru_lambda.astype(jnp.float32))
    a = jnp.exp(log_a)
    mult = jnp.sqrt(-jnp.expm1(2.0 * log_a))
    u = mult * (gate_x * xc.astype(jnp.float32))

    def combine(left, right):
        a_l, b_l = left
        a_r, b_r = right
        return a_l * a_r, a_r * b_l + b_r

    _, hs = lax.associative_scan(combine, (a, u), axis=1)
    return (hs.astype(h.dtype) * y_br) @ w_out


def setup_inputs(seed: int = 0) -> dict:
    key = jax.random.key(seed)
    ks = jax.random.split(key, 20)
    f32 = jnp.float32
    nrm = lambda k, shape, s: jax.random.normal(k, shape, f32) * s
    a_c = jax.random.uniform(ks[17], (N_ODD, LRU_WIDTH), f32, 0.9, 0.999)
    sig = a_c ** (1.0 / RG_C)
    return {
        'x': nrm(ks[0], (BATCH, SEQ, D_MODEL), 1.0),
        'meta_tokens': nrm(ks[1], (N_META, D_MODEL), 1.0),
        'norm_g': 1.0 + nrm(ks[2], (DEPTH, 3, D_MODEL), 0.02),
        'ffn_w_gu': nrm(ks[3], (DEPTH, 2, D_MODEL, 2 * D_FF), D_MODEL ** -0.5),
        'ffn_w_down': nrm(ks[4], (DEPTH, 2, D_FF, D_MODEL), D_FF ** -0.5),
        'attn_w_in': nrm(ks[5], (N_EVEN, D_MODEL, ATTN_IN), D_MODEL ** -0.5),
        'idx_k_ln_g': 1.0 + nrm(ks[6], (N_EVEN, IDX_DIM), 0.02),
        'idx_k_ln_b': nrm(ks[7], (N_EVEN, IDX_DIM), 0.02),
        'diff_lambda': nrm(ks[8], (N_EVEN, 4, B_QK_DIM), 0.1),
        'diff_subln_g': 1.0 + nrm(ks[9], (N_EVEN, B_V_DIM), 0.02),
        'attn_w_out': nrm(ks[10], (N_EVEN, MIX_WIDTH, D_MODEL), MIX_WIDTH ** -0.5),
        'rec_w_in': nrm(ks[11], (N_ODD, D_MODEL, 2 * LRU_WIDTH), D_MODEL ** -0.5),
        'rec_conv_w': nrm(ks[12], (N_ODD, CONV_W, LRU_WIDTH), CONV_W ** -0.5),
        'rec_conv_b': nrm(ks[13], (N_ODD, LRU_WIDTH), 0.02),
        'rec_gate_w': nrm(ks[14], (N_ODD, 2, LRU_BLOCKS, LRU_BLOCK_W, LRU_BLOCK_W), LRU_BLOCK_W ** -0.5),
        'rec_gate_b': nrm(ks[15], (N_ODD, 2, LRU_WIDTH), 0.02),
        'rec_lambda': jnp.log(sig) - jnp.log1p(-sig),
        'rec_w_out': nrm(ks[16], (N_ODD, LRU_WIDTH, D_MODEL), LRU_WIDTH ** -0.5),
        'final_norm_g': 1.0 + nrm(ks[18], (D_MODEL,), 0.02),
    }


def reference(x, meta_tokens, norm_g, ffn_w_gu, ffn_w_down, attn_w_in, idx_k_ln_g, idx_k_ln_b,
              diff_lambda, diff_subln_g, attn_w_out, rec_w_in, rec_conv_w, rec_conv_b, rec_gate_w,
              rec_gate_b, rec_lambda, rec_w_out, final_norm_g):
    b = x.shape[0]
    meta = jnp.broadcast_to(meta_tokens[None].astype(x.dtype), (b, N_META, D_MODEL))
    h = jnp.concatenate([meta, x], axis=1)
    for i in range(DEPTH):
        j = i // 2
        h = h + 0.5 * swiglu(rms_norm(h, norm_g[i, 0]), ffn_w_gu[i, 0], ffn_w_down[i, 0])
        hn = rms_norm(h, norm_g[i, 1])
        if i % 2 == 0:
            lam_init = 0.8 - 0.6 * math.exp(-0.3 * i)
            h = h + attn_mixer(hn, attn_w_in[j], idx_k_ln_g[j], idx_k_ln_b[j], diff_lambda[j],
                               diff_subln_g[j], attn_w_out[j], lam_init)
        else:
            h = h + rglru_mixer(hn, rec_w_in[j], rec_conv_w[j], rec_conv_b[j], rec_gate_w[j],
                                rec_gate_b[j], rec_lambda[j], rec_w_out[j])
        h = h + 0.5 * swiglu(rms_norm(h, norm_g[i, 2]), ffn_w_gu[i, 1], ffn_w_down[i, 1])
    return rms_norm(h[:, N_META:], final_norm_g)
```

```python
import math
from contextlib import ExitStack

import numpy as np
import ml_dtypes
import concourse.bass as bass
import concourse.mybir as mybir
from concourse.bass_utils import run_bass_kernel_spmd

F32 = mybir.dt.float32
BF16 = mybir.dt.bfloat16
AF = mybir.ActivationFunctionType
ALU = mybir.AluOpType
AX = mybir.AxisListType

D = 1024
NMETA = 16
DFF = 2816
EPS = 1e-6
DEPTH = 4
ATTN_IN = 3656
NEG = -1.0e30
RESET_LIMIT = 700


ALL_RES = []


class Res:
    __slots__ = ("last_w", "readers", "parent", "kids")

    def __init__(self, parent=None):
        ALL_RES.append(self)
        self.last_w = None
        self.readers = {}
        self.parent = parent
        self.kids = []
        if parent is not None:
            parent.kids.append(self)

    def related(self):
        out = [self]
        p = self.parent
        while p is not None:
            out.append(p)
            p = p.parent
        stack = list(self.kids)
        while stack:
            k = stack.pop()
            out.append(k)
            stack.extend(k.kids)
        return out


class Tile:
    def __init__(self, K, handle, name):
        self.K = K
        self.h = handle
        self.name = name
        self.res = Res()
        self.subs = {}
        self.slots = {}
        self.dsem = None
        self.dcnt = 0

    def __getitem__(self, idx):
        return self.h[idx]

    def slot(self, key):
        sl = self.slots.get(key)
        if sl is None:
            sl = Tile(self.K, self.h, f"{self.name}_s{key}")
            self.slots[key] = sl
            self.K.phase_tiles.append(sl)
        return sl

    def sub(self, key):
        r = self.subs.get(key)
        if r is None:
            r = Res(self.res)
            self.subs[key] = r
        return r


class Eng:
    def __init__(self, K, name, eng, self_dep):
        self.K = K
        self.name = name
        self.eng = eng
        self.sem = K.nc.alloc_semaphore("es_" + name)
        self.cnt = 0
        self.seen = {}
        self.self_dep = self_dep


class Kb:
    def __init__(self, nc):
        self.nc = nc
        fs = list(nc.free_semaphores)
        nc.gpsimd.sem_clear(range(min(fs), max(fs) + 1))
        self.pe = Eng(self, "pe", nc.tensor, False)
        self.act = Eng(self, "act", nc.scalar, True)
        self.dve = Eng(self, "dve", nc.vector, True)
        self.pool = Eng(self, "pool", nc.gpsimd, True)
        self.sp = Eng(self, "sp", nc.sync, True)
        self.engs = [self.pe, self.act, self.dve, self.pool, self.sp]
        self.sems = {}
        for e in self.engs:
            self.sems[id(e.sem)] = (e.sem, e)
        self.dram_res = {}
        self.phase_tiles = []
        self.n_ops = 0
        self.free_hw = []
        self.sw_holders = []
        self.sw_idx = 0
        self.uid = 0
        for e in self.engs:
            nc.gpsimd.sem_clear(e.sem)
        nc.all_engine_barrier()

    def tile(self, st, name, shape, dtype):
        self.uid += 1
        name = f"{name}_{self.uid}"
        h = st.enter_context(self.nc.sbuf_tensor(name, list(shape), dtype))
        t = Tile(self, h, name)
        self.phase_tiles.append(t)
        return t

    def psum(self, st, name, shape=(128, 512), dtype=F32):
        self.uid += 1
        name = f"{name}_{self.uid}"
        h = st.enter_context(self.nc.psum_tensor(name, list(shape), dtype))
        return Tile(self, h, name)

    def swslot(self):
        if self.sw_idx == len(self.sw_holders):
            hld = Tile(self, None, f"sw{self.sw_idx}")
            hld.dsem = self.nc.alloc_semaphore(f"ds_sw{self.sw_idx}")
            hld.dsem_sw = True
            self.sems[id(hld.dsem)] = (hld.dsem, hld)
            self.sw_holders.append(hld)
        hld = self.sw_holders[self.sw_idx]
        self.sw_idx += 1
        return hld

    def dres(self, *key):
        r = self.dram_res.get(key)
        if r is None:
            r = Res()
            self.dram_res[key] = r
        return r

    def _need(self, reads, writes):
        need = {}

        def add(rec):
            if rec is None:
                return
            k, v = rec
            if need.get(k, 0) < v:
                need[k] = v

        for r in reads:
            for x in r.related():
                add(x.last_w)
        for w in writes:
            for x in w.related():
                add(x.last_w)
                for k, v in x.readers.items():
                    add((k, v))
        return need

    def _wait(self, E, need):
        for k, v in need.items():
            sem, owner = self.sems[k]
            if owner is E and not E.self_dep:
                continue
            if E.seen.get(k, 0) >= v:
                continue
            E.eng.wait_ge(sem, v)
            E.seen[k] = v

    @staticmethod
    def _resl(xs):
        out = []
        for x in xs:
            out.append(x.res if isinstance(x, Tile) else x)
        return out

    def op(self, E, fn, reads=(), writes=(), sig=True):
        reads = self._resl(reads)
        writes = self._resl(writes)
        self._wait(E, self._need(reads, writes))
        ins = fn()
        if sig:
            E.cnt += 1
            ins.then_inc(E.sem, 1)
            rec = (id(E.sem), E.cnt)
        else:
            rec = (id(E.sem), E.cnt + 1)
        for r in reads:
            if r.readers.get(rec[0], 0) < rec[1]:
                r.readers[rec[0]] = rec[1]
        for w in writes:
            w.last_w = rec
            w.readers = {}
        self.n_ops += 1
        return ins

    def mm(self, out, lhsT, rhs, start, stop, reads, writes):
        nc = self.nc
        return self.op(self.pe, lambda: nc.tensor.matmul(out=out, lhsT=lhsT, rhs=rhs, start=start, stop=stop),
                       reads=reads, writes=writes, sig=stop)

    def dma(self, E, out, in_, stile, reads=(), writes=(), **kw):
        reads = self._resl(reads)
        writes = self._resl(writes)
        if stile.dsem is None:
            assert E is not self.pool, "gpsimd DMAs must use K.swslot() holders"
            stile.dsem = self.free_hw.pop() if self.free_hw else self.nc.alloc_semaphore("ds_" + stile.name)
            stile.dsem_sw = False
            self.sems[id(stile.dsem)] = (stile.dsem, stile)
        assert stile.dsem_sw == (E is self.pool)
        self._wait(E, self._need(reads, writes))
        ins = E.eng.dma_start(out=out, in_=in_, **kw)
        stile.dcnt += 16
        ins.then_inc(stile.dsem, 16)
        rec = (id(stile.dsem), stile.dcnt)
        for r in reads:
            if r.readers.get(rec[0], 0) < rec[1]:
                r.readers[rec[0]] = rec[1]
        for w in writes:
            w.last_w = rec
            w.readers = {}
        self.n_ops += 1
        return ins

    def reset(self):
        nc = self.nc
        sp = self.sp
        for e in self.engs:
            if e is not sp and e.cnt > 0 and sp.seen.get(id(e.sem), 0) < e.cnt:
                sp.eng.wait_ge(e.sem, e.cnt)
        for t in self.phase_tiles + self.sw_holders:
            if t.dsem is not None and t.dcnt > 0:
                sp.eng.wait_ge(t.dsem, t.dcnt)
        nc.all_engine_barrier()
        for e in self.engs:
            nc.gpsimd.sem_clear(e.sem)
            e.cnt = 0
            e.seen = {}
        for t in self.phase_tiles:
            if t.dsem is not None:
                nc.gpsimd.sem_clear(t.dsem)
                t.dcnt = 0
        nc.all_engine_barrier()
        for r in ALL_RES:
            r.last_w = None
            r.readers = {}

    def maybe_reset(self, limit=RESET_LIMIT):
        if max(e.cnt for e in self.engs) > limit:
            self.reset()

    def end_phase(self):
        self.reset()
        dsems = []
        for t in self.phase_tiles:
            if t.dsem is not None:
                dsems.append((t.dsem, t.dsem_sw))
                del self.sems[id(t.dsem)]
                t.dsem = None
        del ALL_RES[:]
        for s, sw in dsems:
            self.free_hw.append(s)
        self.sw_idx = 0
        self.phase_tiles = []
        self.dram_res = {}


def token_blocks(S, NB):
    blocks = [(0, NMETA)]
    for i in range(S // NB):
        blocks.append((NMETA + i * NB, NB))
    return blocks


class Prog:
    def __init__(self, S, plan):
        self.S = S
        self.T = S + NMETA
        self.plan = plan
        nc = bass.Bass("TRN2", target_bir_lowering=False)
        self.nc = nc
        T = self.T

        def din(name, shape, dt=F32):
            return nc.dram_tensor(name, list(shape), dt, kind="ExternalInput").ap()

        self.xin = din("xT", [D, T])
        self.norm_gc = din("norm_gc", [128, DEPTH * 3 * 8])
        self.fin_gc = din("fin_gc", [128, 8])
        self.w_gu = din("ffn_w_gu", [DEPTH, 2, D, 2 * DFF])
        self.w_dn = din("ffn_w_down", [DEPTH, 2, DFF, D])
        self.attn_w_in = din("attn_w_in", [2, D, ATTN_IN])
        self.attn_w_out = din("attn_w_out", [2, D, D])
        self.idx_lnc = din("idx_lnc", [64, 4])
        self.dlam_bc = din("dlam_bc", [128, 2 * 256])
        self.subln_c = din("subln_c", [128, 2])
        self.rec_w_in = din("rec_w_in", [2, D, 2 * D])
        self.rec_w_out = din("rec_w_out", [2, D, D])
        self.rec_gate_w = din("rec_gate_w", [2, 2, 4, 256, 256])
        self.rec_cols = din("rec_cols", [128, 2 * 8 * 8])
        self.c_ident_bf = din("c_ident_bf", [128, 128], BF16)
        self.c_negmask = din("c_negmask", [128, 128])
        self.c_cmaskT = din("c_cmaskT", [128, 4 * 512], BF16)
        self.c_cmeta = din("c_cmeta", [16, 16], BF16)
        self.yT = nc.dram_tensor("yT", [D, S], F32, kind="ExternalOutput").ap()
        self.hT = nc.dram_tensor("hT", [D, T], F32).ap()
        self.TOPK = min(256, S // 4)

        def scr(name, shape, dt=BF16):
            return nc.dram_tensor(name, list(shape), dt).ap()

        self.qaT = scr("qaT", [512, T])
        self.kaT = scr("kaT", [512, T])
        self.va = scr("va", [T, 512])
        self.qiT = scr("qiT", [512, T])
        self.kiT = scr("kiT", [64, T])
        self.wi = scr("wi", [T, 8], F32)
        self.qbT = scr("qbT", [512, T])
        self.kbT = scr("kbT", [512, T])
        self.vb = scr("vb", [T, 512])
        self.mixA = scr("mixA", [512, T])
        self.mixB = scr("mixB", [512, T])
        self.K = Kb(nc)
        self.build()

    def build(self):
        src = self.xin
        for ph in self.plan:
            kind = ph[0]
            if kind == "ffn":
                self.ffn_phase(src, ph[1], ph[2])
                src = self.hT
            elif kind == "copy":
                self.copy_phase(src)
                src = self.hT
            elif kind == "rec":
                self.rec_phase(src, ph[1])
                src = self.hT
            elif kind == "aproj":
                self.attn_proj_phase(src, ph[1])
            elif kind == "dsa":
                self.dsa_phase(ph[1])
            elif kind == "diff":
                self.diff_phase(ph[1])
            elif kind == "aout":
                self.attn_out_phase(src, ph[1])
                src = self.hT
            elif kind == "attn":
                self.attn_proj_phase(src, ph[1])
                self.dsa_phase(ph[1])
                self.diff_phase(ph[1])
                self.attn_out_phase(src, ph[1])
                src = self.hT
            else:
                raise ValueError(kind)
        self.final_phase(src)

    def rms_rstd(self, st_tiles, X, n, sqt, ss, rstd, ones):
        K = self.K
        for c in range(8):
            s = sqt[c % 2]
            K.op(K.act, lambda s=s, c=c: K.nc.scalar.activation(out=s[:, :n], in_=X[:, c, :n], func=AF.Square),
                 reads=[X.sub(c)], writes=[s])
            K.op(K.pe, lambda s=s, c=c: K.nc.tensor.matmul(out=ss[:, :n], lhsT=ones[:, :], rhs=s[:, :n],
                                                          start=(c == 0), stop=(c == 7)),
                 reads=[s, ones], writes=[ss])
        K.op(K.act, lambda: K.nc.scalar.activation(out=rstd[:, :n], in_=ss[:, :n], func=AF.Sqrt,
                                                   bias=self.eps_t[:, 0:1], scale=1.0 / D),
             reads=[ss, self.eps_t], writes=[rstd])
        K.op(K.dve, lambda: K.nc.vector.reciprocal(out=rstd[:, :n], in_=rstd[:, :n]), reads=[rstd], writes=[rstd])

    def consts(self, st):
        K = self.K
        nc = self.nc
        self.ones_f = K.tile(st, "ones_f", [128, 128], F32)
        self.ones_b = K.tile(st, "ones_b", [128, 128], BF16)
        self.eps_t = K.tile(st, "eps_t", [128, 1], F32)
        self.one_t = K.tile(st, "one_t", [128, 1], F32)
        K.op(K.dve, lambda: nc.vector.memset(self.one_t[:, :], 1.0), writes=[self.one_t])
        K.op(K.dve, lambda: nc.vector.memset(self.ones_f[:, :], 1.0), writes=[self.ones_f])
        K.op(K.dve, lambda: nc.vector.memset(self.ones_b[:, :], 1.0), writes=[self.ones_b])
        K.op(K.dve, lambda: nc.vector.memset(self.eps_t[:, :], EPS), writes=[self.eps_t])

    def load_cols(self, st, name, src_ap, ncols, parts=128):
        K = self.K
        t = K.tile(st, name, [parts, ncols], F32)
        K.dma(K.sp, t[:, :], src_ap, t, writes=[t])
        return t

    def wload(self, t, dst_ap, src_ap, writes):
        K = self.K
        K.dma(K.pool, dst_ap, src_ap, t, writes=writes, max_dma_last_dim=4096)

    def ffn_phase(self, src, li, fi):
        K = self.K
        nc = self.nc
        NB = 256
        blocks = token_blocks(self.S, NB)
        with ExitStack() as st:
            self.consts(st)
            wgu = K.tile(st, "wgu", [128, 8, 2 * DFF], BF16)
            wd = K.tile(st, "wd", [128, 22, D], BF16)
            ni = li * 3 + (0 if fi == 0 else 2)
            gcol = self.load_cols(st, "gcol", self.norm_gc[:, ni * 8:(ni + 1) * 8], 8)
            for k in range(8):
                self.wload(K.swslot(), wgu[:, k, :], self.w_gu[li, fi, k * 128:(k + 1) * 128, :], [wgu.sub(k)])
            wdv = self.w_dn[li, fi].rearrange("(j p) m -> p j m", p=128)
            for j0 in range(0, 22, 2):
                self.wload(K.swslot(), wd[:, j0:j0 + 2, :], wdv[:, j0:j0 + 2, :], [wd.sub(j0), wd.sub(j0 + 1)])
            xt = [K.tile(st, f"xt{i}", [128, 8, NB], F32) for i in range(2)]
            sqt = [K.tile(st, f"sq{i}", [128, NB], F32) for i in range(2)]
            rstd = K.tile(st, "rstd", [128, NB], F32)
            xn = K.tile(st, "xn", [128, 8, NB], BF16)
            sg = [K.tile(st, f"sg{i}", [128, NB], BF16) for i in range(2)]
            hh = K.tile(st, "hh", [128, 22, NB], BF16)
            ss = K.psum(st, "ss")
            pg = [K.psum(st, f"pg{i}") for i in range(2)]
            pu = [K.psum(st, f"pu{i}") for i in range(2)]
            po = [K.psum(st, f"po{i}") for i in range(2)]
            srcv = src.rearrange("(c p) t -> p c t", p=128)
            dstv = self.hT.rearrange("(c p) t -> p c t", p=128)
            for b, (t0, n) in enumerate(blocks):
                K.maybe_reset()
                X = xt[b % 2]
                K.dma(K.sp, X[:, :, :n], srcv[:, :, t0:t0 + n], X, reads=[K.dres("h", b)], writes=[X])
                self.rms_rstd(None, X, n, sqt, ss, rstd, self.ones_f)
                for c in range(8):
                    K.op(K.dve, lambda c=c: nc.vector.scalar_tensor_tensor(
                        out=xn[:, c, :n], in0=X[:, c, :n], scalar=gcol[:, c:c + 1], in1=rstd[:, :n],
                        op0=ALU.mult, op1=ALU.mult), reads=[X.sub(c), gcol, rstd], writes=[xn.sub(c)])
                for j in range(22):
                    G = pg[j % 2]
                    U = pu[j % 2]
                    for k in range(8):
                        K.op(K.pe, lambda k=k, j=j, G=G: nc.tensor.matmul(
                            out=G[:, :n], lhsT=wgu[:, k, j * 128:(j + 1) * 128], rhs=xn[:, k, :n],
                            start=(k == 0), stop=(k == 7)), reads=[wgu.sub(k), xn.sub(k)], writes=[G], sig=(k == 7))
                    for k in range(8):
                        K.op(K.pe, lambda k=k, j=j, U=U: nc.tensor.matmul(
                            out=U[:, :n], lhsT=wgu[:, k, DFF + j * 128:DFF + (j + 1) * 128], rhs=xn[:, k, :n],
                            start=(k == 0), stop=(k == 7)), reads=[wgu.sub(k), xn.sub(k)], writes=[U], sig=(k == 7))
                    s = sg[j % 2]
                    K.op(K.act, lambda s=s, G=G: nc.scalar.activation(out=s[:, :n], in_=G[:, :n], func=AF.Silu),
                         reads=[G], writes=[s])
                    K.op(K.dve, lambda s=s, U=U, j=j: nc.vector.tensor_tensor(
                        out=hh[:, j, :n], in0=s[:, :n], in1=U[:, :n], op=ALU.mult),
                        reads=[s, U], writes=[hh.sub(j)])
                for m in range(8):
                    P = po[m % 2]
                    for j in range(22):
                        K.op(K.pe, lambda j=j, m=m, P=P: nc.tensor.matmul(
                            out=P[:, :n], lhsT=wd[:, j, m * 128:(m + 1) * 128], rhs=hh[:, j, :n],
                            start=(j == 0), stop=(j == 21)), reads=[wd.sub(j), hh.sub(j)], writes=[P], sig=(j == 21))
                    K.op(K.dve, lambda m=m, P=P, X=X: nc.vector.scalar_tensor_tensor(
                        out=X[:, m, :n], in0=P[:, :n], scalar=0.5, in1=X[:, m, :n],
                        op0=ALU.mult, op1=ALU.add), reads=[P, X.sub(m)], writes=[X.sub(m)])
                K.dma(K.sp, dstv[:, :, t0:t0 + n], X[:, :, :n], X, reads=[X], writes=[K.dres("h", b)])
            K.end_phase()


    def load_norm(self, X, xn, gcol, srcv, t0, n, b, sqt, ss, rstd):
        K = self.K
        nc = self.nc
        K.dma(K.sp, X[:, :, :n], srcv[:, :, t0:t0 + n], X, reads=[K.dres("h", b)], writes=[X])
        self.rms_rstd(None, X, n, sqt, ss, rstd, self.ones_f)
        for c in range(8):
            K.op(K.dve, lambda: nc.vector.scalar_tensor_tensor(
                out=xn[:, c, :n], in0=X[:, c, :n], scalar=gcol[:, c:c + 1], in1=rstd[:, :n],
                op0=ALU.mult, op1=ALU.mult), reads=[X.sub(c), gcol, rstd], writes=[xn.sub(c)])

    def rec_phase(self, src, li):
        K = self.K
        nc = self.nc
        j = li // 2
        NB = 256
        blocks = token_blocks(self.S, NB)
        with ExitStack() as st:
            self.consts(st)
            win = K.tile(st, "rwin", [128, 8, 2 * D], BF16)
            gw = K.tile(st, "rgw", [128, 16, 256], BF16)
            wout = K.tile(st, "rwout", [128, 8, D], BF16)
            gcol = self.load_cols(st, "gcol", self.norm_gc[:, (li * 3 + 1) * 8:(li * 3 + 2) * 8], 8)
            rc = self.load_cols(st, "rc", self.rec_cols[:, j * 64:(j + 1) * 64], 64)
            for k in range(8):
                self.wload(K.swslot(), win[:, k, :], self.rec_w_in[j, k * 128:(k + 1) * 128, :], [win.sub(k)])
            gwv = self.rec_gate_w[j].rearrange("g n (ic p) jj -> p (g n ic) jj", p=128)
            for q in range(2):
                self.wload(K.swslot(), gw[:, q * 8:(q + 1) * 8, :], gwv[:, q * 8:(q + 1) * 8, :], [gw.sub(q)])
            wov = self.rec_w_out[j].rearrange("(k p) m -> p k m", p=128)
            for q in range(2):
                self.wload(K.swslot(), wout[:, q * 4:(q + 1) * 4, :], wov[:, q * 4:(q + 1) * 4, :], [wout.sub(q)])
            clam = K.tile(st, "clam", [128, 8], F32)
            K.op(K.act, lambda: nc.scalar.activation(out=clam[:, :], in_=rc[:, 56:64], func=AF.Exp, scale=-1.0),
                 reads=[rc], writes=[clam])
            K.op(K.dve, lambda: nc.vector.tensor_scalar(out=clam[:, :], in0=clam[:, :], scalar1=1.0, scalar2=None,
                                                        op0=ALU.add), reads=[clam], writes=[clam])
            K.op(K.act, lambda: nc.scalar.activation(out=clam[:, :], in_=clam[:, :], func=AF.Ln),
                 reads=[clam], writes=[clam])
            K.op(K.dve, lambda: nc.vector.tensor_scalar(out=clam[:, :], in0=clam[:, :], scalar1=-8.0, scalar2=None,
                                                        op0=ALU.mult), reads=[clam], writes=[clam])
            xt = [K.tile(st, f"xt{i}", [128, 8, NB], F32) for i in range(2)]
            sqt = [K.tile(st, f"sq{i}", [128, NB], F32) for i in range(2)]
            rstd = K.tile(st, "rstd", [128, NB], F32)
            xn = K.tile(st, "xn", [128, 8, NB], BF16)
            yb = K.tile(st, "yb", [128, 8, NB], BF16)
            xb = [K.tile(st, f"xb{i}", [128, 8, 3 + NB], F32) for i in range(2)]
            xc = K.tile(st, "xc", [128, 8, NB], F32)
            xcb = K.tile(st, "xcb", [128, 8, NB], BF16)
            t1 = [K.tile(st, f"t1{i}", [128, NB], F32) for i in range(2)]
            gx = K.tile(st, "gx", [128, 8, NB], F32)
            at = K.tile(st, "at", [128, 8, NB], F32)
            ga = [K.tile(st, f"ga{i}", [128, NB], F32) for i in range(2)]
            mu = [K.tile(st, f"mu{i}", [128, NB], F32) for i in range(2)]
            ut = K.tile(st, "ut", [128, 8, NB], F32)
            hs = [K.tile(st, f"hs{i}", [128, 8, NB], F32) for i in range(2)]
            zb = K.tile(st, "zb", [128, 8, NB], BF16)
            ss = K.psum(st, "ss")
            py = [K.psum(st, f"py{i}") for i in range(2)]
            pgt = [K.psum(st, f"pgt{i}") for i in range(2)]
            po = [K.psum(st, f"po{i}") for i in range(2)]
            srcv = src.rearrange("(c p) t -> p c t", p=128)
            dstv = self.hT.rearrange("(c p) t -> p c t", p=128)
            K.op(K.dve, lambda: nc.vector.memset(xb[0][:, :, 0:3], 0.0), writes=[xb[0]])
            nprev = 0
            for b, (t0, n) in enumerate(blocks):
                K.maybe_reset()
                X = xt[b % 2]
                XB = xb[b % 2]
                XBn = xb[(b + 1) % 2]
                HS = hs[b % 2]
                HSp = hs[(b + 1) % 2]
                self.load_norm(X, xn, gcol, srcv, t0, n, b, sqt, ss, rstd)
                for m in range(8):
                    P = py[m % 2]
                    T1 = t1[m % 2]
                    for k in range(8):
                        K.op(K.pe, lambda: nc.tensor.matmul(out=P[:, :n], lhsT=win[:, k, m * 128:(m + 1) * 128],
                                                            rhs=xn[:, k, :n], start=(k == 0), stop=(k == 7)),
                             reads=[win.sub(k), xn.sub(k)], writes=[P], sig=(k == 7))
                    K.op(K.act, lambda: nc.scalar.activation(out=T1[:, :n], in_=P[:, :n], func=AF.Square),
                         reads=[P], writes=[T1])
                    K.op(K.dve, lambda: nc.vector.tensor_scalar(out=T1[:, :n], in0=T1[:, :n], scalar1=0.044715,
                                                                scalar2=1.0, op0=ALU.mult, op1=ALU.add),
                         reads=[T1], writes=[T1])
                    K.op(K.dve, lambda: nc.vector.tensor_tensor(out=T1[:, :n], in0=T1[:, :n], in1=P[:, :n], op=ALU.mult),
                         reads=[T1, P], writes=[T1])
                    K.op(K.act, lambda: nc.scalar.activation(out=T1[:, :n], in_=T1[:, :n], func=AF.Sigmoid,
                                                             scale=1.5957691216057308), reads=[T1], writes=[T1])
                    K.op(K.dve, lambda: nc.vector.tensor_tensor(out=yb[:, m, :n], in0=T1[:, :n], in1=P[:, :n], op=ALU.mult),
                         reads=[T1, P], writes=[yb.sub(m)])
                for m in range(8):
                    P = py[m % 2]
                    for k in range(8):
                        K.op(K.pe, lambda: nc.tensor.matmul(out=P[:, :n], lhsT=win[:, k, D + m * 128:D + (m + 1) * 128],
                                                            rhs=xn[:, k, :n], start=(k == 0), stop=(k == 7)),
                             reads=[win.sub(k), xn.sub(k)], writes=[P], sig=(k == 7))
                    K.op(K.act, lambda: nc.scalar.activation(out=XB[:, m, 3:3 + n], in_=P[:, :n], func=AF.Copy),
                         reads=[P], writes=[XB.sub(m)])
                for m in range(8):
                    K.op(K.dve, lambda: nc.vector.tensor_scalar(
                        out=xc[:, m, :n], in0=XB[:, m, 0:n], scalar1=rc[:, m:m + 1], scalar2=rc[:, 32 + m:33 + m],
                        op0=ALU.mult, op1=ALU.add), reads=[XB.sub(m), rc], writes=[xc.sub(m)])
                    for w in range(1, 4):
                        K.op(K.dve, lambda: nc.vector.scalar_tensor_tensor(
                            out=xc[:, m, :n], in0=XB[:, m, w:w + n], scalar=rc[:, w * 8 + m:w * 8 + m + 1],
                            in1=xc[:, m, :n], op0=ALU.mult, op1=ALU.add), reads=[XB.sub(m), rc, xc.sub(m)],
                            writes=[xc.sub(m)])
                    K.op(K.act, lambda: nc.scalar.activation(out=xcb[:, m, :n], in_=xc[:, m, :n], func=AF.Copy),
                         reads=[xc.sub(m)], writes=[xcb.sub(m)])
                K.op(K.act, lambda: nc.scalar.activation(out=XBn[:, :, 0:3], in_=XB[:, :, n:n + 3], func=AF.Copy),
                     reads=[XB], writes=[XBn])
                for oc in range(8):
                    nb_, jc = oc // 2, oc % 2
                    P0 = pgt[0]
                    P1 = pgt[1]
                    GA = ga[oc % 2]
                    MU = mu[oc % 2]
                    for g, P in ((0, P0), (1, P1)):
                        for ic in range(2):
                            K.op(K.pe, lambda: nc.tensor.matmul(
                                out=P[:, :n], lhsT=gw[:, (g * 4 + nb_) * 2 + ic, jc * 128:(jc + 1) * 128],
                                rhs=xcb[:, nb_ * 2 + ic, :n], start=(ic == 0), stop=(ic == 1)),
                                reads=[gw, xcb.sub(nb_ * 2 + ic)], writes=[P], sig=(ic == 1))
                    K.op(K.act, lambda: nc.scalar.activation(out=gx[:, oc, :n], in_=P0[:, :n], func=AF.Sigmoid,
                                                             bias=rc[:, 40 + oc:41 + oc]), reads=[P0, rc], writes=[gx.sub(oc)])
                    K.op(K.act, lambda: nc.scalar.activation(out=GA[:, :n], in_=P1[:, :n], func=AF.Sigmoid,
                                                             bias=rc[:, 48 + oc:49 + oc]), reads=[P1, rc], writes=[GA])
                    K.op(K.act, lambda: nc.scalar.activation(out=at[:, oc, :n], in_=GA[:, :n], func=AF.Exp,
                                                             scale=clam[:, oc:oc + 1]), reads=[GA, clam], writes=[at.sub(oc)])
                    K.op(K.act, lambda: nc.scalar.activation(out=MU[:, :n], in_=at[:, oc, :n], func=AF.Square),
                         reads=[at.sub(oc)], writes=[MU])
                    K.op(K.act, lambda: nc.scalar.activation(out=MU[:, :n], in_=MU[:, :n], func=AF.Sqrt,
                                                             bias=self.one_t[:, 0:1], scale=-1.0),
                         reads=[MU, self.one_t], writes=[MU])
                    K.op(K.dve, lambda: nc.vector.tensor_tensor(out=ut[:, oc, :n], in0=gx[:, oc, :n], in1=xc[:, oc, :n],
                                                                op=ALU.mult), reads=[gx.sub(oc), xc.sub(oc)], writes=[ut.sub(oc)])
                    K.op(K.dve, lambda: nc.vector.tensor_tensor(out=ut[:, oc, :n], in0=ut[:, oc, :n], in1=MU[:, :n],
                                                                op=ALU.mult), reads=[ut.sub(oc), MU], writes=[ut.sub(oc)])
                    init = 0.0 if b == 0 else HSp[:, oc, nprev - 1:nprev]
                    K.op(K.dve, lambda: nc.vector.tensor_tensor_scan(
                        out=HS[:, oc, :n], data0=at[:, oc, :n], data1=ut[:, oc, :n], initial=init,
                        op0=ALU.mult, op1=ALU.add), reads=[at.sub(oc), ut.sub(oc), HSp.sub(oc)], writes=[HS.sub(oc)])
                    K.op(K.dve, lambda: nc.vector.tensor_tensor(out=zb[:, oc, :n], in0=HS[:, oc, :n], in1=yb[:, oc, :n],
                                                                op=ALU.mult), reads=[HS.sub(oc), yb.sub(oc)], writes=[zb.sub(oc)])
                for m in range(8):
                    P = po[m % 2]
                    for k in range(8):
                        K.op(K.pe, lambda: nc.tensor.matmul(out=P[:, :n], lhsT=wout[:, k, m * 128:(m + 1) * 128],
                                                            rhs=zb[:, k, :n], start=(k == 0), stop=(k == 7)),
                             reads=[wout, zb.sub(k)], writes=[P], sig=(k == 7))
                    K.op(K.dve, lambda: nc.vector.tensor_tensor(out=X[:, m, :n], in0=P[:, :n], in1=X[:, m, :n], op=ALU.add),
                         reads=[P, X.sub(m)], writes=[X.sub(m)])
                K.dma(K.sp, dstv[:, :, t0:t0 + n], X[:, :, :n], X, reads=[X], writes=[K.dres("h", b)])
                nprev = n
            K.end_phase()

    def attn_proj_phase(self, src, li):
        K = self.K
        nc = self.nc
        j = li // 2
        NB = 256
        blocks = token_blocks(self.S, NB)
        with ExitStack() as st:
            self.consts(st)
            win = K.tile(st, "awin", [128, 8, ATTN_IN], BF16)
            gcol = self.load_cols(st, "gcol", self.norm_gc[:, (li * 3 + 1) * 8:(li * 3 + 2) * 8], 8)
            lnc = self.load_cols(st, "lnc", self.idx_lnc[:, j * 2:j * 2 + 2], 2, parts=64)
            for k in range(8):
                self.wload(K.swslot(), win[:, k, :], self.attn_w_in[j, k * 128:(k + 1) * 128, :], [win.sub(k)])
            xt = [K.tile(st, f"xt{i}", [128, 8, NB], F32) for i in range(2)]
            sqt = [K.tile(st, f"sq{i}", [128, NB], F32) for i in range(2)]
            rstd = K.tile(st, "rstd", [128, NB], F32)
            xn = K.tile(st, "xn", [128, 8, NB], BF16)
            fo = [K.tile(st, f"fo{i}", [128, 4, NB], BF16) for i in range(2)]
            kf = K.tile(st, "kf", [64, NB], F32)
            kx = K.tile(st, "kx", [64, NB], F32)
            ksq = K.tile(st, "ksq", [64, NB], F32)
            krs = K.tile(st, "krs", [64, NB], F32)
            ko = [K.tile(st, f"ko{i}", [64, NB], BF16) for i in range(2)]
            vo = [K.tile(st, f"vo{i}", [128, 512], BF16) for i in range(4)]
            wo = [K.tile(st, f"wo{i}", [128, 8], F32) for i in range(2)]
            ss = K.psum(st, "ss")
            pf = [K.psum(st, f"pf{i}") for i in range(2)]
            pk = K.psum(st, "pk")
            pk2 = K.psum(st, "pk2")
            pt = [K.psum(st, f"pt{i}") for i in range(2)]
            srcv = src.rearrange("(c p) t -> p c t", p=128)
            groups = [(self.qaT, 0), (self.kaT, 512), (self.qiT, 1536), (self.qbT, 2120), (self.kbT, 2632)]
            gi = 0
            vi = 0
            wi_i = 0
            for b, (t0, n) in enumerate(blocks):
                K.maybe_reset()
                X = xt[b % 2]
                self.load_norm(X, xn, gcol, srcv, t0, n, b, sqt, ss, rstd)
                for (dst, c0) in groups:
                    FO = fo[gi % 2]
                    gi += 1
                    for m in range(4):
                        P = pf[m % 2]
                        for k in range(8):
                            K.mm(P[:, :n], win[:, k, c0 + m * 128:c0 + (m + 1) * 128], xn[:, k, :n], k == 0, k == 7,
                                 [win.sub(k), xn.sub(k)], [P])
                        if m % 2 == 0:
                            K.op(K.act, lambda: nc.scalar.activation(out=FO[:, m, :n], in_=P[:, :n], func=AF.Copy),
                                 reads=[P], writes=[FO.sub(m)])
                        else:
                            K.op(K.dve, lambda: nc.vector.tensor_copy(out=FO[:, m, :n], in_=P[:, :n]),
                                 reads=[P], writes=[FO.sub(m)])
                    K.dma(K.sp, dst.rearrange("(m p) t -> p m t", p=128)[:, :, t0:t0 + n], FO[:, :, :n], FO, reads=[FO])
                for k in range(8):
                    K.mm(pk[:64, :n], win[:, k, 2048:2112], xn[:, k, :n], k == 0, k == 7, [win.sub(k), xn.sub(k)], [pk])
                K.op(K.act, lambda: nc.scalar.activation(out=kf[:, :n], in_=pk[:64, :n], func=AF.Copy), reads=[pk], writes=[kf])
                K.mm(pk2[:64, :n], self.ones_f[:64, :64], kf[:, :n], True, True, [self.ones_f, kf], [pk2])
                K.op(K.dve, lambda: nc.vector.scalar_tensor_tensor(out=kx[:, :n], in0=pk2[:64, :n], scalar=-1.0 / 64,
                                                                   in1=kf[:, :n], op0=ALU.mult, op1=ALU.add),
                     reads=[pk2, kf], writes=[kx])
                K.op(K.act, lambda: nc.scalar.activation(out=ksq[:, :n], in_=kx[:, :n], func=AF.Square), reads=[kx], writes=[ksq])
                K.mm(pk2[:64, :n], self.ones_f[:64, :64], ksq[:, :n], True, True, [self.ones_f, ksq], [pk2])
                K.op(K.act, lambda: nc.scalar.activation(out=krs[:, :n], in_=pk2[:64, :n], func=AF.Sqrt,
                                                         bias=self.eps_t[:64, 0:1], scale=1.0 / 64),
                     reads=[pk2, self.eps_t], writes=[krs])
                K.op(K.dve, lambda: nc.vector.reciprocal(out=krs[:, :n], in_=krs[:, :n]), reads=[krs], writes=[krs])
                K.op(K.dve, lambda: nc.vector.tensor_tensor(out=kx[:, :n], in0=kx[:, :n], in1=krs[:, :n], op=ALU.mult),
                     reads=[kx, krs], writes=[kx])
                KO = ko[b % 2]
                K.op(K.dve, lambda: nc.vector.tensor_scalar(out=KO[:, :n], in0=kx[:, :n], scalar1=lnc[:, 0:1],
                                                            scalar2=lnc[:, 1:2], op0=ALU.mult, op1=ALU.add),
                     reads=[kx, lnc], writes=[KO])
                K.dma(K.sp, self.kiT[:, t0:t0 + n], KO[:, :n], KO, reads=[KO])
                for c_lo in range(0, n, 128):
                    tn = min(128, n - c_lo)
                    for (dst, c0) in ((self.va, 1024), (self.vb, 3144)):
                        P = pt[vi % 2]
                        VO = vo[vi % 4]
                        vi += 1
                        for k in range(8):
                            K.mm(P[:tn, :512], xn[:, k, c_lo:c_lo + tn], win[:, k, c0:c0 + 512], k == 0, k == 7,
                                 [win.sub(k), xn.sub(k)], [P])
                        if vi % 2 == 0:
                            K.op(K.act, lambda: nc.scalar.activation(out=VO[:tn, :], in_=P[:tn, :512], func=AF.Copy),
                                 reads=[P], writes=[VO])
                        else:
                            K.op(K.dve, lambda: nc.vector.tensor_copy(out=VO[:tn, :], in_=P[:tn, :512]), reads=[P], writes=[VO])
                        K.dma(K.sp, dst[t0 + c_lo:t0 + c_lo + tn, :], VO[:tn, :], VO, reads=[VO])
                    WO = wo[wi_i % 2]
                    wi_i += 1
                    for k in range(8):
                        K.mm(pk2[:tn, :8], xn[:, k, c_lo:c_lo + tn], win[:, k, 2112:2120], k == 0, k == 7,
                             [win.sub(k), xn.sub(k)], [pk2])
                    K.op(K.dve, lambda: nc.vector.tensor_scalar(out=WO[:tn, :], in0=pk2[:tn, :8], scalar1=0.044194173824159216,
                                                                scalar2=None, op0=ALU.mult), reads=[pk2], writes=[WO])
                    K.dma(K.sp, self.wi[t0 + c_lo:t0 + c_lo + tn, :], WO[:tn, :], WO, reads=[WO])
            K.end_phase()

    def dsa_phase(self, li):
        K = self.K
        nc = self.nc
        S = self.S
        TOPK = float(self.TOPK)
        NKT = S // 128
        NQB = S // 512
        NIT = 18
        with ExitStack() as st:
            self.consts(st)
            ident = K.tile(st, "ident", [128, 128], BF16)
            K.dma(K.sp, ident[:, :], self.c_ident_bf[:, :], ident, writes=[ident])
            negm = K.tile(st, "negm", [128, 128], F32)
            K.dma(K.sp, negm[:, :], self.c_negmask[:, :], negm, writes=[negm])
            cmeta = K.tile(st, "cmeta", [16, 16], BF16)
            K.dma(K.sp, cmeta[:, :], self.c_cmeta[:, :], cmeta, writes=[cmeta])
            ki_sb = K.tile(st, "ki_sb", [64, S], BF16)
            K.dma(K.sp, ki_sb[:, :], self.kiT[:, NMETA:], ki_sb, writes=[ki_sb])
            va_sb = K.tile(st, "va_sb", [128, NKT, 512], BF16)
            vav = self.va[NMETA:, :].rearrange("(kt p) c -> p kt c", p=128)
            for g0 in range(0, NKT, 8):
                g1 = min(NKT, g0 + 8)
                K.dma(K.sp, va_sb[:, g0:g1, :], vav[:, g0:g1, :], va_sb.slot(g0), writes=[va_sb.sub(g0 // 8)])
            vmeta = K.tile(st, "vmeta", [16, 512], BF16)
            K.dma(K.sp, vmeta[:, :], self.va[0:NMETA, :], vmeta, writes=[vmeta])
            maskT = [K.tile(st, f"maskT{i}", [128, NKT, 512], BF16) for i in range(2)]
            score = K.tile(st, "score", [128, S], F32)
            mask01 = K.tile(st, "mask01", [128, S], BF16)
            rl = [K.tile(st, f"rl{i}", [128, 512], F32) for i in range(2)]
            qi_sb = [K.tile(st, f"qi{i}", [64, 8, 128], BF16) for i in range(2)]
            wi_sb = [K.tile(st, f"wi{i}", [128, 8], F32) for i in range(2)]
            smax = K.tile(st, "smax", [128, 1], F32)
            lo = K.tile(st, "lo", [128, 1], F32)
            w0 = K.tile(st, "w0", [128, 1], F32)
            mid = K.tile(st, "mid", [128, 1], F32)
            cnt = K.tile(st, "cnt", [128, 1], F32)
            gg = K.tile(st, "gg", [128, 1], F32)
            qa_sb = [K.tile(st, f"qa{i}", [64, 512], BF16) for i in range(2)]
            ka_sb = [K.tile(st, f"ka{i}", [64, NMETA + S], BF16) for i in range(2)]
            et = [K.tile(st, f"et{i}", [128, 512], BF16) for i in range(2)]
            ptl = [K.tile(st, f"ptl{i}", [128, 512], BF16) for i in range(2)]
            rs = K.tile(st, "rs", [64, 512], F32)
            oh = [K.tile(st, f"oh{i}", [64, 512], BF16) for i in range(2)]
            pl = [K.psum(st, f"pl{i}") for i in range(2)]
            ptr = K.psum(st, "ptr", (128, 4, 128), BF16)
            pst = [K.psum(st, f"pst{i}") for i in range(2)]
            po = K.psum(st, "po")
            ps = K.psum(st, "ps")
            ei = [0]

            def attend(QA, KA, h, nq, key_tiles, tq):
                nt = len(key_tiles)
                for i, (k0, kn, v_ap, v_res, m_ap, m_res) in enumerate(key_tiles):
                    PST = pst[ei[0] % 2]
                    E_ = et[ei[0] % 2]
                    P_ = ptl[ei[0] % 2]
                    ei[0] += 1
                    K.mm(PST[:kn, :nq], KA[:, k0:k0 + kn], QA[:, :nq], True, True, [KA, QA], [PST])
                    K.op(K.act, lambda: nc.scalar.activation(out=E_[:kn, :nq], in_=PST[:kn, :nq], func=AF.Exp, scale=0.125),
                         reads=[PST], writes=[E_])
                    if m_ap is not None:
                        K.op(K.dve, lambda: nc.vector.tensor_tensor(out=P_[:kn, :nq], in0=E_[:kn, :nq], in1=m_ap, op=ALU.mult),
                             reads=[E_, m_res], writes=[P_])
                        R_ = P_
                    else:
                        R_ = E_
                    K.mm(po[:64, :nq], v_ap, R_[:kn, :nq], i == 0, i == nt - 1, [v_res, R_], [po])
                    K.mm(ps[:64, :nq], self.ones_b[:kn, :64], R_[:kn, :nq], i == 0, i == nt - 1, [self.ones_b, R_], [ps])
                OH = oh[h % 2]
                K.op(K.dve, lambda: nc.vector.reciprocal(out=rs[:, :nq], in_=ps[:64, :nq]), reads=[ps], writes=[rs])
                K.op(K.dve, lambda: nc.vector.tensor_tensor(out=OH[:, :nq], in0=po[:64, :nq], in1=rs[:, :nq], op=ALU.mult),
                     reads=[po, rs], writes=[OH])
                K.dma(K.sp, self.mixA[h * 64:(h + 1) * 64, tq:tq + nq], OH[:, :nq], OH, reads=[OH])

            for h in range(8):
                QA = qa_sb[h % 2]
                KA = ka_sb[h % 2]
                K.dma(K.sp, QA[:, :NMETA], self.qaT[h * 64:(h + 1) * 64, 0:NMETA], QA, writes=[QA])
                K.dma(K.sp, KA[:, :NMETA], self.kaT[h * 64:(h + 1) * 64, 0:NMETA], KA, writes=[KA])
                attend(QA, KA, h, NMETA, [(0, NMETA, vmeta[:NMETA, h * 64:(h + 1) * 64], vmeta, cmeta[:, :], cmeta)], 0)

            for qb in range(NQB):
                nkt = 4 * (qb + 1)
                MT = maskT[qb % 2]
                K.op(K.dve, lambda: nc.vector.memset(MT[:, 4 * qb:4 * qb + 4, :], 0.0), writes=[MT])
                for jq in range(4):
                    K.maybe_reset()
                    qt = 4 * qb + jq
                    nk = (qt + 1) * 128
                    tq0 = NMETA + qt * 128
                    QI = qi_sb[jq % 2]
                    WI = wi_sb[jq % 2]
                    K.dma(K.sp, QI[:, :, :], self.qiT[:, tq0:tq0 + 128].rearrange("(h d) q -> d h q", d=64), QI, writes=[QI])
                    K.dma(K.sp, WI[:, :], self.wi[tq0:tq0 + 128, :], WI, writes=[WI])
                    nch = (nk + 511) // 512
                    ri = 0
                    for h in range(8):
                        for ch in range(nch):
                            c0 = ch * 512
                            w = min(512, nk - c0)
                            PL = pl[ri % 2]
                            RL = rl[ri % 2]
                            ri += 1
                            K.mm(PL[:, :w], QI[:, h, :], ki_sb[:, c0:c0 + w], True, True, [QI, ki_sb], [PL])
                            K.op(K.act, lambda: nc.scalar.activation(out=RL[:, :w], in_=PL[:, :w], func=AF.Relu),
                                 reads=[PL], writes=[RL])
                            if h == 0:
                                K.op(K.dve, lambda: nc.vector.tensor_scalar(out=score[:, c0:c0 + w], in0=RL[:, :w],
                                                                            scalar1=WI[:, 0:1], scalar2=None, op0=ALU.mult),
                                     reads=[RL, WI], writes=[score.sub(ch)])
                            else:
                                K.op(K.dve, lambda: nc.vector.scalar_tensor_tensor(
                                    out=score[:, c0:c0 + w], in0=RL[:, :w], scalar=WI[:, h:h + 1], in1=score[:, c0:c0 + w],
                                    op0=ALU.mult, op1=ALU.add), reads=[RL, WI, score.sub(ch)], writes=[score.sub(ch)])
                    K.op(K.dve, lambda: nc.vector.tensor_reduce(out=smax[:, :], in_=score[:, :nk], axis=AX.X, op=ALU.max),
                         reads=[score], writes=[smax])
                    K.op(K.dve, lambda: nc.vector.tensor_reduce(out=lo[:, :], in_=score[:, :nk], axis=AX.X, op=ALU.min),
                         reads=[score], writes=[lo])
                    K.op(K.dve, lambda: nc.vector.tensor_tensor(out=score[:, qt * 128:(qt + 1) * 128],
                                                                in0=score[:, qt * 128:(qt + 1) * 128], in1=negm[:, :], op=ALU.add),
                         reads=[score, negm], writes=[score])
                    K.op(K.dve, lambda: nc.vector.tensor_tensor(out=w0[:, :], in0=smax[:, :], in1=lo[:, :], op=ALU.subtract),
                         reads=[smax, lo], writes=[w0])
                    for it in range(NIT):
                        c = 2.0 ** -(it + 1)
                        K.op(K.dve, lambda: nc.vector.scalar_tensor_tensor(out=mid[:, :], in0=w0[:, :], scalar=c, in1=lo[:, :],
                                                                           op0=ALU.mult, op1=ALU.add),
                             reads=[w0, lo], writes=[mid])
                        K.op(K.dve, lambda: nc.vector.tensor_scalar(out=mask01[:, :nk], in0=score[:, :nk], scalar1=mid[:, 0:1],
                                                                    scalar2=None, op0=ALU.is_ge, op1=ALU.add,
                                                                    accum_out=cnt[:, 0:1]),
                             reads=[score, mid], writes=[mask01, cnt])
                        K.op(K.dve, lambda: nc.vector.tensor_scalar(out=gg[:, :], in0=cnt[:, :], scalar1=TOPK, scalar2=c,
                                                                    op0=ALU.is_ge, op1=ALU.mult), reads=[cnt], writes=[gg])
                        K.op(K.dve, lambda: nc.vector.scalar_tensor_tensor(out=lo[:, :], in0=gg[:, :], scalar=w0[:, 0:1],
                                                                           in1=lo[:, :], op0=ALU.mult, op1=ALU.add),
                             reads=[gg, w0, lo], writes=[lo])
                    K.op(K.dve, lambda: nc.vector.tensor_scalar(out=mask01[:, :nk], in0=score[:, :nk], scalar1=lo[:, 0:1],
                                                                scalar2=None, op0=ALU.is_ge), reads=[score, lo], writes=[mask01])
                    for kt0 in range(0, qt + 1, 4):
                        g_ = min(4, qt + 1 - kt0)
                        for i in range(g_):
                            K.op(K.pe, lambda: nc.tensor.transpose(out=ptr[:, i, :], in_=mask01[:, (kt0 + i) * 128:(kt0 + i + 1) * 128],
                                                                   identity=ident[:, :]),
                                 reads=[mask01, ident], writes=[ptr], sig=(i == g_ - 1))
                        K.op(K.act, lambda: nc.scalar.activation(out=MT[:, kt0:kt0 + g_, jq * 128:(jq + 1) * 128],
                                                                 in_=ptr[:, 0:g_, :], func=AF.Copy), reads=[ptr], writes=[MT])
                tq = NMETA + qb * 512
                for h in range(8):
                    K.maybe_reset()
                    QA = qa_sb[h % 2]
                    KA = ka_sb[h % 2]
                    K.dma(K.sp, QA[:, :], self.qaT[h * 64:(h + 1) * 64, tq:tq + 512], QA, writes=[QA])
                    K.dma(K.sp, KA[:, :NMETA + nkt * 128], self.kaT[h * 64:(h + 1) * 64, 0:NMETA + nkt * 128], KA, writes=[KA])
                    tiles = [(0, NMETA, vmeta[:NMETA, h * 64:(h + 1) * 64], vmeta, None, None)]
                    for kt in range(nkt):
                        tiles.append((NMETA + kt * 128, 128, va_sb[:, kt, h * 64:(h + 1) * 64], va_sb.sub(kt // 8),
                                      MT[:, kt, :], MT))
                    attend(QA, KA, h, 512, tiles, tq)
            K.end_phase()

    def diff_phase(self, li):
        K = self.K
        nc = self.nc
        S = self.S
        j = li // 2
        lam_init = 0.8 - 0.6 * math.exp(-0.3 * li)
        NKT = S // 128
        NQB = S // 512
        with ExitStack() as st:
            self.consts(st)
            cmT = K.tile(st, "cmT", [128, 4, 512], BF16)
            K.dma(K.sp, cmT[:, :, :], self.c_cmaskT.rearrange("p (j q) -> p j q", q=512), cmT, writes=[cmT])
            cmeta = K.tile(st, "cmeta", [16, 16], BF16)
            K.dma(K.sp, cmeta[:, :], self.c_cmeta[:, :], cmeta, writes=[cmeta])
            dl = self.load_cols(st, "dl", self.dlam_bc[:, j * 256:(j + 1) * 256], 256)
            sgc2 = self.load_cols(st, "sgc", self.subln_c[:, :], 2)
            pr = K.tile(st, "pr", [128, 64], F32)
            s12 = K.tile(st, "s12", [128, 2], F32)
            neglam = K.tile(st, "neglam", [128, 1], F32)
            sgs = K.tile(st, "sgs", [128, 1], F32)
            for r in range(2):
                K.op(K.dve, lambda: nc.vector.tensor_tensor(out=pr[:, :], in0=dl[:, r * 128:r * 128 + 64],
                                                            in1=dl[:, r * 128 + 64:r * 128 + 128], op=ALU.mult),
                     reads=[dl], writes=[pr])
                K.op(K.dve, lambda: nc.vector.tensor_reduce(out=s12[:, r:r + 1], in_=pr[:, :], axis=AX.X, op=ALU.add),
                     reads=[pr], writes=[s12])
            K.op(K.act, lambda: nc.scalar.activation(out=s12[:, :], in_=s12[:, :], func=AF.Exp), reads=[s12], writes=[s12])
            K.op(K.dve, lambda: nc.vector.tensor_tensor(out=neglam[:, :], in0=s12[:, 1:2], in1=s12[:, 0:1], op=ALU.subtract),
                 reads=[s12], writes=[neglam])
            K.op(K.dve, lambda: nc.vector.tensor_scalar(out=neglam[:, :], in0=neglam[:, :], scalar1=-lam_init, scalar2=None,
                                                        op0=ALU.add), reads=[neglam], writes=[neglam])
            K.op(K.dve, lambda: nc.vector.tensor_scalar(out=sgs[:, :], in0=sgc2[:, j:j + 1], scalar1=1.0 - lam_init, scalar2=None,
                                                        op0=ALU.mult), reads=[sgc2], writes=[sgs])
            vb_sb = K.tile(st, "vb_sb", [128, NKT, 512], BF16)
            vbv = self.vb[NMETA:, :].rearrange("(kt p) c -> p kt c", p=128)
            for g0 in range(0, NKT, 8):
                g1 = min(NKT, g0 + 8)
                K.dma(K.sp, vb_sb[:, g0:g1, :], vbv[:, g0:g1, :], vb_sb.slot(g0), writes=[vb_sb.sub(g0 // 8)])
            vmeta = K.tile(st, "vbmeta", [16, 512], BF16)
            K.dma(K.sp, vmeta[:, :], self.vb[0:NMETA, :], vmeta, writes=[vmeta])
            qb_sb = [K.tile(st, f"qb{i}", [64, 2, 512], BF16) for i in range(2)]
            kb_sb = [K.tile(st, f"kb{i}", [64, 2, NMETA + S], BF16) for i in range(2)]
            et = [K.tile(st, f"et{i}", [128, 512], BF16) for i in range(4)]
            r1 = K.tile(st, "r1", [128, 512], F32)
            a1 = K.tile(st, "a1", [128, 512], F32)
            a2 = K.tile(st, "a2", [128, 512], F32)
            sq = K.tile(st, "sqd", [128, 512], F32)
            yt = [K.tile(st, f"yt{i}", [128, 512], BF16) for i in range(2)]
            pst = [K.psum(st, f"pst{i}") for i in range(2)]
            oc_ = [K.psum(st, f"o{i}") for i in range(2)]
            sc_ = [K.psum(st, f"s{i}") for i in range(2)]
            pss = K.psum(st, "pss")
            ei = [0]

            def attend(QB, KB, h, nq, key_tiles, tq):
                nt = len(key_tiles)
                for i, (k0, kn, v_ap, v_res, m_ap, m_res) in enumerate(key_tiles):
                    for c in range(2):
                        PST = pst[ei[0] % 2]
                        E_ = et[ei[0] % 4]
                        ei[0] += 1
                        K.mm(PST[:kn, :nq], KB[:, c, k0:k0 + kn], QB[:, c, :nq], True, True, [KB, QB], [PST])
                        K.op(K.act, lambda: nc.scalar.activation(out=E_[:kn, :nq], in_=PST[:kn, :nq], func=AF.Exp, scale=0.125),
                             reads=[PST], writes=[E_])
                        if m_ap is not None:
                            K.op(K.dve, lambda: nc.vector.tensor_tensor(out=E_[:kn, :nq], in0=E_[:kn, :nq], in1=m_ap, op=ALU.mult),
                                 reads=[E_, m_res], writes=[E_])
                        K.mm(oc_[c][:, :nq], v_ap, E_[:kn, :nq], i == 0, i == nt - 1, [v_res, E_], [oc_[c]])
                        K.mm(sc_[c][:, :nq], self.ones_b[:kn, :], E_[:kn, :nq], i == 0, i == nt - 1, [self.ones_b, E_], [sc_[c]])
                Y = yt[h % 2]
                K.op(K.dve, lambda: nc.vector.reciprocal(out=r1[:, :nq], in_=sc_[0][:, :nq]), reads=[sc_[0]], writes=[r1])
                K.op(K.dve, lambda: nc.vector.tensor_tensor(out=a1[:, :nq], in0=oc_[0][:, :nq], in1=r1[:, :nq], op=ALU.mult),
                     reads=[oc_[0], r1], writes=[a1])
                K.op(K.dve, lambda: nc.vector.reciprocal(out=r1[:, :nq], in_=sc_[1][:, :nq]), reads=[sc_[1]], writes=[r1])
                K.op(K.dve, lambda: nc.vector.tensor_tensor(out=a2[:, :nq], in0=oc_[1][:, :nq], in1=r1[:, :nq], op=ALU.mult),
                     reads=[oc_[1], r1], writes=[a2])
                K.op(K.dve, lambda: nc.vector.scalar_tensor_tensor(out=a1[:, :nq], in0=a2[:, :nq], scalar=neglam[:, 0:1],
                                                                   in1=a1[:, :nq], op0=ALU.mult, op1=ALU.add),
                     reads=[a2, neglam, a1], writes=[a1])
                K.op(K.act, lambda: nc.scalar.activation(out=sq[:, :nq], in_=a1[:, :nq], func=AF.Square), reads=[a1], writes=[sq])
                for c0 in range(0, nq, 256):
                    c1 = min(nq, c0 + 256)
                    K.op(K.pe, lambda: nc.tensor.matmul(out=pss[:, c0:c1], lhsT=self.ones_f[:, :], rhs=sq[:, c0:c1],
                                                        start=True, stop=True), reads=[self.ones_f, sq], writes=[pss])
                K.op(K.act, lambda: nc.scalar.activation(out=r1[:, :nq], in_=pss[:, :nq], func=AF.Sqrt,
                                                         bias=self.eps_t[:, 0:1], scale=1.0 / 128),
                     reads=[pss, self.eps_t], writes=[r1])
                K.op(K.dve, lambda: nc.vector.reciprocal(out=r1[:, :nq], in_=r1[:, :nq]), reads=[r1], writes=[r1])
                K.op(K.dve, lambda: nc.vector.scalar_tensor_tensor(out=Y[:, :nq], in0=a1[:, :nq], scalar=sgs[:, 0:1],
                                                                   in1=r1[:, :nq], op0=ALU.mult, op1=ALU.mult),
                     reads=[a1, sgs, r1], writes=[Y])
                K.dma(K.sp, self.mixB[h * 128:(h + 1) * 128, tq:tq + nq], Y[:, :nq], Y, reads=[Y])

            def load_qk(h, tq, nq, nkeys):
                QB = qb_sb[h % 2]
                KB = kb_sb[h % 2]
                K.dma(K.sp, QB[:, :, :nq], self.qbT[h * 128:(h + 1) * 128, tq:tq + nq].rearrange("(c d) q -> d c q", d=64),
                      QB, writes=[QB])
                K.dma(K.sp, KB[:, :, :nkeys], self.kbT[h * 128:(h + 1) * 128, 0:nkeys].rearrange("(c d) q -> d c q", d=64),
                      KB, writes=[KB])
                return QB, KB

            for h in range(4):
                QB, KB = load_qk(h, 0, NMETA, NMETA)
                attend(QB, KB, h, NMETA, [(0, NMETA, vmeta[:NMETA, h * 128:(h + 1) * 128], vmeta, cmeta[:, :], cmeta)], 0)
            for qb in range(NQB):
                nkt = 4 * (qb + 1)
                tq = NMETA + qb * 512
                for h in range(4):
                    K.maybe_reset()
                    QB, KB = load_qk(h, tq, 512, NMETA + nkt * 128)
                    tiles = [(0, NMETA, vmeta[:NMETA, h * 128:(h + 1) * 128], vmeta, None, None)]
                    for kt in range(nkt):
                        dj = kt - 4 * qb
                        tiles.append((NMETA + kt * 128, 128, vb_sb[:, kt, h * 128:(h + 1) * 128], vb_sb.sub(kt // 8),
                                      cmT[:, dj, :] if dj >= 0 else None, cmT if dj >= 0 else None))
                    attend(QB, KB, h, 512, tiles, tq)
            K.end_phase()

    def attn_out_phase(self, src, li):
        K = self.K
        nc = self.nc
        j = li // 2
        NB = 256
        blocks = token_blocks(self.S, NB)
        with ExitStack() as st:
            woA = K.tile(st, "woA", [64, 8, D], BF16)
            woB = K.tile(st, "woB", [128, 4, D], BF16)
            wav = self.attn_w_out[j, 0:512, :].rearrange("(h d) m -> d h m", d=64)
            wbv = self.attn_w_out[j, 512:1024, :].rearrange("(h p) m -> p h m", p=128)
            for q in range(2):
                self.wload(K.swslot(), woA[:, q * 4:(q + 1) * 4, :], wav[:, q * 4:(q + 1) * 4, :], [woA.sub(q)])
                self.wload(K.swslot(), woB[:, q * 2:(q + 1) * 2, :], wbv[:, q * 2:(q + 1) * 2, :], [woB.sub(q)])
            xt = [K.tile(st, f"xt{i}", [128, 8, NB], F32) for i in range(2)]
            mA = [K.tile(st, f"mA{i}", [64, 8, NB], BF16) for i in range(2)]
            mB = [K.tile(st, f"mB{i}", [128, 4, NB], BF16) for i in range(2)]
            po = [K.psum(st, f"po{i}") for i in range(2)]
            srcv = src.rearrange("(c p) t -> p c t", p=128)
            dstv = self.hT.rearrange("(c p) t -> p c t", p=128)
            for b, (t0, n) in enumerate(blocks):
                K.maybe_reset()
                X = xt[b % 2]
                A = mA[b % 2]
                B_ = mB[b % 2]
                K.dma(K.sp, X[:, :, :n], srcv[:, :, t0:t0 + n], X, reads=[K.dres("h", b)], writes=[X])
                K.dma(K.sp, A[:, :, :n], self.mixA[:, t0:t0 + n].rearrange("(h d) t -> d h t", d=64), A, writes=[A])
                K.dma(K.sp, B_[:, :, :n], self.mixB[:, t0:t0 + n].rearrange("(h p) t -> p h t", p=128), B_, writes=[B_])
                for m in range(8):
                    P = po[m % 2]
                    for h in range(8):
                        K.mm(P[:, :n], woA[:, h, m * 128:(m + 1) * 128], A[:, h, :n], h == 0, False, [woA, A], [P])
                    for hb in range(4):
                        K.mm(P[:, :n], woB[:, hb, m * 128:(m + 1) * 128], B_[:, hb, :n], False, hb == 3, [woB, B_], [P])
                    K.op(K.dve, lambda: nc.vector.tensor_tensor(out=X[:, m, :n], in0=P[:, :n], in1=X[:, m, :n], op=ALU.add),
                         reads=[P, X.sub(m)], writes=[X.sub(m)])
                K.dma(K.sp, dstv[:, :, t0:t0 + n], X[:, :, :n], X, reads=[X], writes=[K.dres("h", b)])
            K.end_phase()

    def copy_phase(self, src):
        K = self.K
        NB = 256
        blocks = token_blocks(self.S, NB)
        with ExitStack() as st:
            xt = [K.tile(st, f"xt{i}", [128, 8, NB], F32) for i in range(2)]
            srcv = src.rearrange("(c p) t -> p c t", p=128)
            dstv = self.hT.rearrange("(c p) t -> p c t", p=128)
            for b, (t0, n) in enumerate(blocks):
                K.maybe_reset()
                X = xt[b % 2]
                K.dma(K.sp, X[:, :, :n], srcv[:, :, t0:t0 + n], X, writes=[X])
                K.dma(K.sp, dstv[:, :, t0:t0 + n], X[:, :, :n], X, reads=[X])
            K.end_phase()

    def final_phase(self, src):
        K = self.K
        nc = self.nc
        NB = 256
        blocks = token_blocks(self.S, NB)[1:]
        with ExitStack() as st:
            self.consts(st)
            gcol = self.load_cols(st, "gcol", self.fin_gc[:, :], 8)
            xt = [K.tile(st, f"xt{i}", [128, 8, NB], F32) for i in range(2)]
            sqt = [K.tile(st, f"sq{i}", [128, NB], F32) for i in range(2)]
            rstd = K.tile(st, "rstd", [128, NB], F32)
            ss = K.psum(st, "ss")
            srcv = src.rearrange("(c p) t -> p c t", p=128)
            dstv = self.yT.rearrange("(c p) t -> p c t", p=128)
            for b, (t0, n) in enumerate(blocks):
                K.maybe_reset()
                X = xt[b % 2]
                K.dma(K.sp, X[:, :, :n], srcv[:, :, t0:t0 + n], X, writes=[X])
                self.rms_rstd(None, X, n, sqt, ss, rstd, self.ones_f)
                for c in range(8):
                    K.op(K.dve, lambda c=c, X=X: nc.vector.scalar_tensor_tensor(
                        out=X[:, c, :n], in0=X[:, c, :n], scalar=gcol[:, c:c + 1], in1=rstd[:, :n],
                        op0=ALU.mult, op1=ALU.mult), reads=[X.sub(c), gcol, rstd], writes=[X.sub(c)])
                K.dma(K.sp, dstv[:, :, t0 - NMETA:t0 - NMETA + n], X[:, :, :n], X, reads=[X])
            K.end_phase()


def full_plan():
    plan = []
    for i in range(DEPTH):
        plan.append(("ffn", i, 0))
        plan.append(("attn", i) if i % 2 == 0 else ("rec", i))
        plan.append(("ffn", i, 1))
    return plan


def cols128(v):
    v = np.asarray(v, np.float32)
    return np.ascontiguousarray(v.reshape(8, 128).T)


def host_inputs(inp, S):
    f32 = np.float32
    shared = {}
    ng = np.asarray(inp["norm_g"], f32)
    shared["norm_gc"] = np.ascontiguousarray(
        np.concatenate([cols128(ng[i, k]) for i in range(DEPTH) for k in range(3)], axis=1))
    shared["fin_gc"] = cols128(inp["final_norm_g"])
    for k in ["ffn_w_gu", "ffn_w_down", "attn_w_in", "attn_w_out", "rec_w_in", "rec_w_out", "rec_gate_w"]:
        shared[k] = np.ascontiguousarray(np.asarray(inp[k], f32))
    lg = np.asarray(inp["idx_k_ln_g"], f32)
    lb = np.asarray(inp["idx_k_ln_b"], f32)
    shared["idx_lnc"] = np.ascontiguousarray(np.stack([lg[0], lb[0], lg[1], lb[1]], axis=1))
    dl = np.asarray(inp["diff_lambda"], f32).reshape(1, 2 * 256)
    shared["dlam_bc"] = np.ascontiguousarray(np.broadcast_to(dl, (128, 512)))
    shared["subln_c"] = np.ascontiguousarray(np.asarray(inp["diff_subln_g"], f32).T)
    rc = []
    for j in range(2):
        for w in range(4):
            rc.append(cols128(inp["rec_conv_w"][j][w]))
        rc.append(cols128(inp["rec_conv_b"][j]))
        rc.append(cols128(inp["rec_gate_b"][j][0]))
        rc.append(cols128(inp["rec_gate_b"][j][1]))
        rc.append(cols128(inp["rec_lambda"][j]))
    shared["rec_cols"] = np.ascontiguousarray(np.concatenate(rc, axis=1))
    shared["c_ident_bf"] = np.eye(128, dtype=f32).astype(ml_dtypes.bfloat16)
    q = np.arange(128)[:, None]
    k = np.arange(128)[None, :]
    shared["c_negmask"] = np.where(k <= q, 0.0, NEG).astype(f32)
    kk = np.arange(128)[:, None, None]
    jj = np.arange(4)[None, :, None]
    qq = np.arange(512)[None, None, :]
    shared["c_cmaskT"] = np.ascontiguousarray(
        ((128 * jj + kk) <= qq).astype(f32).reshape(128, 2048)).astype(ml_dtypes.bfloat16)
    k16 = np.arange(16)[:, None]
    q16 = np.arange(16)[None, :]
    shared["c_cmeta"] = (k16 <= q16).astype(f32).astype(ml_dtypes.bfloat16)
    x = np.asarray(inp["x"], f32)
    meta = np.asarray(inp["meta_tokens"], f32)
    maps = []
    for b in range(x.shape[0]):
        m = dict(shared)
        m["xT"] = np.ascontiguousarray(np.concatenate([meta, x[b]], axis=0).T)
        maps.append(m)
    return maps


_CACHE = {}


def run(inp, plan=None):
    x = np.asarray(inp["x"])
    B, S, _ = x.shape
    plan = full_plan() if plan is None else plan
    key = (S, tuple(plan))
    if key not in _CACHE:
        _CACHE[key] = Prog(S, plan)
    prog = _CACHE[key]
    maps = host_inputs(inp, S)
    res = run_bass_kernel_spmd(prog.nc, maps, core_ids=list(range(B)))
    out = np.stack([np.ascontiguousarray(res.results[b]["yT"].T) for b in range(B)], axis=0)
    return out.astype(np.float32)


def kernel(**inputs):
    return run(inputs)
```

```python
import math
from contextlib import ExitStack

import numpy as np
import ml_dtypes
import concourse.bass as bass
import concourse.mybir as mybir
from concourse.bass_utils import run_bass_kernel_spmd

F32 = mybir.dt.float32
BF16 = mybir.dt.bfloat16
AF = mybir.ActivationFunctionType
ALU = mybir.AluOpType
AX = mybir.AxisListType

D = 1024
NMETA = 16
DFF = 2816
EPS = 1e-6
DEPTH = 4
ATTN_IN = 3656
NEG = -1.0e30
RESET_LIMIT = 700


ALL_RES = []


class Res:
    __slots__ = ("last_w", "readers", "parent", "kids")

    def __init__(self, parent=None):
        ALL_RES.append(self)
        self.last_w = None
        self.readers = {}
        self.parent = parent
        self.kids = []
        if parent is not None:
            parent.kids.append(self)

    def related(self):
        out = [self]
        p = self.parent
        while p is not None:
            out.append(p)
            p = p.parent
        stack = list(self.kids)
        while stack:
            k = stack.pop()
            out.append(k)
            stack.extend(k.kids)
        return out


class Tile:
    def __init__(self, K, handle, name):
        self.K = K
        self.h = handle
        self.name = name
        self.res = Res()
        self.subs = {}
        self.slots = {}
        self.dsem = None
        self.dcnt = 0

    def __getitem__(self, idx):
        return self.h[idx]

    def slot(self, key):
        sl = self.slots.get(key)
        if sl is None:
            sl = Tile(self.K, self.h, f"{self.name}_s{key}")
            self.slots[key] = sl
            self.K.phase_tiles.append(sl)
        return sl

    def sub(self, key):
        r = self.subs.get(key)
        if r is None:
            r = Res(self.res)
            self.subs[key] = r
        return r


class Eng:
    def __init__(self, K, name, eng, self_dep):
        self.K = K
        self.name = name
        self.eng = eng
        self.sem = K.nc.alloc_semaphore("es_" + name)
        self.cnt = 0
        self.seen = {}
        self.self_dep = self_dep


class Kb:
    def __init__(self, nc):
        self.nc = nc
        fs = list(nc.free_semaphores)
        nc.gpsimd.sem_clear(range(min(fs), max(fs) + 1))
        self.pe = Eng(self, "pe", nc.tensor, False)
        self.act = Eng(self, "act", nc.scalar, True)
        self.dve = Eng(self, "dve", nc.vector, True)
        self.pool = Eng(self, "pool", nc.gpsimd, True)
        self.sp = Eng(self, "sp", nc.sync, True)
        self.engs = [self.pe, self.act, self.dve, self.pool, self.sp]
        self.sems = {}
        for e in self.engs:
            self.sems[id(e.sem)] = (e.sem, e)
        self.dram_res = {}
        self.phase_tiles = []
        self.n_ops = 0
        self.free_hw = []
        self.sw_holders = []
        self.sw_idx = 0
        self.uid = 0
        for e in self.engs:
            nc.gpsimd.sem_clear(e.sem)
        nc.all_engine_barrier()

    def tile(self, st, name, shape, dtype):
        self.uid += 1
        name = f"{name}_{self.uid}"
        h = st.enter_context(self.nc.sbuf_tensor(name, list(shape), dtype))
        t = Tile(self, h, name)
        self.phase_tiles.append(t)
        return t

    def psum(self, st, name, shape=(128, 512), dtype=F32):
        self.uid += 1
        name = f"{name}_{self.uid}"
        h = st.enter_context(self.nc.psum_tensor(name, list(shape), dtype))
        return Tile(self, h, name)

    def swslot(self):
        if self.sw_idx == len(self.sw_holders):
            hld = Tile(self, None, f"sw{self.sw_idx}")
            hld.dsem = self.nc.alloc_semaphore(f"ds_sw{self.sw_idx}")
            hld.dsem_sw = True
            self.sems[id(hld.dsem)] = (hld.dsem, hld)
            self.sw_holders.append(hld)
        hld = self.sw_holders[self.sw_idx]
        self.sw_idx += 1
        return hld

    def dres(self, *key):
        r = self.dram_res.get(key)
        if r is None:
            r = Res()
            self.dram_res[key] = r
        return r

    def _need(self, reads, writes):
        need = {}

        def add(rec):
            if rec is None:
                return
            k, v = rec
            if need.get(k, 0) < v:
                need[k] = v

        for r in reads:
            for x in r.related():
                add(x.last_w)
        for w in writes:
            for x in w.related():
                add(x.last_w)
                for k, v in x.readers.items():
                    add((k, v))
        return need

    def _wait(self, E, need):
        for k, v in need.items():
            sem, owner = self.sems[k]
            if owner is E and not E.self_dep:
                continue
            if E.seen.get(k, 0) >= v:
                continue
            E.eng.wait_ge(sem, v)
            E.seen[k] = v

    @staticmethod
    def _resl(xs):
        out = []
        for x in xs:
            out.append(x.res if isinstance(x, Tile) else x)
        return out

    def op(self, E, fn, reads=(), writes=(), sig=True):
        reads = self._resl(reads)
        writes = self._resl(writes)
        self._wait(E, self._need(reads, writes))
        ins = fn()
        if sig:
            E.cnt += 1
            ins.then_inc(E.sem, 1)
            rec = (id(E.sem), E.cnt)
        else:
            rec = (id(E.sem), E.cnt + 1)
        for r in reads:
            if r.readers.get(rec[0], 0) < rec[1]:
                r.readers[rec[0]] = rec[1]
        for w in writes:
            w.last_w = rec
            w.readers = {}
        self.n_ops += 1
        return ins

    def mm(self, out, lhsT, rhs, start, stop, reads, writes, sig=None):
        nc = self.nc
        return self.op(self.pe, lambda: nc.tensor.matmul(out=out, lhsT=lhsT, rhs=rhs, start=start, stop=stop),
                       reads=reads, writes=writes, sig=stop if sig is None else sig)

    def dma(self, E, out, in_, stile, reads=(), writes=(), **kw):
        reads = self._resl(reads)
        writes = self._resl(writes)
        if stile.dsem is None:
            assert E is not self.pool, "gpsimd DMAs must use K.swslot() holders"
            stile.dsem = self.free_hw.pop() if self.free_hw else self.nc.alloc_semaphore("ds_" + stile.name)
            stile.dsem_sw = False
            self.sems[id(stile.dsem)] = (stile.dsem, stile)
        assert stile.dsem_sw == (E is self.pool)
        self._wait(E, self._need(reads, writes))
        ins = E.eng.dma_start(out=out, in_=in_, **kw)
        stile.dcnt += 16
        ins.then_inc(stile.dsem, 16)
        rec = (id(stile.dsem), stile.dcnt)
        for r in reads:
            if r.readers.get(rec[0], 0) < rec[1]:
                r.readers[rec[0]] = rec[1]
        for w in writes:
            w.last_w = rec
            w.readers = {}
        self.n_ops += 1
        return ins

    def reset(self):
        nc = self.nc
        sp = self.sp
        for e in self.engs:
            if e is not sp and e.cnt > 0 and sp.seen.get(id(e.sem), 0) < e.cnt:
                sp.eng.wait_ge(e.sem, e.cnt)
        for t in self.phase_tiles + self.sw_holders:
            if t.dsem is not None and t.dcnt > 0:
                sp.eng.wait_ge(t.dsem, t.dcnt)
        nc.all_engine_barrier()
        for e in self.engs:
            nc.gpsimd.sem_clear(e.sem)
            e.cnt = 0
            e.seen = {}
        for t in self.phase_tiles:
            if t.dsem is not None:
                nc.gpsimd.sem_clear(t.dsem)
                t.dcnt = 0
        nc.all_engine_barrier()
        for r in ALL_RES:
            r.last_w = None
            r.readers = {}

    def maybe_reset(self, limit=RESET_LIMIT):
        if max(e.cnt for e in self.engs) > limit:
            self.reset()

    def end_phase(self):
        self.reset()
        dsems = []
        for t in self.phase_tiles:
            if t.dsem is not None:
                dsems.append((t.dsem, t.dsem_sw))
                del self.sems[id(t.dsem)]
                t.dsem = None
        del ALL_RES[:]
        for s, sw in dsems:
            self.free_hw.append(s)
        self.sw_idx = 0
        self.phase_tiles = []
        self.dram_res = {}


def token_blocks(S, NB):
    blocks = [(0, NMETA)]
    for i in range(S // NB):
        blocks.append((NMETA + i * NB, NB))
    return blocks


class Prog:
    def __init__(self, S, plan):
        self.S = S
        self.T = S + NMETA
        self.plan = plan
        nc = bass.Bass("TRN2", target_bir_lowering=False)
        self.nc = nc
        T = self.T

        def din(name, shape, dt=F32):
            return nc.dram_tensor(name, list(shape), dt, kind="ExternalInput").ap()

        self.xin = din("xT", [D, T])
        self.norm_gc = din("norm_gc", [128, DEPTH * 3 * 8])
        self.fin_gc = din("fin_gc", [128, 8])
        self.w_gu = din("ffn_w_gu", [DEPTH, 2, D, 2 * DFF])
        self.w_dn = din("ffn_w_down", [DEPTH, 2, DFF, D])
        self.attn_w_in = din("attn_w_in", [2, D, ATTN_IN])
        self.attn_w_out = din("attn_w_out", [2, D, D])
        self.idx_lnc = din("idx_lnc", [64, 4])
        self.dlam_bc = din("dlam_bc", [128, 2 * 256])
        self.subln_c = din("subln_c", [128, 2])
        self.rec_w_in = din("rec_w_in", [2, D, 2 * D])
        self.rec_w_out = din("rec_w_out", [2, D, D])
        self.rec_gate_w = din("rec_gate_w", [2, 2, 4, 256, 256])
        self.rec_cols = din("rec_cols", [128, 2 * 8 * 8])
        self.c_ident_bf = din("c_ident_bf", [128, 128], BF16)
        self.c_negmask = din("c_negmask", [128, 128])
        self.c_cmaskT = din("c_cmaskT", [128, 4 * 512], BF16)
        self.c_cmeta = din("c_cmeta", [16, 16], BF16)
        self.yT = nc.dram_tensor("yT", [D, S], F32, kind="ExternalOutput").ap()
        self.hT = nc.dram_tensor("hT", [D, T], F32).ap()
        self.TOPK = min(256, S // 4)

        def scr(name, shape, dt=BF16):
            return nc.dram_tensor(name, list(shape), dt).ap()

        self.qaT = scr("qaT", [512, T])
        self.kaT = scr("kaT", [512, T])
        self.va = scr("va", [T, 512])
        self.qiT = scr("qiT", [512, T])
        self.kiT = scr("kiT", [64, T])
        self.wi = scr("wi", [T, 8], F32)
        self.qbT = scr("qbT", [512, T])
        self.kbT = scr("kbT", [512, T])
        self.vb = scr("vb", [T, 512])
        self.mixA = scr("mixA", [512, T])
        self.mixB = scr("mixB", [512, T])
        self.K = Kb(nc)
        self.build()

    def build(self):
        src = self.xin
        for ph in self.plan:
            kind = ph[0]
            if kind == "ffn":
                self.ffn_phase(src, ph[1], ph[2])
                src = self.hT
            elif kind == "copy":
                self.copy_phase(src)
                src = self.hT
            elif kind == "rec":
                self.rec_phase(src, ph[1])
                src = self.hT
            elif kind == "aproj":
                self.attn_proj_phase(src, ph[1])
            elif kind == "dsa":
                self.dsa_phase(ph[1])
            elif kind == "diff":
                self.diff_phase(ph[1])
            elif kind == "aout":
                self.attn_out_phase(src, ph[1])
                src = self.hT
            elif kind == "attn":
                self.attn_proj_phase(src, ph[1])
                self.dsa_phase(ph[1])
                self.diff_phase(ph[1])
                self.attn_out_phase(src, ph[1])
                src = self.hT
            else:
                raise ValueError(kind)
        self.final_phase(src)

    def rms_rstd(self, st_tiles, X, n, sqt, ss, rstd, ones):
        K = self.K
        for c in range(8):
            s = sqt[c % 2]
            K.op(K.act, lambda s=s, c=c: K.nc.scalar.activation(out=s[:, :n], in_=X[:, c, :n], func=AF.Square),
                 reads=[X.sub(c)], writes=[s])
            K.op(K.pe, lambda s=s, c=c: K.nc.tensor.matmul(out=ss[:, :n], lhsT=ones[:, :], rhs=s[:, :n],
                                                          start=(c == 0), stop=(c == 7)),
                 reads=[s, ones], writes=[ss])
        K.op(K.act, lambda: K.nc.scalar.activation(out=rstd[:, :n], in_=ss[:, :n], func=AF.Sqrt,
                                                   bias=self.eps_t[:, 0:1], scale=1.0 / D),
             reads=[ss, self.eps_t], writes=[rstd])
        K.op(K.dve, lambda: K.nc.vector.reciprocal(out=rstd[:, :n], in_=rstd[:, :n]), reads=[rstd], writes=[rstd])

    def consts(self, st):
        K = self.K
        nc = self.nc
        self.ones_f = K.tile(st, "ones_f", [128, 128], F32)
        self.ones_b = K.tile(st, "ones_b", [128, 128], BF16)
        self.eps_t = K.tile(st, "eps_t", [128, 1], F32)
        self.one_t = K.tile(st, "one_t", [128, 1], F32)
        K.op(K.dve, lambda: nc.vector.memset(self.one_t[:, :], 1.0), writes=[self.one_t])
        K.op(K.dve, lambda: nc.vector.memset(self.ones_f[:, :], 1.0), writes=[self.ones_f])
        K.op(K.dve, lambda: nc.vector.memset(self.ones_b[:, :], 1.0), writes=[self.ones_b])
        K.op(K.dve, lambda: nc.vector.memset(self.eps_t[:, :], EPS), writes=[self.eps_t])

    def load_cols(self, st, name, src_ap, ncols, parts=128):
        K = self.K
        t = K.tile(st, name, [parts, ncols], F32)
        K.dma(K.sp, t[:, :], src_ap, t, writes=[t])
        return t

    def wload(self, t, dst_ap, src_ap, writes):
        K = self.K
        K.dma(K.pool, dst_ap, src_ap, t, writes=writes, max_dma_last_dim=4096)

    def ffn_phase(self, src, li, fi):
        K = self.K
        nc = self.nc
        NB = 256
        blocks = token_blocks(self.S, NB)
        with ExitStack() as st:
            self.consts(st)
            wgu = K.tile(st, "wgu", [128, 8, 2 * DFF], BF16)
            wd = K.tile(st, "wd", [128, 22, D], BF16)
            ni = li * 3 + (0 if fi == 0 else 2)
            gcol = self.load_cols(st, "gcol", self.norm_gc[:, ni * 8:(ni + 1) * 8], 8)
            for k in range(8):
                self.wload(K.swslot(), wgu[:, k, :], self.w_gu[li, fi, k * 128:(k + 1) * 128, :], [wgu.sub(k)])
            wdv = self.w_dn[li, fi].rearrange("(j p) m -> p j m", p=128)
            for j0 in range(0, 22, 2):
                self.wload(K.swslot(), wd[:, j0:j0 + 2, :], wdv[:, j0:j0 + 2, :], [wd.sub(j0), wd.sub(j0 + 1)])
            xt = [K.tile(st, f"xt{i}", [128, 8, NB], F32) for i in range(2)]
            sqt = [K.tile(st, f"sq{i}", [128, NB], F32) for i in range(2)]
            rstd = K.tile(st, "rstd", [128, NB], F32)
            xn = K.tile(st, "xn", [128, 8, NB], BF16)
            sg = [K.tile(st, f"sg{i}", [128, NB], BF16) for i in range(2)]
            hh = K.tile(st, "hh", [128, 22, NB], BF16)
            ss = K.psum(st, "ss")
            pg = [K.psum(st, f"pg{i}") for i in range(2)]
            pu = [K.psum(st, f"pu{i}") for i in range(2)]
            po = [K.psum(st, f"po{i}") for i in range(2)]
            srcv = src.rearrange("(c p) t -> p c t", p=128)
            dstv = self.hT.rearrange("(c p) t -> p c t", p=128)
            for b, (t0, n) in enumerate(blocks):
                K.maybe_reset()
                X = xt[b % 2]
                K.dma(K.sp, X[:, :, :n], srcv[:, :, t0:t0 + n], X, reads=[K.dres("h", b)], writes=[X])
                self.rms_rstd(None, X, n, sqt, ss, rstd, self.ones_f)
                for c in range(8):
                    K.op(K.dve, lambda c=c: nc.vector.scalar_tensor_tensor(
                        out=xn[:, c, :n], in0=X[:, c, :n], scalar=gcol[:, c:c + 1], in1=rstd[:, :n],
                        op0=ALU.mult, op1=ALU.mult), reads=[X.sub(c), gcol, rstd], writes=[xn.sub(c)])
                for j in range(22):
                    G = pg[j % 2]
                    U = pu[j % 2]
                    for k in range(8):
                        K.op(K.pe, lambda k=k, j=j, G=G: nc.tensor.matmul(
                            out=G[:, :n], lhsT=wgu[:, k, j * 128:(j + 1) * 128], rhs=xn[:, k, :n],
                            start=(k == 0), stop=(k == 7)), reads=[wgu.sub(k), xn.sub(k)], writes=[G], sig=(k == 7))
                    for k in range(8):
                        K.op(K.pe, lambda k=k, j=j, U=U: nc.tensor.matmul(
                            out=U[:, :n], lhsT=wgu[:, k, DFF + j * 128:DFF + (j + 1) * 128], rhs=xn[:, k, :n],
                            start=(k == 0), stop=(k == 7)), reads=[wgu.sub(k), xn.sub(k)], writes=[U], sig=(k == 7))
                    s = sg[j % 2]
                    K.op(K.act, lambda s=s, G=G: nc.scalar.activation(out=s[:, :n], in_=G[:, :n], func=AF.Silu),
                         reads=[G], writes=[s])
                    K.op(K.dve, lambda s=s, U=U, j=j: nc.vector.tensor_tensor(
                        out=hh[:, j, :n], in0=s[:, :n], in1=U[:, :n], op=ALU.mult),
                        reads=[s, U], writes=[hh.sub(j)])
                for m in range(8):
                    P = po[m % 2]
                    for j in range(22):
                        K.op(K.pe, lambda j=j, m=m, P=P: nc.tensor.matmul(
                            out=P[:, :n], lhsT=wd[:, j, m * 128:(m + 1) * 128], rhs=hh[:, j, :n],
                            start=(j == 0), stop=(j == 21)), reads=[wd.sub(j), hh.sub(j)], writes=[P], sig=(j == 21))
                    K.op(K.dve, lambda m=m, P=P, X=X: nc.vector.scalar_tensor_tensor(
                        out=X[:, m, :n], in0=P[:, :n], scalar=0.5, in1=X[:, m, :n],
                        op0=ALU.mult, op1=ALU.add), reads=[P, X.sub(m)], writes=[X.sub(m)])
                K.dma(K.sp, dstv[:, :, t0:t0 + n], X[:, :, :n], X, reads=[X], writes=[K.dres("h", b)])
            K.end_phase()


    def load_norm(self, X, xn, gcol, srcv, t0, n, b, sqt, ss, rstd):
        K = self.K
        nc = self.nc
        K.dma(K.sp, X[:, :, :n], srcv[:, :, t0:t0 + n], X, reads=[K.dres("h", b)], writes=[X])
        self.rms_rstd(None, X, n, sqt, ss, rstd, self.ones_f)
        for c in range(8):
            K.op(K.dve, lambda: nc.vector.scalar_tensor_tensor(
                out=xn[:, c, :n], in0=X[:, c, :n], scalar=gcol[:, c:c + 1], in1=rstd[:, :n],
                op0=ALU.mult, op1=ALU.mult), reads=[X.sub(c), gcol, rstd], writes=[xn.sub(c)])

    def rec_phase(self, src, li):
        K = self.K
        nc = self.nc
        j = li // 2
        NB = 256
        blocks = token_blocks(self.S, NB)
        with ExitStack() as st:
            self.consts(st)
            win = K.tile(st, "rwin", [128, 8, 2 * D], BF16)
            gw = K.tile(st, "rgw", [128, 16, 256], BF16)
            wout = K.tile(st, "rwout", [128, 8, D], BF16)
            gcol = self.load_cols(st, "gcol", self.norm_gc[:, (li * 3 + 1) * 8:(li * 3 + 2) * 8], 8)
            rc = self.load_cols(st, "rc", self.rec_cols[:, j * 64:(j + 1) * 64], 64)
            for k in range(8):
                self.wload(K.swslot(), win[:, k, :], self.rec_w_in[j, k * 128:(k + 1) * 128, :], [win.sub(k)])
            gwv = self.rec_gate_w[j].rearrange("g n (ic p) jj -> p (g n ic) jj", p=128)
            for q in range(2):
                self.wload(K.swslot(), gw[:, q * 8:(q + 1) * 8, :], gwv[:, q * 8:(q + 1) * 8, :], [gw.sub(q)])
            wov = self.rec_w_out[j].rearrange("(k p) m -> p k m", p=128)
            for q in range(2):
                self.wload(K.swslot(), wout[:, q * 4:(q + 1) * 4, :], wov[:, q * 4:(q + 1) * 4, :], [wout.sub(q)])
            clam = K.tile(st, "clam", [128, 8], F32)
            K.op(K.act, lambda: nc.scalar.activation(out=clam[:, :], in_=rc[:, 56:64], func=AF.Exp, scale=-1.0),
                 reads=[rc], writes=[clam])
            K.op(K.dve, lambda: nc.vector.tensor_scalar(out=clam[:, :], in0=clam[:, :], scalar1=1.0, scalar2=None,
                                                        op0=ALU.add), reads=[clam], writes=[clam])
            K.op(K.act, lambda: nc.scalar.activation(out=clam[:, :], in_=clam[:, :], func=AF.Ln),
                 reads=[clam], writes=[clam])
            K.op(K.dve, lambda: nc.vector.tensor_scalar(out=clam[:, :], in0=clam[:, :], scalar1=-8.0, scalar2=None,
                                                        op0=ALU.mult), reads=[clam], writes=[clam])
            xt = [K.tile(st, f"xt{i}", [128, 8, NB], F32) for i in range(2)]
            sqt = [K.tile(st, f"sq{i}", [128, NB], F32) for i in range(2)]
            rstd = K.tile(st, "rstd", [128, NB], F32)
            xn = K.tile(st, "xn", [128, 8, NB], BF16)
            yb = K.tile(st, "yb", [128, 8, NB], BF16)
            xb = [K.tile(st, f"xb{i}", [128, 8, 3 + NB], F32) for i in range(2)]
            xc = K.tile(st, "xc", [128, 8, NB], F32)
            xcb = K.tile(st, "xcb", [128, 8, NB], BF16)
            t1 = [K.tile(st, f"t1{i}", [128, NB], F32) for i in range(2)]
            gx = K.tile(st, "gx", [128, 8, NB], F32)
            at = K.tile(st, "at", [128, 8, NB], F32)
            ga = [K.tile(st, f"ga{i}", [128, NB], F32) for i in range(2)]
            mu = [K.tile(st, f"mu{i}", [128, NB], F32) for i in range(2)]
            ut = K.tile(st, "ut", [128, 8, NB], F32)
            hs = [K.tile(st, f"hs{i}", [128, 8, NB], F32) for i in range(2)]
            zb = K.tile(st, "zb", [128, 8, NB], BF16)
            ss = K.psum(st, "ss")
            py = [K.psum(st, f"py{i}") for i in range(2)]
            pgt = [K.psum(st, f"pgt{i}") for i in range(2)]
            po = [K.psum(st, f"po{i}") for i in range(2)]
            srcv = src.rearrange("(c p) t -> p c t", p=128)
            dstv = self.hT.rearrange("(c p) t -> p c t", p=128)
            K.op(K.dve, lambda: nc.vector.memset(xb[0][:, :, 0:3], 0.0), writes=[xb[0]])
            nprev = 0
            for b, (t0, n) in enumerate(blocks):
                K.maybe_reset()
                X = xt[b % 2]
                XB = xb[b % 2]
                XBn = xb[(b + 1) % 2]
                HS = hs[b % 2]
                HSp = hs[(b + 1) % 2]
                self.load_norm(X, xn, gcol, srcv, t0, n, b, sqt, ss, rstd)
                for m in range(8):
                    P = py[m % 2]
                    T1 = t1[m % 2]
                    for k in range(8):
                        K.op(K.pe, lambda: nc.tensor.matmul(out=P[:, :n], lhsT=win[:, k, m * 128:(m + 1) * 128],
                                                            rhs=xn[:, k, :n], start=(k == 0), stop=(k == 7)),
                             reads=[win.sub(k), xn.sub(k)], writes=[P], sig=(k == 7))
                    K.op(K.act, lambda: nc.scalar.activation(out=T1[:, :n], in_=P[:, :n], func=AF.Square),
                         reads=[P], writes=[T1])
                    K.op(K.dve, lambda: nc.vector.tensor_scalar(out=T1[:, :n], in0=T1[:, :n], scalar1=0.044715,
                                                                scalar2=1.0, op0=ALU.mult, op1=ALU.add),
                         reads=[T1], writes=[T1])
                    K.op(K.dve, lambda: nc.vector.tensor_tensor(out=T1[:, :n], in0=T1[:, :n], in1=P[:, :n], op=ALU.mult),
                         reads=[T1, P], writes=[T1])
                    K.op(K.act, lambda: nc.scalar.activation(out=T1[:, :n], in_=T1[:, :n], func=AF.Sigmoid,
                                                             scale=1.5957691216057308), reads=[T1], writes=[T1])
                    K.op(K.dve, lambda: nc.vector.tensor_tensor(out=yb[:, m, :n], in0=T1[:, :n], in1=P[:, :n], op=ALU.mult),
                         reads=[T1, P], writes=[yb.sub(m)])
                for m in range(8):
                    P = py[m % 2]
                    for k in range(8):
                        K.op(K.pe, lambda: nc.tensor.matmul(out=P[:, :n], lhsT=win[:, k, D + m * 128:D + (m + 1) * 128],
                                                            rhs=xn[:, k, :n], start=(k == 0), stop=(k == 7)),
                             reads=[win.sub(k), xn.sub(k)], writes=[P], sig=(k == 7))
                    K.op(K.act, lambda: nc.scalar.activation(out=XB[:, m, 3:3 + n], in_=P[:, :n], func=AF.Copy),
                         reads=[P], writes=[XB.sub(m)])
                for m in range(8):
                    K.op(K.dve, lambda: nc.vector.tensor_scalar(
                        out=xc[:, m, :n], in0=XB[:, m, 0:n], scalar1=rc[:, m:m + 1], scalar2=rc[:, 32 + m:33 + m],
                        op0=ALU.mult, op1=ALU.add), reads=[XB.sub(m), rc], writes=[xc.sub(m)])
                    for w in range(1, 4):
                        K.op(K.dve, lambda: nc.vector.scalar_tensor_tensor(
                            out=xc[:, m, :n], in0=XB[:, m, w:w + n], scalar=rc[:, w * 8 + m:w * 8 + m + 1],
                            in1=xc[:, m, :n], op0=ALU.mult, op1=ALU.add), reads=[XB.sub(m), rc, xc.sub(m)],
                            writes=[xc.sub(m)])
                    K.op(K.act, lambda: nc.scalar.activation(out=xcb[:, m, :n], in_=xc[:, m, :n], func=AF.Copy),
                         reads=[xc.sub(m)], writes=[xcb.sub(m)])
                K.op(K.act, lambda: nc.scalar.activation(out=XBn[:, :, 0:3], in_=XB[:, :, n:n + 3], func=AF.Copy),
                     reads=[XB], writes=[XBn])
                for oc in range(8):
                    nb_, jc = oc // 2, oc % 2
                    P0 = pgt[0]
                    P1 = pgt[1]
                    GA = ga[oc % 2]
                    MU = mu[oc % 2]
                    for g, P in ((0, P0), (1, P1)):
                        for ic in range(2):
                            K.op(K.pe, lambda: nc.tensor.matmul(
                                out=P[:, :n], lhsT=gw[:, (g * 4 + nb_) * 2 + ic, jc * 128:(jc + 1) * 128],
                                rhs=xcb[:, nb_ * 2 + ic, :n], start=(ic == 0), stop=(ic == 1)),
                                reads=[gw, xcb.sub(nb_ * 2 + ic)], writes=[P], sig=(ic == 1))
                    K.op(K.act, lambda: nc.scalar.activation(out=gx[:, oc, :n], in_=P0[:, :n], func=AF.Sigmoid,
                                                             bias=rc[:, 40 + oc:41 + oc]), reads=[P0, rc], writes=[gx.sub(oc)])
                    K.op(K.act, lambda: nc.scalar.activation(out=GA[:, :n], in_=P1[:, :n], func=AF.Sigmoid,
                                                             bias=rc[:, 48 + oc:49 + oc]), reads=[P1, rc], writes=[GA])
                    K.op(K.act, lambda: nc.scalar.activation(out=at[:, oc, :n], in_=GA[:, :n], func=AF.Exp,
                                                             scale=clam[:, oc:oc + 1]), reads=[GA, clam], writes=[at.sub(oc)])
                    K.op(K.act, lambda: nc.scalar.activation(out=MU[:, :n], in_=at[:, oc, :n], func=AF.Square),
                         reads=[at.sub(oc)], writes=[MU])
                    K.op(K.act, lambda: nc.scalar.activation(out=MU[:, :n], in_=MU[:, :n], func=AF.Sqrt,
                                                             bias=self.one_t[:, 0:1], scale=-1.0),
                         reads=[MU, self.one_t], writes=[MU])
                    K.op(K.dve, lambda: nc.vector.tensor_tensor(out=ut[:, oc, :n], in0=gx[:, oc, :n], in1=xc[:, oc, :n],
                                                                op=ALU.mult), reads=[gx.sub(oc), xc.sub(oc)], writes=[ut.sub(oc)])
                    K.op(K.dve, lambda: nc.vector.tensor_tensor(out=ut[:, oc, :n], in0=ut[:, oc, :n], in1=MU[:, :n],
                                                                op=ALU.mult), reads=[ut.sub(oc), MU], writes=[ut.sub(oc)])
                    init = 0.0 if b == 0 else HSp[:, oc, nprev - 1:nprev]
                    K.op(K.dve, lambda: nc.vector.tensor_tensor_scan(
                        out=HS[:, oc, :n], data0=at[:, oc, :n], data1=ut[:, oc, :n], initial=init,
                        op0=ALU.mult, op1=ALU.add), reads=[at.sub(oc), ut.sub(oc), HSp.sub(oc)], writes=[HS.sub(oc)])
                    K.op(K.dve, lambda: nc.vector.tensor_tensor(out=zb[:, oc, :n], in0=HS[:, oc, :n], in1=yb[:, oc, :n],
                                                                op=ALU.mult), reads=[HS.sub(oc), yb.sub(oc)], writes=[zb.sub(oc)])
                for m in range(8):
                    P = po[m % 2]
                    for k in range(8):
                        K.op(K.pe, lambda: nc.tensor.matmul(out=P[:, :n], lhsT=wout[:, k, m * 128:(m + 1) * 128],
                                                            rhs=zb[:, k, :n], start=(k == 0), stop=(k == 7)),
                             reads=[wout, zb.sub(k)], writes=[P], sig=(k == 7))
                    K.op(K.dve, lambda: nc.vector.tensor_tensor(out=X[:, m, :n], in0=P[:, :n], in1=X[:, m, :n], op=ALU.add),
                         reads=[P, X.sub(m)], writes=[X.sub(m)])
                K.dma(K.sp, dstv[:, :, t0:t0 + n], X[:, :, :n], X, reads=[X], writes=[K.dres("h", b)])
                nprev = n
            K.end_phase()

    def attn_proj_phase(self, src, li):
        K = self.K
        nc = self.nc
        j = li // 2
        NB = 256
        blocks = token_blocks(self.S, NB)
        with ExitStack() as st:
            self.consts(st)
            win = K.tile(st, "awin", [128, 8, ATTN_IN], BF16)
            gcol = self.load_cols(st, "gcol", self.norm_gc[:, (li * 3 + 1) * 8:(li * 3 + 2) * 8], 8)
            lnc = self.load_cols(st, "lnc", self.idx_lnc[:, j * 2:j * 2 + 2], 2, parts=64)
            for k in range(8):
                self.wload(K.swslot(), win[:, k, :], self.attn_w_in[j, k * 128:(k + 1) * 128, :], [win.sub(k)])
            xt = [K.tile(st, f"xt{i}", [128, 8, NB], F32) for i in range(2)]
            sqt = [K.tile(st, f"sq{i}", [128, NB], F32) for i in range(2)]
            rstd = K.tile(st, "rstd", [128, NB], F32)
            xn = K.tile(st, "xn", [128, 8, NB], BF16)
            fo = [K.tile(st, f"fo{i}", [128, 4, NB], BF16) for i in range(2)]
            kf = K.tile(st, "kf", [64, NB], F32)
            kx = K.tile(st, "kx", [64, NB], F32)
            ksq = K.tile(st, "ksq", [64, NB], F32)
            krs = K.tile(st, "krs", [64, NB], F32)
            ko = [K.tile(st, f"ko{i}", [64, NB], BF16) for i in range(2)]
            vo = [K.tile(st, f"vo{i}", [128, 512], BF16) for i in range(4)]
            wo = [K.tile(st, f"wo{i}", [128, 8], F32) for i in range(2)]
            ss = K.psum(st, "ss")
            pf = [K.psum(st, f"pf{i}") for i in range(2)]
            pk = K.psum(st, "pk")
            pk2 = K.psum(st, "pk2")
            pt = [K.psum(st, f"pt{i}") for i in range(2)]
            srcv = src.rearrange("(c p) t -> p c t", p=128)
            groups = [(self.qaT, 0), (self.kaT, 512), (self.qiT, 1536), (self.qbT, 2120), (self.kbT, 2632)]
            gi = 0
            vi = 0
            wi_i = 0
            for b, (t0, n) in enumerate(blocks):
                K.maybe_reset()
                X = xt[b % 2]
                self.load_norm(X, xn, gcol, srcv, t0, n, b, sqt, ss, rstd)
                for (dst, c0) in groups:
                    FO = fo[gi % 2]
                    gi += 1
                    for m in range(4):
                        P = pf[m % 2]
                        for k in range(8):
                            K.mm(P[:, :n], win[:, k, c0 + m * 128:c0 + (m + 1) * 128], xn[:, k, :n], k == 0, k == 7,
                                 [win.sub(k), xn.sub(k)], [P])
                        if m % 2 == 0:
                            K.op(K.act, lambda: nc.scalar.activation(out=FO[:, m, :n], in_=P[:, :n], func=AF.Copy),
                                 reads=[P], writes=[FO.sub(m)])
                        else:
                            K.op(K.dve, lambda: nc.vector.tensor_copy(out=FO[:, m, :n], in_=P[:, :n]),
                                 reads=[P], writes=[FO.sub(m)])
                    K.dma(K.sp, dst.rearrange("(m p) t -> p m t", p=128)[:, :, t0:t0 + n], FO[:, :, :n], FO, reads=[FO])
                for k in range(8):
                    K.mm(pk[:64, :n], win[:, k, 2048:2112], xn[:, k, :n], k == 0, k == 7, [win.sub(k), xn.sub(k)], [pk])
                K.op(K.act, lambda: nc.scalar.activation(out=kf[:, :n], in_=pk[:64, :n], func=AF.Copy), reads=[pk], writes=[kf])
                K.mm(pk2[:64, :n], self.ones_f[:64, :64], kf[:, :n], True, True, [self.ones_f, kf], [pk2])
                K.op(K.dve, lambda: nc.vector.scalar_tensor_tensor(out=kx[:, :n], in0=pk2[:64, :n], scalar=-1.0 / 64,
                                                                   in1=kf[:, :n], op0=ALU.mult, op1=ALU.add),
                     reads=[pk2, kf], writes=[kx])
                K.op(K.act, lambda: nc.scalar.activation(out=ksq[:, :n], in_=kx[:, :n], func=AF.Square), reads=[kx], writes=[ksq])
                K.mm(pk2[:64, :n], self.ones_f[:64, :64], ksq[:, :n], True, True, [self.ones_f, ksq], [pk2])
                K.op(K.act, lambda: nc.scalar.activation(out=krs[:, :n], in_=pk2[:64, :n], func=AF.Sqrt,
                                                         bias=self.eps_t[:64, 0:1], scale=1.0 / 64),
                     reads=[pk2, self.eps_t], writes=[krs])
                K.op(K.dve, lambda: nc.vector.reciprocal(out=krs[:, :n], in_=krs[:, :n]), reads=[krs], writes=[krs])
                K.op(K.dve, lambda: nc.vector.tensor_tensor(out=kx[:, :n], in0=kx[:, :n], in1=krs[:, :n], op=ALU.mult),
                     reads=[kx, krs], writes=[kx])
                KO = ko[b % 2]
                K.op(K.dve, lambda: nc.vector.tensor_scalar(out=KO[:, :n], in0=kx[:, :n], scalar1=lnc[:, 0:1],
                                                            scalar2=lnc[:, 1:2], op0=ALU.mult, op1=ALU.add),
                     reads=[kx, lnc], writes=[KO])
                K.dma(K.sp, self.kiT[:, t0:t0 + n], KO[:, :n], KO, reads=[KO])
                for c_lo in range(0, n, 128):
                    tn = min(128, n - c_lo)
                    for (dst, c0) in ((self.va, 1024), (self.vb, 3144)):
                        P = pt[vi % 2]
                        VO = vo[vi % 4]
                        vi += 1
                        for k in range(8):
                            K.mm(P[:tn, :512], xn[:, k, c_lo:c_lo + tn], win[:, k, c0:c0 + 512], k == 0, k == 7,
                                 [win.sub(k), xn.sub(k)], [P])
                        if vi % 2 == 0:
                            K.op(K.act, lambda: nc.scalar.activation(out=VO[:tn, :], in_=P[:tn, :512], func=AF.Copy),
                                 reads=[P], writes=[VO])
                        else:
                            K.op(K.dve, lambda: nc.vector.tensor_copy(out=VO[:tn, :], in_=P[:tn, :512]), reads=[P], writes=[VO])
                        K.dma(K.sp, dst[t0 + c_lo:t0 + c_lo + tn, :], VO[:tn, :], VO, reads=[VO])
                    WO = wo[wi_i % 2]
                    wi_i += 1
                    for k in range(8):
                        K.mm(pk2[:tn, :8], xn[:, k, c_lo:c_lo + tn], win[:, k, 2112:2120], k == 0, k == 7,
                             [win.sub(k), xn.sub(k)], [pk2])
                    K.op(K.dve, lambda: nc.vector.tensor_scalar(out=WO[:tn, :], in0=pk2[:tn, :8], scalar1=0.044194173824159216,
                                                                scalar2=None, op0=ALU.mult), reads=[pk2], writes=[WO])
                    K.dma(K.sp, self.wi[t0 + c_lo:t0 + c_lo + tn, :], WO[:tn, :], WO, reads=[WO])
            K.end_phase()

    def dsa_phase(self, li):
        K = self.K
        nc = self.nc
        S = self.S
        TOPK = float(self.TOPK)
        NKT = S // 128
        NQB = S // 512
        NIT = 18
        with ExitStack() as st:
            self.consts(st)
            ident = K.tile(st, "ident", [128, 128], BF16)
            K.dma(K.sp, ident[:, :], self.c_ident_bf[:, :], ident, writes=[ident])
            negm = K.tile(st, "negm", [128, 128], F32)
            K.dma(K.sp, negm[:, :], self.c_negmask[:, :], negm, writes=[negm])
            cmeta = K.tile(st, "cmeta", [16, 16], BF16)
            K.dma(K.sp, cmeta[:, :], self.c_cmeta[:, :], cmeta, writes=[cmeta])
            ki_sb = K.tile(st, "ki_sb", [64, S], BF16)
            K.dma(K.sp, ki_sb[:, :], self.kiT[:, NMETA:], ki_sb, writes=[ki_sb])
            va_sb = K.tile(st, "va_sb", [128, NKT, 512], BF16)
            vav = self.va[NMETA:, :].rearrange("(kt p) c -> p kt c", p=128)
            for g0 in range(0, NKT, 8):
                g1 = min(NKT, g0 + 8)
                K.dma(K.sp, va_sb[:, g0:g1, :], vav[:, g0:g1, :], va_sb.slot(g0), writes=[va_sb.sub(g0 // 8)])
            vmeta = K.tile(st, "vmeta", [16, 512], BF16)
            K.dma(K.sp, vmeta[:, :], self.va[0:NMETA, :], vmeta, writes=[vmeta])
            maskT = [K.tile(st, f"maskT{i}", [128, NKT, 512], BF16) for i in range(2)]
            score = K.tile(st, "score", [128, S], F32)
            mask01 = K.tile(st, "mask01", [128, S], BF16)
            rl = [K.tile(st, f"rl{i}", [128, 512], F32) for i in range(2)]
            qi_sb = [K.tile(st, f"qi{i}", [64, 8, 128], BF16) for i in range(2)]
            wi_sb = [K.tile(st, f"wi{i}", [128, 8], F32) for i in range(2)]
            smax = K.tile(st, "smax", [128, 1], F32)
            lo = K.tile(st, "lo", [128, 1], F32)
            w0 = K.tile(st, "w0", [128, 1], F32)
            mid = K.tile(st, "mid", [128, 1], F32)
            cnt = K.tile(st, "cnt", [128, 1], F32)
            gg = K.tile(st, "gg", [128, 1], F32)
            qa_sb = [K.tile(st, f"qa{i}", [64, 512], BF16) for i in range(2)]
            ka_sb = [K.tile(st, f"ka{i}", [64, NMETA + S], BF16) for i in range(2)]
            et = [K.tile(st, f"et{i}", [128, 512], BF16) for i in range(2)]
            ptl = [K.tile(st, f"ptl{i}", [128, 512], BF16) for i in range(2)]
            rs = K.tile(st, "rs", [64, 512], F32)
            oh = [K.tile(st, f"oh{i}", [64, 512], BF16) for i in range(2)]
            pl = [K.psum(st, f"pl{i}") for i in range(2)]
            ptr = K.psum(st, "ptr", (128, 4, 128), BF16)
            pst = [K.psum(st, f"pst{i}") for i in range(2)]
            po = K.psum(st, "po")
            ps = K.psum(st, "ps")
            ei = [0]

            po_sb = K.tile(st, "po_sb", [64, 512], F32)
            ps_sb = K.tile(st, "ps_sb", [64, 512], F32)

            def attend(QA, KA, h, nq, key_tiles, tq):
                nt = len(key_tiles)
                base = ei[0]
                ei[0] += nt

                def emit_score(i):
                    k0, kn = key_tiles[i][0], key_tiles[i][1]
                    PST = pst[(base + i) % 2]
                    K.mm(PST[:kn, :nq], KA[:, k0:k0 + kn], QA[:, :nq], True, True, [KA, QA], [PST])

                emit_score(0)
                if nt > 1:
                    emit_score(1)
                for i, (k0, kn, v_ap, v_res, m_ap, m_res) in enumerate(key_tiles):
                    PST = pst[(base + i) % 2]
                    E_ = et[(base + i) % 2]
                    P_ = ptl[(base + i) % 2]
                    K.op(K.act, lambda: nc.scalar.activation(out=E_[:kn, :nq], in_=PST[:kn, :nq], func=AF.Exp, scale=0.125),
                         reads=[PST], writes=[E_])
                    if m_ap is not None:
                        K.op(K.dve, lambda: nc.vector.tensor_tensor(out=P_[:kn, :nq], in0=E_[:kn, :nq], in1=m_ap, op=ALU.mult),
                             reads=[E_, m_res], writes=[P_])
                        R_ = P_
                    else:
                        R_ = E_
                    if i + 2 < nt:
                        emit_score(i + 2)
                    K.mm(po[:64, :nq], v_ap, R_[:kn, :nq], i == 0, i == nt - 1, [v_res, R_], [po], sig=True)
                    K.mm(ps[:64, :nq], self.ones_b[:kn, :64], R_[:kn, :nq], i == 0, i == nt - 1, [self.ones_b, R_], [ps], sig=True)
                OH = oh[h % 2]
                K.op(K.act, lambda: nc.scalar.activation(out=ps_sb[:, :nq], in_=ps[:64, :nq], func=AF.Copy), reads=[ps], writes=[ps_sb])
                K.op(K.act, lambda: nc.scalar.activation(out=po_sb[:, :nq], in_=po[:64, :nq], func=AF.Copy), reads=[po], writes=[po_sb])
                K.op(K.dve, lambda: nc.vector.reciprocal(out=rs[:, :nq], in_=ps_sb[:, :nq]), reads=[ps_sb], writes=[rs])
                K.op(K.dve, lambda: nc.vector.tensor_tensor(out=OH[:, :nq], in0=po_sb[:, :nq], in1=rs[:, :nq], op=ALU.mult),
                     reads=[po_sb, rs], writes=[OH])
                K.dma(K.sp, self.mixA[h * 64:(h + 1) * 64, tq:tq + nq], OH[:, :nq], OH, reads=[OH])

            for h in range(8):
                QA = qa_sb[h % 2]
                KA = ka_sb[h % 2]
                K.dma(K.sp, QA[:, :NMETA], self.qaT[h * 64:(h + 1) * 64, 0:NMETA], QA, writes=[QA])
                K.dma(K.sp, KA[:, :NMETA], self.kaT[h * 64:(h + 1) * 64, 0:NMETA], KA, writes=[KA])
                attend(QA, KA, h, NMETA, [(0, NMETA, vmeta[:NMETA, h * 64:(h + 1) * 64], vmeta, cmeta[:, :], cmeta)], 0)

            for qb in range(NQB):
                nkt = 4 * (qb + 1)
                MT = maskT[qb % 2]
                K.op(K.dve, lambda: nc.vector.memset(MT[:, 4 * qb:4 * qb + 4, :], 0.0), writes=[MT])
                for jq in range(4):
                    K.maybe_reset()
                    qt = 4 * qb + jq
                    nk = (qt + 1) * 128
                    tq0 = NMETA + qt * 128
                    QI = qi_sb[jq % 2]
                    WI = wi_sb[jq % 2]
                    K.dma(K.sp, QI[:, :, :], self.qiT[:, tq0:tq0 + 128].rearrange("(h d) q -> d h q", d=64), QI, writes=[QI])
                    K.dma(K.sp, WI[:, :], self.wi[tq0:tq0 + 128, :], WI, writes=[WI])
                    nch = (nk + 511) // 512
                    ri = 0
                    for h in range(8):
                        for ch in range(nch):
                            c0 = ch * 512
                            w = min(512, nk - c0)
                            PL = pl[ri % 2]
                            RL = rl[ri % 2]
                            ri += 1
                            K.mm(PL[:, :w], QI[:, h, :], ki_sb[:, c0:c0 + w], True, True, [QI, ki_sb], [PL])
                            K.op(K.act, lambda: nc.scalar.activation(out=RL[:, :w], in_=PL[:, :w], func=AF.Relu),
                                 reads=[PL], writes=[RL])
                            if h == 0:
                                K.op(K.dve, lambda: nc.vector.tensor_scalar(out=score[:, c0:c0 + w], in0=RL[:, :w],
                                                                            scalar1=WI[:, 0:1], scalar2=None, op0=ALU.mult),
                                     reads=[RL, WI], writes=[score.sub(ch)])
                            else:
                                K.op(K.dve, lambda: nc.vector.scalar_tensor_tensor(
                                    out=score[:, c0:c0 + w], in0=RL[:, :w], scalar=WI[:, h:h + 1], in1=score[:, c0:c0 + w],
                                    op0=ALU.mult, op1=ALU.add), reads=[RL, WI, score.sub(ch)], writes=[score.sub(ch)])
                    K.op(K.dve, lambda: nc.vector.tensor_reduce(out=smax[:, :], in_=score[:, :nk], axis=AX.X, op=ALU.max),
                         reads=[score], writes=[smax])
                    K.op(K.dve, lambda: nc.vector.tensor_reduce(out=lo[:, :], in_=score[:, :nk], axis=AX.X, op=ALU.min),
                         reads=[score], writes=[lo])
                    K.op(K.dve, lambda: nc.vector.tensor_tensor(out=score[:, qt * 128:(qt + 1) * 128],
                                                                in0=score[:, qt * 128:(qt + 1) * 128], in1=negm[:, :], op=ALU.add),
                         reads=[score, negm], writes=[score])
                    K.op(K.dve, lambda: nc.vector.tensor_tensor(out=w0[:, :], in0=smax[:, :], in1=lo[:, :], op=ALU.subtract),
                         reads=[smax, lo], writes=[w0])
                    for it in range(NIT):
                        c = 2.0 ** -(it + 1)
                        K.op(K.dve, lambda: nc.vector.scalar_tensor_tensor(out=mid[:, :], in0=w0[:, :], scalar=c, in1=lo[:, :],
                                                                           op0=ALU.mult, op1=ALU.add),
                             reads=[w0, lo], writes=[mid])
                        K.op(K.dve, lambda: nc.vector.tensor_scalar(out=mask01[:, :nk], in0=score[:, :nk], scalar1=mid[:, 0:1],
                                                                    scalar2=None, op0=ALU.is_ge, op1=ALU.add,
                                                                    accum_out=cnt[:, 0:1]),
                             reads=[score, mid], writes=[mask01, cnt])
                        K.op(K.dve, lambda: nc.vector.tensor_scalar(out=gg[:, :], in0=cnt[:, :], scalar1=TOPK, scalar2=c,
                                                                    op0=ALU.is_ge, op1=ALU.mult), reads=[cnt], writes=[gg])
                        K.op(K.dve, lambda: nc.vector.scalar_tensor_tensor(out=lo[:, :], in0=gg[:, :], scalar=w0[:, 0:1],
                                                                           in1=lo[:, :], op0=ALU.mult, op1=ALU.add),
                             reads=[gg, w0, lo], writes=[lo])
                    K.op(K.dve, lambda: nc.vector.tensor_scalar(out=mask01[:, :nk], in0=score[:, :nk], scalar1=lo[:, 0:1],
                                                                scalar2=None, op0=ALU.is_ge), reads=[score, lo], writes=[mask01])
                    for kt0 in range(0, qt + 1, 4):
                        g_ = min(4, qt + 1 - kt0)
                        for i in range(g_):
                            K.op(K.pe, lambda: nc.tensor.transpose(out=ptr[:, i, :], in_=mask01[:, (kt0 + i) * 128:(kt0 + i + 1) * 128],
                                                                   identity=ident[:, :]),
                                 reads=[mask01, ident], writes=[ptr], sig=(i == g_ - 1))
                        K.op(K.act, lambda: nc.scalar.activation(out=MT[:, kt0:kt0 + g_, jq * 128:(jq + 1) * 128],
                                                                 in_=ptr[:, 0:g_, :], func=AF.Copy), reads=[ptr], writes=[MT])
                tq = NMETA + qb * 512

                def load_head(h):
                    QA = qa_sb[h % 2]
                    KA = ka_sb[h % 2]
                    K.dma(K.sp, QA[:, :], self.qaT[h * 64:(h + 1) * 64, tq:tq + 512], QA, writes=[QA])
                    K.dma(K.sp, KA[:, :NMETA + nkt * 128], self.kaT[h * 64:(h + 1) * 64, 0:NMETA + nkt * 128], KA, writes=[KA])
                    return QA, KA

                nxt = load_head(0)
                for h in range(8):
                    K.maybe_reset()
                    QA, KA = nxt
                    if h + 1 < 8:
                        nxt = load_head(h + 1)
                    tiles = [(0, NMETA, vmeta[:NMETA, h * 64:(h + 1) * 64], vmeta, None, None)]
                    for kt in range(nkt):
                        tiles.append((NMETA + kt * 128, 128, va_sb[:, kt, h * 64:(h + 1) * 64], va_sb.sub(kt // 8),
                                      MT[:, kt, :], MT))
                    attend(QA, KA, h, 512, tiles, tq)
            K.end_phase()

    def diff_phase(self, li):
        K = self.K
        nc = self.nc
        S = self.S
        j = li // 2
        lam_init = 0.8 - 0.6 * math.exp(-0.3 * li)
        NKT = S // 128
        NQB = S // 512
        with ExitStack() as st:
            self.consts(st)
            cmT = K.tile(st, "cmT", [128, 4, 512], BF16)
            K.dma(K.sp, cmT[:, :, :], self.c_cmaskT.rearrange("p (j q) -> p j q", q=512), cmT, writes=[cmT])
            cmeta = K.tile(st, "cmeta", [16, 16], BF16)
            K.dma(K.sp, cmeta[:, :], self.c_cmeta[:, :], cmeta, writes=[cmeta])
            dl = self.load_cols(st, "dl", self.dlam_bc[:, j * 256:(j + 1) * 256], 256)
            sgc2 = self.load_cols(st, "sgc", self.subln_c[:, :], 2)
            pr = K.tile(st, "pr", [128, 64], F32)
            s12 = K.tile(st, "s12", [128, 2], F32)
            neglam = K.tile(st, "neglam", [128, 1], F32)
            sgs = K.tile(st, "sgs", [128, 1], F32)
            for r in range(2):
                K.op(K.dve, lambda: nc.vector.tensor_tensor(out=pr[:, :], in0=dl[:, r * 128:r * 128 + 64],
                                                            in1=dl[:, r * 128 + 64:r * 128 + 128], op=ALU.mult),
                     reads=[dl], writes=[pr])
                K.op(K.dve, lambda: nc.vector.tensor_reduce(out=s12[:, r:r + 1], in_=pr[:, :], axis=AX.X, op=ALU.add),
                     reads=[pr], writes=[s12])
            K.op(K.act, lambda: nc.scalar.activation(out=s12[:, :], in_=s12[:, :], func=AF.Exp), reads=[s12], writes=[s12])
            K.op(K.dve, lambda: nc.vector.tensor_tensor(out=neglam[:, :], in0=s12[:, 1:2], in1=s12[:, 0:1], op=ALU.subtract),
                 reads=[s12], writes=[neglam])
            K.op(K.dve, lambda: nc.vector.tensor_scalar(out=neglam[:, :], in0=neglam[:, :], scalar1=-lam_init, scalar2=None,
                                                        op0=ALU.add), reads=[neglam], writes=[neglam])
            K.op(K.dve, lambda: nc.vector.tensor_scalar(out=sgs[:, :], in0=sgc2[:, j:j + 1], scalar1=1.0 - lam_init, scalar2=None,
                                                        op0=ALU.mult), reads=[sgc2], writes=[sgs])
            vb_sb = K.tile(st, "vb_sb", [128, NKT, 512], BF16)
            vbv = self.vb[NMETA:, :].rearrange("(kt p) c -> p kt c", p=128)
            for g0 in range(0, NKT, 8):
                g1 = min(NKT, g0 + 8)
                K.dma(K.sp, vb_sb[:, g0:g1, :], vbv[:, g0:g1, :], vb_sb.slot(g0), writes=[vb_sb.sub(g0 // 8)])
            vmeta = K.tile(st, "vbmeta", [16, 512], BF16)
            K.dma(K.sp, vmeta[:, :], self.vb[0:NMETA, :], vmeta, writes=[vmeta])
            qb_sb = [K.tile(st, f"qb{i}", [64, 2, 512], BF16) for i in range(2)]
            kb_sb = [K.tile(st, f"kb{i}", [64, 2, NMETA + S], BF16) for i in range(2)]
            et = [K.tile(st, f"et{i}", [128, 512], BF16) for i in range(4)]
            r1 = K.tile(st, "r1", [128, 512], F32)
            a1 = K.tile(st, "a1", [128, 512], F32)
            a2 = K.tile(st, "a2", [128, 512], F32)
            sq = K.tile(st, "sqd", [128, 512], F32)
            yt = [K.tile(st, f"yt{i}", [128, 512], BF16) for i in range(2)]
            pst = [K.psum(st, f"pst{i}") for i in range(2)]
            oc_ = [K.psum(st, f"o{i}") for i in range(2)]
            sc_ = [K.psum(st, f"s{i}") for i in range(2)]
            pss = K.psum(st, "pss")
            ei = [0]

            def attend(QB, KB, h, nq, key_tiles, tq):
                steps = [(i, c) for i in range(len(key_tiles)) for c in range(2)]
                nt = len(key_tiles)
                ns = len(steps)
                base = ei[0]
                ei[0] += ns

                def emit_score(si):
                    i, c = steps[si]
                    k0, kn = key_tiles[i][0], key_tiles[i][1]
                    PST = pst[(base + si) % 2]
                    K.mm(PST[:kn, :nq], KB[:, c, k0:k0 + kn], QB[:, c, :nq], True, True, [KB, QB], [PST])

                emit_score(0)
                emit_score(1)
                for si, (i, c) in enumerate(steps):
                    (k0, kn, v_ap, v_res, m_ap, m_res) = key_tiles[i]
                    PST = pst[(base + si) % 2]
                    E_ = et[(base + si) % 4]
                    K.op(K.act, lambda: nc.scalar.activation(out=E_[:kn, :nq], in_=PST[:kn, :nq], func=AF.Exp, scale=0.125),
                         reads=[PST], writes=[E_])
                    if m_ap is not None:
                        K.op(K.dve, lambda: nc.vector.tensor_tensor(out=E_[:kn, :nq], in0=E_[:kn, :nq], in1=m_ap, op=ALU.mult),
                             reads=[E_, m_res], writes=[E_])
                    if si + 2 < ns:
                        emit_score(si + 2)
                    K.mm(oc_[c][:, :nq], v_ap, E_[:kn, :nq], i == 0, i == nt - 1, [v_res, E_], [oc_[c]], sig=True)
                    K.mm(sc_[c][:, :nq], self.ones_b[:kn, :], E_[:kn, :nq], i == 0, i == nt - 1, [self.ones_b, E_], [sc_[c]], sig=True)
                Y = yt[h % 2]
                K.op(K.dve, lambda: nc.vector.reciprocal(out=r1[:, :nq], in_=sc_[0][:, :nq]), reads=[sc_[0]], writes=[r1])
                K.op(K.dve, lambda: nc.vector.tensor_tensor(out=a1[:, :nq], in0=oc_[0][:, :nq], in1=r1[:, :nq], op=ALU.mult),
                     reads=[oc_[0], r1], writes=[a1])
                K.op(K.dve, lambda: nc.vector.reciprocal(out=r1[:, :nq], in_=sc_[1][:, :nq]), reads=[sc_[1]], writes=[r1])
                K.op(K.dve, lambda: nc.vector.tensor_tensor(out=a2[:, :nq], in0=oc_[1][:, :nq], in1=r1[:, :nq], op=ALU.mult),
                     reads=[oc_[1], r1], writes=[a2])
                K.op(K.dve, lambda: nc.vector.scalar_tensor_tensor(out=a1[:, :nq], in0=a2[:, :nq], scalar=neglam[:, 0:1],
                                                                   in1=a1[:, :nq], op0=ALU.mult, op1=ALU.add),
                     reads=[a2, neglam, a1], writes=[a1])
                K.op(K.act, lambda: nc.scalar.activation(out=sq[:, :nq], in_=a1[:, :nq], func=AF.Square), reads=[a1], writes=[sq])
                for c0 in range(0, nq, 256):
                    c1 = min(nq, c0 + 256)
                    K.op(K.pe, lambda: nc.tensor.matmul(out=pss[:, c0:c1], lhsT=self.ones_f[:, :], rhs=sq[:, c0:c1],
                                                        start=True, stop=True), reads=[self.ones_f, sq], writes=[pss])
                K.op(K.act, lambda: nc.scalar.activation(out=r1[:, :nq], in_=pss[:, :nq], func=AF.Sqrt,
                                                         bias=self.eps_t[:, 0:1], scale=1.0 / 128),
                     reads=[pss, self.eps_t], writes=[r1])
                K.op(K.dve, lambda: nc.vector.reciprocal(out=r1[:, :nq], in_=r1[:, :nq]), reads=[r1], writes=[r1])
                K.op(K.dve, lambda: nc.vector.scalar_tensor_tensor(out=Y[:, :nq], in0=a1[:, :nq], scalar=sgs[:, 0:1],
                                                                   in1=r1[:, :nq], op0=ALU.mult, op1=ALU.mult),
                     reads=[a1, sgs, r1], writes=[Y])
                K.dma(K.sp, self.mixB[h * 128:(h + 1) * 128, tq:tq + nq], Y[:, :nq], Y, reads=[Y])

            def load_qk(h, tq, nq, nkeys):
                QB = qb_sb[h % 2]
                KB = kb_sb[h % 2]
                K.dma(K.sp, QB[:, :, :nq], self.qbT[h * 128:(h + 1) * 128, tq:tq + nq].rearrange("(c d) q -> d c q", d=64),
                      QB, writes=[QB])
                K.dma(K.sp, KB[:, :, :nkeys], self.kbT[h * 128:(h + 1) * 128, 0:nkeys].rearrange("(c d) q -> d c q", d=64),
                      KB, writes=[KB])
                return QB, KB

            for h in range(4):
                QB, KB = load_qk(h, 0, NMETA, NMETA)
                attend(QB, KB, h, NMETA, [(0, NMETA, vmeta[:NMETA, h * 128:(h + 1) * 128], vmeta, cmeta[:, :], cmeta)], 0)
            for qb in range(NQB):
                nkt = 4 * (qb + 1)
                tq = NMETA + qb * 512
                nxt = load_qk(0, tq, 512, NMETA + nkt * 128)
                for h in range(4):
                    K.maybe_reset()
                    QB, KB = nxt
                    if h + 1 < 4:
                        nxt = load_qk(h + 1, tq, 512, NMETA + nkt * 128)
                    tiles = [(0, NMETA, vmeta[:NMETA, h * 128:(h + 1) * 128], vmeta, None, None)]
                    for kt in range(nkt):
                        dj = kt - 4 * qb
                        tiles.append((NMETA + kt * 128, 128, vb_sb[:, kt, h * 128:(h + 1) * 128], vb_sb.sub(kt // 8),
                                      cmT[:, dj, :] if dj >= 0 else None, cmT if dj >= 0 else None))
                    attend(QB, KB, h, 512, tiles, tq)
            K.end_phase()

    def attn_out_phase(self, src, li):
        K = self.K
        nc = self.nc
        j = li // 2
        NB = 256
        blocks = token_blocks(self.S, NB)
        with ExitStack() as st:
            woA = K.tile(st, "woA", [64, 8, D], BF16)
            woB = K.tile(st, "woB", [128, 4, D], BF16)
            wav = self.attn_w_out[j, 0:512, :].rearrange("(h d) m -> d h m", d=64)
            wbv = self.attn_w_out[j, 512:1024, :].rearrange("(h p) m -> p h m", p=128)
            for q in range(2):
                self.wload(K.swslot(), woA[:, q * 4:(q + 1) * 4, :], wav[:, q * 4:(q + 1) * 4, :], [woA.sub(q)])
                self.wload(K.swslot(), woB[:, q * 2:(q + 1) * 2, :], wbv[:, q * 2:(q + 1) * 2, :], [woB.sub(q)])
            xt = [K.tile(st, f"xt{i}", [128, 8, NB], F32) for i in range(2)]
            mA = [K.tile(st, f"mA{i}", [64, 8, NB], BF16) for i in range(2)]
            mB = [K.tile(st, f"mB{i}", [128, 4, NB], BF16) for i in range(2)]
            po = [K.psum(st, f"po{i}") for i in range(2)]
            srcv = src.rearrange("(c p) t -> p c t", p=128)
            dstv = self.hT.rearrange("(c p) t -> p c t", p=128)
            for b, (t0, n) in enumerate(blocks):
                K.maybe_reset()
                X = xt[b % 2]
                A = mA[b % 2]
                B_ = mB[b % 2]
                K.dma(K.sp, X[:, :, :n], srcv[:, :, t0:t0 + n], X, reads=[K.dres("h", b)], writes=[X])
                K.dma(K.sp, A[:, :, :n], self.mixA[:, t0:t0 + n].rearrange("(h d) t -> d h t", d=64), A, writes=[A])
                K.dma(K.sp, B_[:, :, :n], self.mixB[:, t0:t0 + n].rearrange("(h p) t -> p h t", p=128), B_, writes=[B_])
                for m in range(8):
                    P = po[m % 2]
                    for h in range(8):
                        K.mm(P[:, :n], woA[:, h, m * 128:(m + 1) * 128], A[:, h, :n], h == 0, False, [woA, A], [P])
                    for hb in range(4):
                        K.mm(P[:, :n], woB[:, hb, m * 128:(m + 1) * 128], B_[:, hb, :n], False, hb == 3, [woB, B_], [P])
                    K.op(K.dve, lambda: nc.vector.tensor_tensor(out=X[:, m, :n], in0=P[:, :n], in1=X[:, m, :n], op=ALU.add),
                         reads=[P, X.sub(m)], writes=[X.sub(m)])
                K.dma(K.sp, dstv[:, :, t0:t0 + n], X[:, :, :n], X, reads=[X], writes=[K.dres("h", b)])
            K.end_phase()

    def copy_phase(self, src):
        K = self.K
        NB = 256
        blocks = token_blocks(self.S, NB)
        with ExitStack() as st:
            xt = [K.tile(st, f"xt{i}", [128, 8, NB], F32) for i in range(2)]
            srcv = src.rearrange("(c p) t -> p c t", p=128)
            dstv = self.hT.rearrange("(c p) t -> p c t", p=128)
            for b, (t0, n) in enumerate(blocks):
                K.maybe_reset()
                X = xt[b % 2]
                K.dma(K.sp, X[:, :, :n], srcv[:, :, t0:t0 + n], X, writes=[X])
                K.dma(K.sp, dstv[:, :, t0:t0 + n], X[:, :, :n], X, reads=[X])
            K.end_phase()

    def final_phase(self, src):
        K = self.K
        nc = self.nc
        NB = 256
        blocks = token_blocks(self.S, NB)[1:]
        with ExitStack() as st:
            self.consts(st)
            gcol = self.load_cols(st, "gcol", self.fin_gc[:, :], 8)
            xt = [K.tile(st, f"xt{i}", [128, 8, NB], F32) for i in range(2)]
            sqt = [K.tile(st, f"sq{i}", [128, NB], F32) for i in range(2)]
            rstd = K.tile(st, "rstd", [128, NB], F32)
            ss = K.psum(st, "ss")
            srcv = src.rearrange("(c p) t -> p c t", p=128)
            dstv = self.yT.rearrange("(c p) t -> p c t", p=128)
            for b, (t0, n) in enumerate(blocks):
                K.maybe_reset()
                X = xt[b % 2]
                K.dma(K.sp, X[:, :, :n], srcv[:, :, t0:t0 + n], X, writes=[X])
                self.rms_rstd(None, X, n, sqt, ss, rstd, self.ones_f)
                for c in range(8):
                    K.op(K.dve, lambda c=c, X=X: nc.vector.scalar_tensor_tensor(
                        out=X[:, c, :n], in0=X[:, c, :n], scalar=gcol[:, c:c + 1], in1=rstd[:, :n],
                        op0=ALU.mult, op1=ALU.mult), reads=[X.sub(c), gcol, rstd], writes=[X.sub(c)])
                K.dma(K.sp, dstv[:, :, t0 - NMETA:t0 - NMETA + n], X[:, :, :n], X, reads=[X])
            K.end_phase()


def full_plan():
    plan = []
    for i in range(DEPTH):
        plan.append(("ffn", i, 0))
        plan.append(("attn", i) if i % 2 == 0 else ("rec", i))
        plan.append(("ffn", i, 1))
    return plan


def cols128(v):
    v = np.asarray(v, np.float32)
    return np.ascontiguousarray(v.reshape(8, 128).T)


def host_inputs(inp, S):
    f32 = np.float32
    shared = {}
    ng = np.asarray(inp["norm_g"], f32)
    shared["norm_gc"] = np.ascontiguousarray(
        np.concatenate([cols128(ng[i, k]) for i in range(DEPTH) for k in range(3)], axis=1))
    shared["fin_gc"] = cols128(inp["final_norm_g"])
    for k in ["ffn_w_gu", "ffn_w_down", "attn_w_in", "attn_w_out", "rec_w_in", "rec_w_out", "rec_gate_w"]:
        shared[k] = np.ascontiguousarray(np.asarray(inp[k], f32))
    lg = np.asarray(inp["idx_k_ln_g"], f32)
    lb = np.asarray(inp["idx_k_ln_b"], f32)
    shared["idx_lnc"] = np.ascontiguousarray(np.stack([lg[0], lb[0], lg[1], lb[1]], axis=1))
    dl = np.asarray(inp["diff_lambda"], f32).reshape(1, 2 * 256)
    shared["dlam_bc"] = np.ascontiguousarray(np.broadcast_to(dl, (128, 512)))
    shared["subln_c"] = np.ascontiguousarray(np.asarray(inp["diff_subln_g"], f32).T)
    rc = []
    for j in range(2):
        for w in range(4):
            rc.append(cols128(inp["rec_conv_w"][j][w]))
        rc.append(cols128(inp["rec_conv_b"][j]))
        rc.append(cols128(inp["rec_gate_b"][j][0]))
        rc.append(cols128(inp["rec_gate_b"][j][1]))
        rc.append(cols128(inp["rec_lambda"][j]))
    shared["rec_cols"] = np.ascontiguousarray(np.concatenate(rc, axis=1))
    shared["c_ident_bf"] = np.eye(128, dtype=f32).astype(ml_dtypes.bfloat16)
    q = np.arange(128)[:, None]
    k = np.arange(128)[None, :]
    shared["c_negmask"] = np.where(k <= q, 0.0, NEG).astype(f32)
    kk = np.arange(128)[:, None, None]
    jj = np.arange(4)[None, :, None]
    qq = np.arange(512)[None, None, :]
    shared["c_cmaskT"] = np.ascontiguousarray(
        ((128 * jj + kk) <= qq).astype(f32).reshape(128, 2048)).astype(ml_dtypes.bfloat16)
    k16 = np.arange(16)[:, None]
    q16 = np.arange(16)[None, :]
    shared["c_cmeta"] = (k16 <= q16).astype(f32).astype(ml_dtypes.bfloat16)
    x = np.asarray(inp["x"], f32)
    meta = np.asarray(inp["meta_tokens"], f32)
    maps = []
    for b in range(x.shape[0]):
        m = dict(shared)
        m["xT"] = np.ascontiguousarray(np.concatenate([meta, x[b]], axis=0).T)
        maps.append(m)
    return maps


_CACHE = {}


def run(inp, plan=None):
    x = np.asarray(inp["x"])
    B, S, _ = x.shape
    plan = full_plan() if plan is None else plan
    key = (S, tuple(plan))
    if key not in _CACHE:
        _CACHE[key] = Prog(S, plan)
    prog = _CACHE[key]
    maps = host_inputs(inp, S)
    res = run_bass_kernel_spmd(prog.nc, maps, core_ids=list(range(B)))
    out = np.stack([np.ascontiguousarray(res.results[b]["yT"].T) for b in range(B)], axis=0)
    return out.astype(np.float32)


def kernel(**inputs):
    return run(inputs)
```

```python
import math
from contextlib import ExitStack

import numpy as np
import ml_dtypes
import concourse.bass as bass
import concourse.mybir as mybir
from concourse.bass_utils import run_bass_kernel_spmd

F32 = mybir.dt.float32
BF16 = mybir.dt.bfloat16
AF = mybir.ActivationFunctionType
ALU = mybir.AluOpType
AX = mybir.AxisListType

D = 1024
NMETA = 16
DFF = 2816
EPS = 1e-6
DEPTH = 4
ATTN_IN = 3656
NEG = -1.0e30
RESET_LIMIT = 700


ALL_RES = []


class Res:
    __slots__ = ("last_w", "readers", "parent", "kids")

    def __init__(self, parent=None):
        ALL_RES.append(self)
        self.last_w = None
        self.readers = {}
        self.parent = parent
        self.kids = []
        if parent is not None:
            parent.kids.append(self)

    def related(self):
        out = [self]
        p = self.parent
        while p is not None:
            out.append(p)
            p = p.parent
        stack = list(self.kids)
        while stack:
            k = stack.pop()
            out.append(k)
            stack.extend(k.kids)
        return out


class Tile:
    def __init__(self, K, handle, name):
        self.K = K
        self.h = handle
        self.name = name
        self.res = Res()
        self.subs = {}
        self.slots = {}
        self.dsem = None
        self.dcnt = 0

    def __getitem__(self, idx):
        return self.h[idx]

    def slot(self, key):
        sl = self.slots.get(key)
        if sl is None:
            sl = Tile(self.K, self.h, f"{self.name}_s{key}")
            self.slots[key] = sl
            self.K.phase_tiles.append(sl)
        return sl

    def sub(self, key):
        r = self.subs.get(key)
        if r is None:
            r = Res(self.res)
            self.subs[key] = r
        return r


class Eng:
    def __init__(self, K, name, eng, self_dep):
        self.K = K
        self.name = name
        self.eng = eng
        self.sem = K.nc.alloc_semaphore("es_" + name)
        self.cnt = 0
        self.seen = {}
        self.self_dep = self_dep


class Kb:
    def __init__(self, nc):
        self.nc = nc
        fs = list(nc.free_semaphores)
        nc.gpsimd.sem_clear(range(min(fs), max(fs) + 1))
        self.pe = Eng(self, "pe", nc.tensor, False)
        self.act = Eng(self, "act", nc.scalar, True)
        self.dve = Eng(self, "dve", nc.vector, True)
        self.pool = Eng(self, "pool", nc.gpsimd, True)
        self.sp = Eng(self, "sp", nc.sync, True)
        self.engs = [self.pe, self.act, self.dve, self.pool, self.sp]
        self.sems = {}
        for e in self.engs:
            self.sems[id(e.sem)] = (e.sem, e)
        self.dram_res = {}
        self.phase_tiles = []
        self.n_ops = 0
        self.free_hw = []
        self.sw_holders = []
        self.sw_idx = 0
        self.uid = 0
        for e in self.engs:
            nc.gpsimd.sem_clear(e.sem)
        nc.all_engine_barrier()

    def tile(self, st, name, shape, dtype):
        self.uid += 1
        name = f"{name}_{self.uid}"
        h = st.enter_context(self.nc.sbuf_tensor(name, list(shape), dtype))
        t = Tile(self, h, name)
        self.phase_tiles.append(t)
        return t

    def psum(self, st, name, shape=(128, 512), dtype=F32):
        self.uid += 1
        name = f"{name}_{self.uid}"
        h = st.enter_context(self.nc.psum_tensor(name, list(shape), dtype))
        return Tile(self, h, name)

    def swslot(self):
        if self.sw_idx == len(self.sw_holders):
            hld = Tile(self, None, f"sw{self.sw_idx}")
            hld.dsem = self.nc.alloc_semaphore(f"ds_sw{self.sw_idx}")
            hld.dsem_sw = True
            self.sems[id(hld.dsem)] = (hld.dsem, hld)
            self.sw_holders.append(hld)
        hld = self.sw_holders[self.sw_idx]
        self.sw_idx += 1
        return hld

    def dres(self, *key):
        r = self.dram_res.get(key)
        if r is None:
            r = Res()
            self.dram_res[key] = r
        return r

    def _need(self, reads, writes):
        need = {}

        def add(rec):
            if rec is None:
                return
            k, v = rec
            if need.get(k, 0) < v:
                need[k] = v

        for r in reads:
            for x in r.related():
                add(x.last_w)
        for w in writes:
            for x in w.related():
                add(x.last_w)
                for k, v in x.readers.items():
                    add((k, v))
        return need

    def _wait(self, E, need):
        for k, v in need.items():
            sem, owner = self.sems[k]
            if owner is E and not E.self_dep:
                continue
            if E.seen.get(k, 0) >= v:
                continue
            E.eng.wait_ge(sem, v)
            E.seen[k] = v

    @staticmethod
    def _resl(xs):
        out = []
        for x in xs:
            out.append(x.res if isinstance(x, Tile) else x)
        return out

    def op(self, E, fn, reads=(), writes=(), sig=True):
        reads = self._resl(reads)
        writes = self._resl(writes)
        self._wait(E, self._need(reads, writes))
        ins = fn()
        if sig:
            E.cnt += 1
            ins.then_inc(E.sem, 1)
            rec = (id(E.sem), E.cnt)
        else:
            rec = (id(E.sem), E.cnt + 1)
        for r in reads:
            if r.readers.get(rec[0], 0) < rec[1]:
                r.readers[rec[0]] = rec[1]
        for w in writes:
            w.last_w = rec
            w.readers = {}
        self.n_ops += 1
        return ins

    def mm(self, out, lhsT, rhs, start, stop, reads, writes, sig=None):
        nc = self.nc
        return self.op(self.pe, lambda: nc.tensor.matmul(out=out, lhsT=lhsT, rhs=rhs, start=start, stop=stop),
                       reads=reads, writes=writes, sig=stop if sig is None else sig)

    def dma(self, E, out, in_, stile, reads=(), writes=(), **kw):
        reads = self._resl(reads)
        writes = self._resl(writes)
        if stile.dsem is None:
            assert E is not self.pool, "gpsimd DMAs must use K.swslot() holders"
            stile.dsem = self.free_hw.pop() if self.free_hw else self.nc.alloc_semaphore("ds_" + stile.name)
            stile.dsem_sw = False
            self.sems[id(stile.dsem)] = (stile.dsem, stile)
        assert stile.dsem_sw == (E is self.pool)
        self._wait(E, self._need(reads, writes))
        ins = E.eng.dma_start(out=out, in_=in_, **kw)
        stile.dcnt += 16
        ins.then_inc(stile.dsem, 16)
        rec = (id(stile.dsem), stile.dcnt)
        for r in reads:
            if r.readers.get(rec[0], 0) < rec[1]:
                r.readers[rec[0]] = rec[1]
        for w in writes:
            w.last_w = rec
            w.readers = {}
        self.n_ops += 1
        return ins

    def reset(self):
        nc = self.nc
        sp = self.sp
        for e in self.engs:
            if e is not sp and e.cnt > 0 and sp.seen.get(id(e.sem), 0) < e.cnt:
                sp.eng.wait_ge(e.sem, e.cnt)
        for t in self.phase_tiles + self.sw_holders:
            if t.dsem is not None and t.dcnt > 0:
                sp.eng.wait_ge(t.dsem, t.dcnt)
        nc.all_engine_barrier()
        for e in self.engs:
            nc.gpsimd.sem_clear(e.sem)
            e.cnt = 0
            e.seen = {}
        for t in self.phase_tiles:
            if t.dsem is not None:
                nc.gpsimd.sem_clear(t.dsem)
                t.dcnt = 0
        nc.all_engine_barrier()
        for r in ALL_RES:
            r.last_w = None
            r.readers = {}

    def maybe_reset(self, limit=RESET_LIMIT):
        if max(e.cnt for e in self.engs) > limit:
            self.reset()

    def end_phase(self):
        self.reset()
        dsems = []
        for t in self.phase_tiles:
            if t.dsem is not None:
                dsems.append((t.dsem, t.dsem_sw))
                del self.sems[id(t.dsem)]
                t.dsem = None
        del ALL_RES[:]
        for s, sw in dsems:
            self.free_hw.append(s)
        self.sw_idx = 0
        self.phase_tiles = []
        self.dram_res = {}


def token_blocks(S, NB):
    blocks = [(0, NMETA)]
    for i in range(S // NB):
        blocks.append((NMETA + i * NB, NB))
    return blocks


class Prog:
    def __init__(self, S, plan):
        self.S = S
        self.T = S + NMETA
        self.plan = plan
        nc = bass.Bass("TRN2", target_bir_lowering=False)
        self.nc = nc
        T = self.T

        def din(name, shape, dt=F32):
            return nc.dram_tensor(name, list(shape), dt, kind="ExternalInput").ap()

        self.xin = din("xT", [D, T])
        self.norm_gc = din("norm_gc", [128, DEPTH * 3 * 8])
        self.fin_gc = din("fin_gc", [128, 8])
        self.w_gu = din("ffn_w_gu", [DEPTH, 2, D, 2 * DFF])
        self.w_dn = din("ffn_w_down", [DEPTH, 2, DFF, D])
        self.attn_w_in = din("attn_w_in", [2, D, ATTN_IN])
        self.attn_w_out = din("attn_w_out", [2, D, D])
        self.idx_lnc = din("idx_lnc", [64, 4])
        self.dlam_bc = din("dlam_bc", [128, 2 * 256])
        self.subln_c = din("subln_c", [128, 2])
        self.rec_w_in = din("rec_w_in", [2, D, 2 * D])
        self.rec_w_out = din("rec_w_out", [2, D, D])
        self.rec_gate_w = din("rec_gate_w", [2, 2, 4, 256, 256])
        self.rec_cols = din("rec_cols", [128, 2 * 8 * 8])
        self.c_ident_bf = din("c_ident_bf", [128, 128], BF16)
        self.c_negmask = din("c_negmask", [128, 128])
        self.c_cmaskT = din("c_cmaskT", [128, 4 * 512], BF16)
        self.c_cmeta = din("c_cmeta", [16, 16], BF16)
        self.yT = nc.dram_tensor("yT", [D, S], F32, kind="ExternalOutput").ap()
        self.hT = nc.dram_tensor("hT", [D, T], F32).ap()
        self.TOPK = min(256, S // 4)

        def scr(name, shape, dt=BF16):
            return nc.dram_tensor(name, list(shape), dt).ap()

        self.qaT = scr("qaT", [512, T])
        self.kaT = scr("kaT", [512, T])
        self.va = scr("va", [T, 512])
        self.qiT = scr("qiT", [512, T])
        self.kiT = scr("kiT", [64, T])
        self.wi = scr("wi", [T, 8], F32)
        self.qbT = scr("qbT", [512, T])
        self.kbT = scr("kbT", [512, T])
        self.vb = scr("vb", [T, 512])
        self.mixA = scr("mixA", [512, T])
        self.mixB = scr("mixB", [512, T])
        self.K = Kb(nc)
        self.build()

    def build(self):
        src = self.xin
        for ph in self.plan:
            kind = ph[0]
            if kind == "ffn":
                self.ffn_phase(src, ph[1], ph[2])
                src = self.hT
            elif kind == "copy":
                self.copy_phase(src)
                src = self.hT
            elif kind == "rec":
                self.rec_phase(src, ph[1])
                src = self.hT
            elif kind == "aproj":
                self.attn_proj_phase(src, ph[1])
            elif kind == "dsa":
                self.dsa_phase(ph[1])
            elif kind == "diff":
                self.diff_phase(ph[1])
            elif kind == "aout":
                self.attn_out_phase(src, ph[1])
                src = self.hT
            elif kind == "attn":
                self.attn_proj_phase(src, ph[1])
                self.dsa_phase(ph[1])
                self.diff_phase(ph[1])
                self.attn_out_phase(src, ph[1])
                src = self.hT
            else:
                raise ValueError(kind)
        self.final_phase(src)

    def rms_rstd(self, st_tiles, X, n, sqt, ss, rstd, ones):
        K = self.K
        for c in range(8):
            s = sqt[c % 2]
            if getattr(self, "dve_square", False):
                K.op(K.dve, lambda s=s, c=c: K.nc.vector.tensor_tensor(out=s[:, :n], in0=X[:, c, :n], in1=X[:, c, :n], op=ALU.mult),
                     reads=[X.sub(c)], writes=[s])
            else:
                K.op(K.act, lambda s=s, c=c: K.nc.scalar.activation(out=s[:, :n], in_=X[:, c, :n], func=AF.Square),
                     reads=[X.sub(c)], writes=[s])
            K.op(K.pe, lambda s=s, c=c: K.nc.tensor.matmul(out=ss[:, :n], lhsT=ones[:, :], rhs=s[:, :n],
                                                          start=(c == 0), stop=(c == 7)),
                 reads=[s, ones], writes=[ss])
        K.op(K.act, lambda: K.nc.scalar.activation(out=rstd[:, :n], in_=ss[:, :n], func=AF.Sqrt,
                                                   bias=self.eps_t[:, 0:1], scale=1.0 / D),
             reads=[ss, self.eps_t], writes=[rstd])
        K.op(K.dve, lambda: K.nc.vector.reciprocal(out=rstd[:, :n], in_=rstd[:, :n]), reads=[rstd], writes=[rstd])

    def consts(self, st):
        K = self.K
        nc = self.nc
        self.ones_f = K.tile(st, "ones_f", [128, 128], F32)
        self.ones_b = K.tile(st, "ones_b", [128, 128], BF16)
        self.eps_t = K.tile(st, "eps_t", [128, 1], F32)
        self.one_t = K.tile(st, "one_t", [128, 1], F32)
        K.op(K.dve, lambda: nc.vector.memset(self.one_t[:, :], 1.0), writes=[self.one_t])
        K.op(K.dve, lambda: nc.vector.memset(self.ones_f[:, :], 1.0), writes=[self.ones_f])
        K.op(K.dve, lambda: nc.vector.memset(self.ones_b[:, :], 1.0), writes=[self.ones_b])
        K.op(K.dve, lambda: nc.vector.memset(self.eps_t[:, :], EPS), writes=[self.eps_t])

    def load_cols(self, st, name, src_ap, ncols, parts=128):
        K = self.K
        t = K.tile(st, name, [parts, ncols], F32)
        K.dma(K.sp, t[:, :], src_ap, t, writes=[t])
        return t

    def wload(self, t, dst_ap, src_ap, writes):
        K = self.K
        K.dma(K.pool, dst_ap, src_ap, t, writes=writes, max_dma_last_dim=4096)

    def ffn_phase(self, src, li, fi):
        K = self.K
        nc = self.nc
        NB = 256
        blocks = token_blocks(self.S, NB)
        with ExitStack() as st:
            self.consts(st)
            wgu = K.tile(st, "wgu", [128, 8, 2 * DFF], BF16)
            wd = K.tile(st, "wd", [128, 22, D], BF16)
            ni = li * 3 + (0 if fi == 0 else 2)
            gcol = self.load_cols(st, "gcol", self.norm_gc[:, ni * 8:(ni + 1) * 8], 8)
            for k in range(8):
                self.wload(K.swslot(), wgu[:, k, :], self.w_gu[li, fi, k * 128:(k + 1) * 128, :], [wgu.sub(k)])
            wdv = self.w_dn[li, fi].rearrange("(j p) m -> p j m", p=128)
            for j0 in range(0, 22, 2):
                self.wload(K.swslot(), wd[:, j0:j0 + 2, :], wdv[:, j0:j0 + 2, :], [wd.sub(j0), wd.sub(j0 + 1)])
            xt = [K.tile(st, f"xt{i}", [128, 8, NB], F32) for i in range(2)]
            sqt = [K.tile(st, f"sq{i}", [128, NB], F32) for i in range(2)]
            rstd = K.tile(st, "rstd", [128, NB], F32)
            xn = K.tile(st, "xn", [128, 8, NB], BF16)
            sg = [K.tile(st, f"sg{i}", [128, NB], BF16) for i in range(2)]
            hh = K.tile(st, "hh", [128, 22, NB], BF16)
            ss = K.psum(st, "ss")
            pg = [K.psum(st, f"pg{i}") for i in range(2)]
            pu = [K.psum(st, f"pu{i}") for i in range(2)]
            po = [K.psum(st, f"po{i}") for i in range(2)]
            srcv = src.rearrange("(c p) t -> p c t", p=128)
            dstv = self.hT.rearrange("(c p) t -> p c t", p=128)
            for b, (t0, n) in enumerate(blocks):
                K.maybe_reset()
                X = xt[b % 2]
                K.dma(K.sp, X[:, :, :n], srcv[:, :, t0:t0 + n], X, reads=[K.dres("h", b)], writes=[X])
                self.rms_rstd(None, X, n, sqt, ss, rstd, self.ones_f)
                for c in range(8):
                    K.op(K.dve, lambda c=c: nc.vector.scalar_tensor_tensor(
                        out=xn[:, c, :n], in0=X[:, c, :n], scalar=gcol[:, c:c + 1], in1=rstd[:, :n],
                        op0=ALU.mult, op1=ALU.mult), reads=[X.sub(c), gcol, rstd], writes=[xn.sub(c)])
                for j in range(22):
                    G = pg[j % 2]
                    U = pu[j % 2]
                    for k in range(8):
                        K.op(K.pe, lambda k=k, j=j, G=G: nc.tensor.matmul(
                            out=G[:, :n], lhsT=wgu[:, k, j * 128:(j + 1) * 128], rhs=xn[:, k, :n],
                            start=(k == 0), stop=(k == 7)), reads=[wgu.sub(k), xn.sub(k)], writes=[G], sig=(k == 7))
                    for k in range(8):
                        K.op(K.pe, lambda k=k, j=j, U=U: nc.tensor.matmul(
                            out=U[:, :n], lhsT=wgu[:, k, DFF + j * 128:DFF + (j + 1) * 128], rhs=xn[:, k, :n],
                            start=(k == 0), stop=(k == 7)), reads=[wgu.sub(k), xn.sub(k)], writes=[U], sig=(k == 7))
                    s = sg[j % 2]
                    K.op(K.act, lambda s=s, G=G: nc.scalar.activation(out=s[:, :n], in_=G[:, :n], func=AF.Silu),
                         reads=[G], writes=[s])
                    K.op(K.dve, lambda s=s, U=U, j=j: nc.vector.tensor_tensor(
                        out=hh[:, j, :n], in0=s[:, :n], in1=U[:, :n], op=ALU.mult),
                        reads=[s, U], writes=[hh.sub(j)])
                for m in range(8):
                    P = po[m % 2]
                    for j in range(22):
                        K.op(K.pe, lambda j=j, m=m, P=P: nc.tensor.matmul(
                            out=P[:, :n], lhsT=wd[:, j, m * 128:(m + 1) * 128], rhs=hh[:, j, :n],
                            start=(j == 0), stop=(j == 21)), reads=[wd.sub(j), hh.sub(j)], writes=[P], sig=(j == 21))
                    K.op(K.dve, lambda m=m, P=P, X=X: nc.vector.scalar_tensor_tensor(
                        out=X[:, m, :n], in0=P[:, :n], scalar=0.5, in1=X[:, m, :n],
                        op0=ALU.mult, op1=ALU.add), reads=[P, X.sub(m)], writes=[X.sub(m)])
                K.dma(K.sp, dstv[:, :, t0:t0 + n], X[:, :, :n], X, reads=[X], writes=[K.dres("h", b)])
            K.end_phase()


    def load_norm(self, X, xn, gcol, srcv, t0, n, b, sqt, ss, rstd):
        K = self.K
        nc = self.nc
        K.dma(K.sp, X[:, :, :n], srcv[:, :, t0:t0 + n], X, reads=[K.dres("h", b)], writes=[X])
        self.rms_rstd(None, X, n, sqt, ss, rstd, self.ones_f)
        for c in range(8):
            K.op(K.dve, lambda: nc.vector.scalar_tensor_tensor(
                out=xn[:, c, :n], in0=X[:, c, :n], scalar=gcol[:, c:c + 1], in1=rstd[:, :n],
                op0=ALU.mult, op1=ALU.mult), reads=[X.sub(c), gcol, rstd], writes=[xn.sub(c)])

    def rec_phase(self, src, li):
        K = self.K
        nc = self.nc
        j = li // 2
        NB = 256
        blocks = token_blocks(self.S, NB)
        with ExitStack() as st:
            self.consts(st)
            win = K.tile(st, "rwin", [128, 8, 2 * D], BF16)
            gw = K.tile(st, "rgw", [128, 16, 256], BF16)
            wout = K.tile(st, "rwout", [128, 8, D], BF16)
            gcol = self.load_cols(st, "gcol", self.norm_gc[:, (li * 3 + 1) * 8:(li * 3 + 2) * 8], 8)
            rc = self.load_cols(st, "rc", self.rec_cols[:, j * 64:(j + 1) * 64], 64)
            for k in range(8):
                self.wload(K.swslot(), win[:, k, :], self.rec_w_in[j, k * 128:(k + 1) * 128, :], [win.sub(k)])
            gwv = self.rec_gate_w[j].rearrange("g n (ic p) jj -> p (g n ic) jj", p=128)
            for q in range(2):
                self.wload(K.swslot(), gw[:, q * 8:(q + 1) * 8, :], gwv[:, q * 8:(q + 1) * 8, :], [gw.sub(q)])
            wov = self.rec_w_out[j].rearrange("(k p) m -> p k m", p=128)
            for q in range(2):
                self.wload(K.swslot(), wout[:, q * 4:(q + 1) * 4, :], wov[:, q * 4:(q + 1) * 4, :], [wout.sub(q)])
            clam = K.tile(st, "clam", [128, 8], F32)
            K.op(K.act, lambda: nc.scalar.activation(out=clam[:, :], in_=rc[:, 56:64], func=AF.Exp, scale=-1.0),
                 reads=[rc], writes=[clam])
            K.op(K.dve, lambda: nc.vector.tensor_scalar(out=clam[:, :], in0=clam[:, :], scalar1=1.0, scalar2=None,
                                                        op0=ALU.add), reads=[clam], writes=[clam])
            K.op(K.act, lambda: nc.scalar.activation(out=clam[:, :], in_=clam[:, :], func=AF.Ln),
                 reads=[clam], writes=[clam])
            K.op(K.dve, lambda: nc.vector.tensor_scalar(out=clam[:, :], in0=clam[:, :], scalar1=-8.0, scalar2=None,
                                                        op0=ALU.mult), reads=[clam], writes=[clam])
            xt = [K.tile(st, f"xt{i}", [128, 8, NB], F32) for i in range(2)]
            sqt = [K.tile(st, f"sq{i}", [128, NB], F32) for i in range(2)]
            rstd = K.tile(st, "rstd", [128, NB], F32)
            xn = K.tile(st, "xn", [128, 8, NB], BF16)
            yb = K.tile(st, "yb", [128, 8, NB], BF16)
            xb = [K.tile(st, f"xb{i}", [128, 8, 3 + NB], F32) for i in range(2)]
            xc = K.tile(st, "xc", [128, 8, NB], F32)
            xcb = K.tile(st, "xcb", [128, 8, NB], BF16)
            t1 = [K.tile(st, f"t1{i}", [128, NB], F32) for i in range(2)]
            gx = K.tile(st, "gx", [128, 8, NB], F32)
            at = K.tile(st, "at", [128, 8, NB], F32)
            ga8 = K.tile(st, "ga8", [128, 8, NB], F32)
            mu8 = K.tile(st, "mu8", [128, 8, NB], F32)
            ut = K.tile(st, "ut", [128, 8, NB], F32)
            hs = [K.tile(st, f"hs{i}", [128, 8, NB], F32) for i in range(2)]
            zb = K.tile(st, "zb", [128, 8, NB], BF16)
            ss = K.psum(st, "ss")
            py = [K.psum(st, f"py{i}") for i in range(2)]
            pgt = [K.psum(st, f"pgt{i}") for i in range(2)]
            po = [K.psum(st, f"po{i}") for i in range(2)]
            srcv = src.rearrange("(c p) t -> p c t", p=128)
            dstv = self.hT.rearrange("(c p) t -> p c t", p=128)
            K.op(K.dve, lambda: nc.vector.memset(xb[0][:, :, 0:3], 0.0), writes=[xb[0]])
            nprev = 0
            for b, (t0, n) in enumerate(blocks):
                K.maybe_reset()
                X = xt[b % 2]
                XB = xb[b % 2]
                XBn = xb[(b + 1) % 2]
                HS = hs[b % 2]
                HSp = hs[(b + 1) % 2]
                self.dve_square = True
                self.load_norm(X, xn, gcol, srcv, t0, n, b, sqt, ss, rstd)
                self.dve_square = False
                for m in range(8):
                    P = py[m % 2]
                    T1 = t1[m % 2]
                    for k in range(8):
                        K.op(K.pe, lambda: nc.tensor.matmul(out=P[:, :n], lhsT=win[:, k, m * 128:(m + 1) * 128],
                                                            rhs=xn[:, k, :n], start=(k == 0), stop=(k == 7)),
                             reads=[win.sub(k), xn.sub(k)], writes=[P], sig=(k == 7))
                    K.op(K.dve, lambda: nc.vector.tensor_scalar(out=T1[:, :n], in0=P[:, :n], scalar1=0.044715,
                                                                scalar2=None, op0=ALU.mult), reads=[P], writes=[T1])
                    K.op(K.dve, lambda: nc.vector.tensor_tensor(out=T1[:, :n], in0=T1[:, :n], in1=P[:, :n], op=ALU.mult),
                         reads=[T1, P], writes=[T1])
                    K.op(K.dve, lambda: nc.vector.scalar_tensor_tensor(out=T1[:, :n], in0=T1[:, :n], scalar=1.0, in1=P[:, :n],
                                                                       op0=ALU.add, op1=ALU.mult),
                         reads=[T1, P], writes=[T1])
                    K.op(K.act, lambda: nc.scalar.activation(out=T1[:, :n], in_=T1[:, :n], func=AF.Sigmoid,
                                                             scale=1.5957691216057308), reads=[T1], writes=[T1])
                    K.op(K.dve, lambda: nc.vector.tensor_tensor(out=yb[:, m, :n], in0=T1[:, :n], in1=P[:, :n], op=ALU.mult),
                         reads=[T1, P], writes=[yb.sub(m)])
                for m in range(8):
                    P = py[m % 2]
                    for k in range(8):
                        K.op(K.pe, lambda: nc.tensor.matmul(out=P[:, :n], lhsT=win[:, k, D + m * 128:D + (m + 1) * 128],
                                                            rhs=xn[:, k, :n], start=(k == 0), stop=(k == 7)),
                             reads=[win.sub(k), xn.sub(k)], writes=[P], sig=(k == 7))
                    K.op(K.act, lambda: nc.scalar.activation(out=XB[:, m, 3:3 + n], in_=P[:, :n], func=AF.Copy),
                         reads=[P], writes=[XB.sub(m)])
                for m in range(8):
                    K.op(K.dve, lambda: nc.vector.tensor_scalar(
                        out=xc[:, m, :n], in0=XB[:, m, 0:n], scalar1=rc[:, m:m + 1], scalar2=rc[:, 32 + m:33 + m],
                        op0=ALU.mult, op1=ALU.add), reads=[XB.sub(m), rc], writes=[xc.sub(m)])
                    for w in range(1, 4):
                        K.op(K.dve, lambda: nc.vector.scalar_tensor_tensor(
                            out=xc[:, m, :n], in0=XB[:, m, w:w + n], scalar=rc[:, w * 8 + m:w * 8 + m + 1],
                            in1=xc[:, m, :n], op0=ALU.mult, op1=ALU.add), reads=[XB.sub(m), rc, xc.sub(m)],
                            writes=[xc.sub(m)])
                    K.op(K.pool, lambda: nc.gpsimd.tensor_copy(out=xcb[:, m, :n], in_=xc[:, m, :n]),
                         reads=[xc.sub(m)], writes=[xcb.sub(m)])
                K.op(K.pool, lambda: nc.gpsimd.tensor_copy(out=XBn[:, :, 0:3], in_=XB[:, :, n:n + 3]),
                     reads=[XB], writes=[XBn])
                for oc in range(8):
                    nb_, jc = oc // 2, oc % 2
                    P0 = pgt[0]
                    P1 = pgt[1]
                    for g, P in ((0, P0), (1, P1)):
                        for ic in range(2):
                            K.op(K.pe, lambda: nc.tensor.matmul(
                                out=P[:, :n], lhsT=gw[:, (g * 4 + nb_) * 2 + ic, jc * 128:(jc + 1) * 128],
                                rhs=xcb[:, nb_ * 2 + ic, :n], start=(ic == 0), stop=(ic == 1)),
                                reads=[gw, xcb.sub(nb_ * 2 + ic)], writes=[P], sig=(ic == 1))
                    K.op(K.act, lambda: nc.scalar.activation(out=gx[:, oc, :n], in_=P0[:, :n], func=AF.Sigmoid,
                                                             bias=rc[:, 40 + oc:41 + oc]), reads=[P0, rc], writes=[gx.sub(oc)])
                    K.op(K.act, lambda: nc.scalar.activation(out=ga8[:, oc, :n], in_=P1[:, :n], func=AF.Sigmoid,
                                                             bias=rc[:, 48 + oc:49 + oc]), reads=[P1, rc], writes=[ga8.sub(oc)])
                for oc in range(8):
                    K.op(K.act, lambda: nc.scalar.activation(out=at[:, oc, :n], in_=ga8[:, oc, :n], func=AF.Exp,
                                                             scale=clam[:, oc:oc + 1]), reads=[ga8.sub(oc), clam], writes=[at.sub(oc)])
                    K.op(K.dve, lambda: nc.vector.tensor_tensor(out=mu8[:, oc, :n], in0=at[:, oc, :n], in1=at[:, oc, :n],
                                                                op=ALU.mult), reads=[at.sub(oc)], writes=[mu8.sub(oc)])
                    K.op(K.dve, lambda: nc.vector.tensor_tensor(out=ut[:, oc, :n], in0=gx[:, oc, :n], in1=xc[:, oc, :n],
                                                                op=ALU.mult), reads=[gx.sub(oc), xc.sub(oc)], writes=[ut.sub(oc)])
                for oc in range(8):
                    K.op(K.act, lambda: nc.scalar.activation(out=mu8[:, oc, :n], in_=mu8[:, oc, :n], func=AF.Sqrt,
                                                             bias=self.one_t[:, 0:1], scale=-1.0),
                         reads=[mu8.sub(oc), self.one_t], writes=[mu8.sub(oc)])
                    K.op(K.dve, lambda: nc.vector.tensor_tensor(out=ut[:, oc, :n], in0=ut[:, oc, :n], in1=mu8[:, oc, :n],
                                                                op=ALU.mult), reads=[ut.sub(oc), mu8.sub(oc)], writes=[ut.sub(oc)])
                    init = 0.0 if b == 0 else HSp[:, oc, nprev - 1:nprev]
                    K.op(K.dve, lambda: nc.vector.tensor_tensor_scan(
                        out=HS[:, oc, :n], data0=at[:, oc, :n], data1=ut[:, oc, :n], initial=init,
                        op0=ALU.mult, op1=ALU.add), reads=[at.sub(oc), ut.sub(oc), HSp.sub(oc)], writes=[HS.sub(oc)])
                    K.op(K.dve, lambda: nc.vector.tensor_tensor(out=zb[:, oc, :n], in0=HS[:, oc, :n], in1=yb[:, oc, :n],
                                                                op=ALU.mult), reads=[HS.sub(oc), yb.sub(oc)], writes=[zb.sub(oc)])
                for m in range(8):
                    P = po[m % 2]
                    for k in range(8):
                        K.op(K.pe, lambda: nc.tensor.matmul(out=P[:, :n], lhsT=wout[:, k, m * 128:(m + 1) * 128],
                                                            rhs=zb[:, k, :n], start=(k == 0), stop=(k == 7)),
                             reads=[wout, zb.sub(k)], writes=[P], sig=(k == 7))
                    K.op(K.dve, lambda: nc.vector.tensor_tensor(out=X[:, m, :n], in0=P[:, :n], in1=X[:, m, :n], op=ALU.add),
                         reads=[P, X.sub(m)], writes=[X.sub(m)])
                K.dma(K.sp, dstv[:, :, t0:t0 + n], X[:, :, :n], X, reads=[X], writes=[K.dres("h", b)])
                nprev = n
            K.end_phase()

    def attn_proj_phase(self, src, li):
        K = self.K
        nc = self.nc
        j = li // 2
        NB = 256
        blocks = token_blocks(self.S, NB)
        with ExitStack() as st:
            self.consts(st)
            win = K.tile(st, "awin", [128, 8, ATTN_IN], BF16)
            gcol = self.load_cols(st, "gcol", self.norm_gc[:, (li * 3 + 1) * 8:(li * 3 + 2) * 8], 8)
            lnc = self.load_cols(st, "lnc", self.idx_lnc[:, j * 2:j * 2 + 2], 2, parts=64)
            for k in range(8):
                self.wload(K.swslot(), win[:, k, :], self.attn_w_in[j, k * 128:(k + 1) * 128, :], [win.sub(k)])
            xt = [K.tile(st, f"xt{i}", [128, 8, NB], F32) for i in range(2)]
            sqt = [K.tile(st, f"sq{i}", [128, NB], F32) for i in range(2)]
            rstd = K.tile(st, "rstd", [128, NB], F32)
            xn = K.tile(st, "xn", [128, 8, NB], BF16)
            fo = [K.tile(st, f"fo{i}", [128, 4, NB], BF16) for i in range(2)]
            kf = K.tile(st, "kf", [64, NB], F32)
            kx = K.tile(st, "kx", [64, NB], F32)
            ksq = K.tile(st, "ksq", [64, NB], F32)
            krs = K.tile(st, "krs", [64, NB], F32)
            ko = [K.tile(st, f"ko{i}", [64, NB], BF16) for i in range(2)]
            vo = [K.tile(st, f"vo{i}", [128, 512], BF16) for i in range(4)]
            wo = [K.tile(st, f"wo{i}", [128, 8], F32) for i in range(2)]
            ss = K.psum(st, "ss")
            pf = [K.psum(st, f"pf{i}") for i in range(2)]
            pk = K.psum(st, "pk")
            pk2 = K.psum(st, "pk2")
            pt = [K.psum(st, f"pt{i}") for i in range(2)]
            srcv = src.rearrange("(c p) t -> p c t", p=128)
            groups = [(self.qaT, 0), (self.kaT, 512), (self.qiT, 1536), (self.qbT, 2120), (self.kbT, 2632)]
            gi = 0
            vi = 0
            wi_i = 0
            for b, (t0, n) in enumerate(blocks):
                K.maybe_reset()
                X = xt[b % 2]
                self.load_norm(X, xn, gcol, srcv, t0, n, b, sqt, ss, rstd)
                for (dst, c0) in groups:
                    FO = fo[gi % 2]
                    gi += 1
                    for m in range(4):
                        P = pf[m % 2]
                        for k in range(8):
                            K.mm(P[:, :n], win[:, k, c0 + m * 128:c0 + (m + 1) * 128], xn[:, k, :n], k == 0, k == 7,
                                 [win.sub(k), xn.sub(k)], [P])
                        if m % 2 == 0:
                            K.op(K.act, lambda: nc.scalar.activation(out=FO[:, m, :n], in_=P[:, :n], func=AF.Copy),
                                 reads=[P], writes=[FO.sub(m)])
                        else:
                            K.op(K.dve, lambda: nc.vector.tensor_copy(out=FO[:, m, :n], in_=P[:, :n]),
                                 reads=[P], writes=[FO.sub(m)])
                    K.dma(K.sp, dst.rearrange("(m p) t -> p m t", p=128)[:, :, t0:t0 + n], FO[:, :, :n], FO, reads=[FO])
                for k in range(8):
                    K.mm(pk[:64, :n], win[:, k, 2048:2112], xn[:, k, :n], k == 0, k == 7, [win.sub(k), xn.sub(k)], [pk])
                K.op(K.act, lambda: nc.scalar.activation(out=kf[:, :n], in_=pk[:64, :n], func=AF.Copy), reads=[pk], writes=[kf])
                K.mm(pk2[:64, :n], self.ones_f[:64, :64], kf[:, :n], True, True, [self.ones_f, kf], [pk2])
                K.op(K.dve, lambda: nc.vector.scalar_tensor_tensor(out=kx[:, :n], in0=pk2[:64, :n], scalar=-1.0 / 64,
                                                                   in1=kf[:, :n], op0=ALU.mult, op1=ALU.add),
                     reads=[pk2, kf], writes=[kx])
                K.op(K.act, lambda: nc.scalar.activation(out=ksq[:, :n], in_=kx[:, :n], func=AF.Square), reads=[kx], writes=[ksq])
                K.mm(pk2[:64, :n], self.ones_f[:64, :64], ksq[:, :n], True, True, [self.ones_f, ksq], [pk2])
                K.op(K.act, lambda: nc.scalar.activation(out=krs[:, :n], in_=pk2[:64, :n], func=AF.Sqrt,
                                                         bias=self.eps_t[:64, 0:1], scale=1.0 / 64),
                     reads=[pk2, self.eps_t], writes=[krs])
                K.op(K.dve, lambda: nc.vector.reciprocal(out=krs[:, :n], in_=krs[:, :n]), reads=[krs], writes=[krs])
                K.op(K.dve, lambda: nc.vector.tensor_tensor(out=kx[:, :n], in0=kx[:, :n], in1=krs[:, :n], op=ALU.mult),
                     reads=[kx, krs], writes=[kx])
                KO = ko[b % 2]
                K.op(K.dve, lambda: nc.vector.tensor_scalar(out=KO[:, :n], in0=kx[:, :n], scalar1=lnc[:, 0:1],
                                                            scalar2=lnc[:, 1:2], op0=ALU.mult, op1=ALU.add),
                     reads=[kx, lnc], writes=[KO])
                K.dma(K.sp, self.kiT[:, t0:t0 + n], KO[:, :n], KO, reads=[KO])
                for c_lo in range(0, n, 128):
                    tn = min(128, n - c_lo)
                    for (dst, c0) in ((self.va, 1024), (self.vb, 3144)):
                        P = pt[vi % 2]
                        VO = vo[vi % 4]
                        vi += 1
                        for k in range(8):
                            K.mm(P[:tn, :512], xn[:, k, c_lo:c_lo + tn], win[:, k, c0:c0 + 512], k == 0, k == 7,
                                 [win.sub(k), xn.sub(k)], [P])
                        if vi % 2 == 0:
                            K.op(K.act, lambda: nc.scalar.activation(out=VO[:tn, :], in_=P[:tn, :512], func=AF.Copy),
                                 reads=[P], writes=[VO])
                        else:
                            K.op(K.dve, lambda: nc.vector.tensor_copy(out=VO[:tn, :], in_=P[:tn, :512]), reads=[P], writes=[VO])
                        K.dma(K.sp, dst[t0 + c_lo:t0 + c_lo + tn, :], VO[:tn, :], VO, reads=[VO])
                    WO = wo[wi_i % 2]
                    wi_i += 1
                    for k in range(8):
                        K.mm(pk2[:tn, :8], xn[:, k, c_lo:c_lo + tn], win[:, k, 2112:2120], k == 0, k == 7,
                             [win.sub(k), xn.sub(k)], [pk2])
                    K.op(K.dve, lambda: nc.vector.tensor_scalar(out=WO[:tn, :], in0=pk2[:tn, :8], scalar1=0.044194173824159216,
                                                                scalar2=None, op0=ALU.mult), reads=[pk2], writes=[WO])
                    K.dma(K.sp, self.wi[t0 + c_lo:t0 + c_lo + tn, :], WO[:tn, :], WO, reads=[WO])
            K.end_phase()

    def dsa_phase(self, li):
        K = self.K
        nc = self.nc
        S = self.S
        TOPK = float(self.TOPK)
        NKT = S // 128
        NQB = S // 512
        NIT = 18
        with ExitStack() as st:
            self.consts(st)
            ident = K.tile(st, "ident", [128, 128], BF16)
            K.dma(K.sp, ident[:, :], self.c_ident_bf[:, :], ident, writes=[ident])
            negm = K.tile(st, "negm", [128, 128], F32)
            K.dma(K.sp, negm[:, :], self.c_negmask[:, :], negm, writes=[negm])
            cmeta = K.tile(st, "cmeta", [16, 16], BF16)
            K.dma(K.sp, cmeta[:, :], self.c_cmeta[:, :], cmeta, writes=[cmeta])
            ki_sb = K.tile(st, "ki_sb", [64, S], BF16)
            K.dma(K.sp, ki_sb[:, :], self.kiT[:, NMETA:], ki_sb, writes=[ki_sb])
            va_sb = K.tile(st, "va_sb", [128, NKT, 512], BF16)
            vav = self.va[NMETA:, :].rearrange("(kt p) c -> p kt c", p=128)
            for g0 in range(0, NKT, 8):
                g1 = min(NKT, g0 + 8)
                K.dma(K.sp, va_sb[:, g0:g1, :], vav[:, g0:g1, :], va_sb.slot(g0), writes=[va_sb.sub(g0 // 8)])
            vmeta = K.tile(st, "vmeta", [16, 512], BF16)
            K.dma(K.sp, vmeta[:, :], self.va[0:NMETA, :], vmeta, writes=[vmeta])
            maskT = [K.tile(st, f"maskT{i}", [128, NKT, 512], BF16) for i in range(2)]
            score = K.tile(st, "score", [128, S], F32)
            mask01 = K.tile(st, "mask01", [128, S], BF16)
            rl = [K.tile(st, f"rl{i}", [128, 512], F32) for i in range(2)]
            qi_sb = [K.tile(st, f"qi{i}", [64, 8, 128], BF16) for i in range(2)]
            wi_sb = [K.tile(st, f"wi{i}", [128, 8], F32) for i in range(2)]
            smax = K.tile(st, "smax", [128, 1], F32)
            lo = K.tile(st, "lo", [128, 1], F32)
            w0 = K.tile(st, "w0", [128, 1], F32)
            mid = K.tile(st, "mid", [128, 1], F32)
            cnt = K.tile(st, "cnt", [128, 1], F32)
            gg = K.tile(st, "gg", [128, 1], F32)
            qa_sb = [K.tile(st, f"qa{i}", [64, 512], BF16) for i in range(2)]
            ka_sb = [K.tile(st, f"ka{i}", [64, NMETA + S], BF16) for i in range(2)]
            et = [K.tile(st, f"et{i}", [128, 512], BF16) for i in range(2)]
            ptl = [K.tile(st, f"ptl{i}", [128, 512], BF16) for i in range(2)]
            rs = K.tile(st, "rs", [64, 512], F32)
            oh = [K.tile(st, f"oh{i}", [64, 512], BF16) for i in range(2)]
            pl = [K.psum(st, f"pl{i}") for i in range(2)]
            ptr = K.psum(st, "ptr", (128, 4, 128), BF16)
            pst = [K.psum(st, f"pst{i}") for i in range(2)]
            po = K.psum(st, "po")
            ps = K.psum(st, "ps")
            ei = [0]

            po_sb = K.tile(st, "po_sb", [64, 512], F32)
            ps_sb = K.tile(st, "ps_sb", [64, 512], F32)

            def attend(QA, KA, h, nq, key_tiles, tq):
                nt = len(key_tiles)
                base = ei[0]
                ei[0] += nt

                def emit_score(i):
                    k0, kn = key_tiles[i][0], key_tiles[i][1]
                    PST = pst[(base + i) % 2]
                    K.mm(PST[:kn, :nq], KA[:, k0:k0 + kn], QA[:, :nq], True, True, [KA, QA], [PST])

                emit_score(0)
                if nt > 1:
                    emit_score(1)
                for i, (k0, kn, v_ap, v_res, m_ap, m_res) in enumerate(key_tiles):
                    PST = pst[(base + i) % 2]
                    E_ = et[(base + i) % 2]
                    P_ = ptl[(base + i) % 2]
                    K.op(K.act, lambda: nc.scalar.activation(out=E_[:kn, :nq], in_=PST[:kn, :nq], func=AF.Exp, scale=0.125),
                         reads=[PST], writes=[E_])
                    if m_ap is not None:
                        K.op(K.pool, lambda: nc.gpsimd.tensor_tensor(out=P_[:kn, :nq], in0=E_[:kn, :nq], in1=m_ap, op=ALU.mult),
                             reads=[E_, m_res], writes=[P_])
                        R_ = P_
                    else:
                        R_ = E_
                    if i + 2 < nt:
                        emit_score(i + 2)
                    K.mm(po[:64, :nq], v_ap, R_[:kn, :nq], i == 0, i == nt - 1, [v_res, R_], [po], sig=True)
                    K.mm(ps[:64, :nq], self.ones_b[:kn, :64], R_[:kn, :nq], i == 0, i == nt - 1, [self.ones_b, R_], [ps], sig=True)
                    yield
                OH = oh[h % 2]
                K.op(K.act, lambda: nc.scalar.activation(out=ps_sb[:, :nq], in_=ps[:64, :nq], func=AF.Copy), reads=[ps], writes=[ps_sb])
                K.op(K.act, lambda: nc.scalar.activation(out=po_sb[:, :nq], in_=po[:64, :nq], func=AF.Copy), reads=[po], writes=[po_sb])
                K.op(K.dve, lambda: nc.vector.reciprocal(out=rs[:, :nq], in_=ps_sb[:, :nq]), reads=[ps_sb], writes=[rs])
                K.op(K.dve, lambda: nc.vector.tensor_tensor(out=OH[:, :nq], in0=po_sb[:, :nq], in1=rs[:, :nq], op=ALU.mult),
                     reads=[po_sb, rs], writes=[OH])
                K.dma(K.sp, self.mixA[h * 64:(h + 1) * 64, tq:tq + nq], OH[:, :nq], OH, reads=[OH])
                yield

            for h in range(8):
                QA = qa_sb[h % 2]
                KA = ka_sb[h % 2]
                K.dma(K.sp, QA[:, :NMETA], self.qaT[h * 64:(h + 1) * 64, 0:NMETA], QA, writes=[QA])
                K.dma(K.sp, KA[:, :NMETA], self.kaT[h * 64:(h + 1) * 64, 0:NMETA], KA, writes=[KA])
                for _ in attend(QA, KA, h, NMETA, [(0, NMETA, vmeta[:NMETA, h * 64:(h + 1) * 64], vmeta, cmeta[:, :], cmeta)], 0):
                    pass

            def gen_index(qb):
                nkt = 4 * (qb + 1)
                MT = maskT[qb % 2]
                K.op(K.dve, lambda: nc.vector.memset(MT[:, 4 * qb:4 * qb + 4, :], 0.0), writes=[MT])
                for jq in range(4):
                    K.maybe_reset()
                    qt = 4 * qb + jq
                    nk = (qt + 1) * 128
                    tq0 = NMETA + qt * 128
                    QI = qi_sb[jq % 2]
                    WI = wi_sb[jq % 2]
                    K.dma(K.sp, QI[:, :, :], self.qiT[:, tq0:tq0 + 128].rearrange("(h d) q -> d h q", d=64), QI, writes=[QI])
                    K.dma(K.sp, WI[:, :], self.wi[tq0:tq0 + 128, :], WI, writes=[WI])
                    nch = (nk + 511) // 512
                    ri = 0
                    for h in range(8):
                        for ch in range(nch):
                            c0 = ch * 512
                            w = min(512, nk - c0)
                            PL = pl[ri % 2]
                            RL = rl[ri % 2]
                            ri += 1
                            K.mm(PL[:, :w], QI[:, h, :], ki_sb[:, c0:c0 + w], True, True, [QI, ki_sb], [PL])
                            K.op(K.act, lambda: nc.scalar.activation(out=RL[:, :w], in_=PL[:, :w], func=AF.Relu),
                                 reads=[PL], writes=[RL])
                            if h == 0:
                                K.op(K.dve, lambda: nc.vector.tensor_scalar(out=score[:, c0:c0 + w], in0=RL[:, :w],
                                                                            scalar1=WI[:, 0:1], scalar2=None, op0=ALU.mult),
                                     reads=[RL, WI], writes=[score.sub(ch)])
                            else:
                                K.op(K.dve, lambda: nc.vector.scalar_tensor_tensor(
                                    out=score[:, c0:c0 + w], in0=RL[:, :w], scalar=WI[:, h:h + 1], in1=score[:, c0:c0 + w],
                                    op0=ALU.mult, op1=ALU.add), reads=[RL, WI, score.sub(ch)], writes=[score.sub(ch)])
                            yield
                    K.op(K.dve, lambda: nc.vector.tensor_reduce(out=smax[:, :], in_=score[:, :nk], axis=AX.X, op=ALU.max),
                         reads=[score], writes=[smax])
                    K.op(K.dve, lambda: nc.vector.tensor_reduce(out=lo[:, :], in_=score[:, :nk], axis=AX.X, op=ALU.min),
                         reads=[score], writes=[lo])
                    K.op(K.dve, lambda: nc.vector.tensor_tensor(out=score[:, qt * 128:(qt + 1) * 128],
                                                                in0=score[:, qt * 128:(qt + 1) * 128], in1=negm[:, :], op=ALU.add),
                         reads=[score, negm], writes=[score])
                    K.op(K.dve, lambda: nc.vector.tensor_tensor(out=w0[:, :], in0=smax[:, :], in1=lo[:, :], op=ALU.subtract),
                         reads=[smax, lo], writes=[w0])
                    for it in range(NIT):
                        c = 2.0 ** -(it + 1)
                        K.op(K.dve, lambda: nc.vector.scalar_tensor_tensor(out=mid[:, :], in0=w0[:, :], scalar=c, in1=lo[:, :],
                                                                           op0=ALU.mult, op1=ALU.add),
                             reads=[w0, lo], writes=[mid])
                        K.op(K.dve, lambda: nc.vector.tensor_scalar(out=mask01[:, :nk], in0=score[:, :nk], scalar1=mid[:, 0:1],
                                                                    scalar2=None, op0=ALU.is_ge, op1=ALU.add,
                                                                    accum_out=cnt[:, 0:1]),
                             reads=[score, mid], writes=[mask01, cnt])
                        K.op(K.dve, lambda: nc.vector.tensor_scalar(out=gg[:, :], in0=cnt[:, :], scalar1=TOPK, scalar2=c,
                                                                    op0=ALU.is_ge, op1=ALU.mult), reads=[cnt], writes=[gg])
                        K.op(K.dve, lambda: nc.vector.scalar_tensor_tensor(out=lo[:, :], in0=gg[:, :], scalar=w0[:, 0:1],
                                                                           in1=lo[:, :], op0=ALU.mult, op1=ALU.add),
                             reads=[gg, w0, lo], writes=[lo])
                        for _w in range(max(1, nk // 768)):
                            yield
                    K.op(K.dve, lambda: nc.vector.tensor_scalar(out=mask01[:, :nk], in0=score[:, :nk], scalar1=lo[:, 0:1],
                                                                scalar2=None, op0=ALU.is_ge), reads=[score, lo], writes=[mask01])
                    for kt0 in range(0, qt + 1, 4):
                        g_ = min(4, qt + 1 - kt0)
                        for i in range(g_):
                            K.op(K.pe, lambda: nc.tensor.transpose(out=ptr[:, i, :], in_=mask01[:, (kt0 + i) * 128:(kt0 + i + 1) * 128],
                                                                   identity=ident[:, :]),
                                 reads=[mask01, ident], writes=[ptr], sig=True)
                        K.op(K.act, lambda: nc.scalar.activation(out=MT[:, kt0:kt0 + g_, jq * 128:(jq + 1) * 128],
                                                                 in_=ptr[:, 0:g_, :], func=AF.Copy), reads=[ptr], writes=[MT])
                        yield
            def gen_attend(qb):
                nkt = 4 * (qb + 1)
                MT = maskT[qb % 2]
                tq = NMETA + qb * 512

                def load_head(h):
                    QA = qa_sb[h % 2]
                    KA = ka_sb[h % 2]
                    K.dma(K.sp, QA[:, :], self.qaT[h * 64:(h + 1) * 64, tq:tq + 512], QA, writes=[QA])
                    K.dma(K.sp, KA[:, :NMETA + nkt * 128], self.kaT[h * 64:(h + 1) * 64, 0:NMETA + nkt * 128], KA, writes=[KA])
                    return QA, KA

                nxt = load_head(0)
                for h in range(8):
                    K.maybe_reset()
                    QA, KA = nxt
                    if h + 1 < 8:
                        nxt = load_head(h + 1)
                    tiles = [(0, NMETA, vmeta[:NMETA, h * 64:(h + 1) * 64], vmeta, None, None)]
                    for kt in range(nkt):
                        tiles.append((NMETA + kt * 128, 128, va_sb[:, kt, h * 64:(h + 1) * 64], va_sb.sub(kt // 8),
                                      MT[:, kt, :], MT))
                    yield from attend(QA, KA, h, 512, tiles, tq)

            def n_index(qb):
                tot = 0
                for jq in range(4):
                    nk = (4 * qb + jq + 1) * 128
                    tot += 8 * ((nk + 511) // 512) + NIT * max(1, nk // 768) + (4 * qb + jq + 4) // 4
                return tot

            for _ in gen_index(0):
                pass
            for qb in range(NQB):
                ga_ = gen_attend(qb)
                gi_ = gen_index(qb + 1) if qb + 1 < NQB else None
                na = 8 * (4 * (qb + 1) + 2)
                ni = n_index(qb + 1) if gi_ is not None else 0
                acc = 0.0
                alive_i = gi_ is not None
                for _ in ga_:
                    if alive_i:
                        acc += ni / float(na)
                        while acc >= 1.0 and alive_i:
                            acc -= 1.0
                            try:
                                next(gi_)
                            except StopIteration:
                                alive_i = False
                if alive_i:
                    for _ in gi_:
                        pass
            K.end_phase()

    def diff_phase(self, li):
        K = self.K
        nc = self.nc
        S = self.S
        j = li // 2
        lam_init = 0.8 - 0.6 * math.exp(-0.3 * li)
        NKT = S // 128
        NQB = S // 512
        with ExitStack() as st:
            self.consts(st)
            cmT = K.tile(st, "cmT", [128, 4, 512], BF16)
            K.dma(K.sp, cmT[:, :, :], self.c_cmaskT.rearrange("p (j q) -> p j q", q=512), cmT, writes=[cmT])
            cmeta = K.tile(st, "cmeta", [16, 16], BF16)
            K.dma(K.sp, cmeta[:, :], self.c_cmeta[:, :], cmeta, writes=[cmeta])
            dl = self.load_cols(st, "dl", self.dlam_bc[:, j * 256:(j + 1) * 256], 256)
            sgc2 = self.load_cols(st, "sgc", self.subln_c[:, :], 2)
            pr = K.tile(st, "pr", [128, 64], F32)
            s12 = K.tile(st, "s12", [128, 2], F32)
            neglam = K.tile(st, "neglam", [128, 1], F32)
            sgs = K.tile(st, "sgs", [128, 1], F32)
            for r in range(2):
                K.op(K.dve, lambda: nc.vector.tensor_tensor(out=pr[:, :], in0=dl[:, r * 128:r * 128 + 64],
                                                            in1=dl[:, r * 128 + 64:r * 128 + 128], op=ALU.mult),
                     reads=[dl], writes=[pr])
                K.op(K.dve, lambda: nc.vector.tensor_reduce(out=s12[:, r:r + 1], in_=pr[:, :], axis=AX.X, op=ALU.add),
                     reads=[pr], writes=[s12])
            K.op(K.act, lambda: nc.scalar.activation(out=s12[:, :], in_=s12[:, :], func=AF.Exp), reads=[s12], writes=[s12])
            K.op(K.dve, lambda: nc.vector.tensor_tensor(out=neglam[:, :], in0=s12[:, 1:2], in1=s12[:, 0:1], op=ALU.subtract),
                 reads=[s12], writes=[neglam])
            K.op(K.dve, lambda: nc.vector.tensor_scalar(out=neglam[:, :], in0=neglam[:, :], scalar1=-lam_init, scalar2=None,
                                                        op0=ALU.add), reads=[neglam], writes=[neglam])
            K.op(K.dve, lambda: nc.vector.tensor_scalar(out=sgs[:, :], in0=sgc2[:, j:j + 1], scalar1=1.0 - lam_init, scalar2=None,
                                                        op0=ALU.mult), reads=[sgc2], writes=[sgs])
            vb_sb = K.tile(st, "vb_sb", [128, NKT, 512], BF16)
            vbv = self.vb[NMETA:, :].rearrange("(kt p) c -> p kt c", p=128)
            for g0 in range(0, NKT, 8):
                g1 = min(NKT, g0 + 8)
                K.dma(K.sp, vb_sb[:, g0:g1, :], vbv[:, g0:g1, :], vb_sb.slot(g0), writes=[vb_sb.sub(g0 // 8)])
            vmeta = K.tile(st, "vbmeta", [16, 512], BF16)
            K.dma(K.sp, vmeta[:, :], self.vb[0:NMETA, :], vmeta, writes=[vmeta])
            qb_sb = [K.tile(st, f"qb{i}", [64, 2, 512], BF16) for i in range(2)]
            kb_sb = [K.tile(st, f"kb{i}", [64, 2, NMETA + S], BF16) for i in range(2)]
            et = [K.tile(st, f"et{i}", [128, 512], BF16) for i in range(4)]
            r1 = K.tile(st, "r1", [128, 512], F32)
            a1 = K.tile(st, "a1", [128, 512], F32)
            a2 = K.tile(st, "a2", [128, 512], F32)
            sq = K.tile(st, "sqd", [128, 512], F32)
            yt = [K.tile(st, f"yt{i}", [128, 512], BF16) for i in range(2)]
            pst = [K.psum(st, f"pst{i}") for i in range(2)]
            oc_ = [K.psum(st, f"o{i}") for i in range(2)]
            sc_ = [K.psum(st, f"s{i}") for i in range(2)]
            pss = K.psum(st, "pss")
            ei = [0]

            def attend(QB, KB, h, nq, key_tiles, tq):
                steps = [(i, c) for i in range(len(key_tiles)) for c in range(2)]
                nt = len(key_tiles)
                ns = len(steps)
                base = ei[0]
                ei[0] += ns

                def emit_score(si):
                    i, c = steps[si]
                    k0, kn = key_tiles[i][0], key_tiles[i][1]
                    PST = pst[(base + si) % 2]
                    K.mm(PST[:kn, :nq], KB[:, c, k0:k0 + kn], QB[:, c, :nq], True, True, [KB, QB], [PST])

                emit_score(0)
                emit_score(1)
                for si, (i, c) in enumerate(steps):
                    (k0, kn, v_ap, v_res, m_ap, m_res) = key_tiles[i]
                    PST = pst[(base + si) % 2]
                    E_ = et[(base + si) % 4]
                    K.op(K.act, lambda: nc.scalar.activation(out=E_[:kn, :nq], in_=PST[:kn, :nq], func=AF.Exp, scale=0.125),
                         reads=[PST], writes=[E_])
                    if m_ap is not None:
                        K.op(K.dve, lambda: nc.vector.tensor_tensor(out=E_[:kn, :nq], in0=E_[:kn, :nq], in1=m_ap, op=ALU.mult),
                             reads=[E_, m_res], writes=[E_])
                    if si + 2 < ns:
                        emit_score(si + 2)
                    K.mm(oc_[c][:, :nq], v_ap, E_[:kn, :nq], i == 0, i == nt - 1, [v_res, E_], [oc_[c]], sig=True)
                    K.mm(sc_[c][:, :nq], self.ones_b[:kn, :], E_[:kn, :nq], i == 0, i == nt - 1, [self.ones_b, E_], [sc_[c]], sig=True)
                Y = yt[h % 2]
                K.op(K.dve, lambda: nc.vector.reciprocal(out=r1[:, :nq], in_=sc_[0][:, :nq]), reads=[sc_[0]], writes=[r1])
                K.op(K.dve, lambda: nc.vector.tensor_tensor(out=a1[:, :nq], in0=oc_[0][:, :nq], in1=r1[:, :nq], op=ALU.mult),
                     reads=[oc_[0], r1], writes=[a1])
                K.op(K.dve, lambda: nc.vector.reciprocal(out=r1[:, :nq], in_=sc_[1][:, :nq]), reads=[sc_[1]], writes=[r1])
                K.op(K.dve, lambda: nc.vector.tensor_tensor(out=a2[:, :nq], in0=oc_[1][:, :nq], in1=r1[:, :nq], op=ALU.mult),
                     reads=[oc_[1], r1], writes=[a2])
                K.op(K.dve, lambda: nc.vector.scalar_tensor_tensor(out=a1[:, :nq], in0=a2[:, :nq], scalar=neglam[:, 0:1],
                                                                   in1=a1[:, :nq], op0=ALU.mult, op1=ALU.add),
                     reads=[a2, neglam, a1], writes=[a1])
                K.op(K.act, lambda: nc.scalar.activation(out=sq[:, :nq], in_=a1[:, :nq], func=AF.Square), reads=[a1], writes=[sq])
                for c0 in range(0, nq, 256):
                    c1 = min(nq, c0 + 256)
                    K.op(K.pe, lambda: nc.tensor.matmul(out=pss[:, c0:c1], lhsT=self.ones_f[:, :], rhs=sq[:, c0:c1],
                                                        start=True, stop=True), reads=[self.ones_f, sq], writes=[pss])
                K.op(K.act, lambda: nc.scalar.activation(out=r1[:, :nq], in_=pss[:, :nq], func=AF.Sqrt,
                                                         bias=self.eps_t[:, 0:1], scale=1.0 / 128),
                     reads=[pss, self.eps_t], writes=[r1])
                K.op(K.dve, lambda: nc.vector.reciprocal(out=r1[:, :nq], in_=r1[:, :nq]), reads=[r1], writes=[r1])
                K.op(K.dve, lambda: nc.vector.scalar_tensor_tensor(out=Y[:, :nq], in0=a1[:, :nq], scalar=sgs[:, 0:1],
                                                                   in1=r1[:, :nq], op0=ALU.mult, op1=ALU.mult),
                     reads=[a1, sgs, r1], writes=[Y])
                K.dma(K.sp, self.mixB[h * 128:(h + 1) * 128, tq:tq + nq], Y[:, :nq], Y, reads=[Y])

            def load_qk(h, tq, nq, nkeys):
                QB = qb_sb[h % 2]
                KB = kb_sb[h % 2]
                K.dma(K.sp, QB[:, :, :nq], self.qbT[h * 128:(h + 1) * 128, tq:tq + nq].rearrange("(c d) q -> d c q", d=64),
                      QB, writes=[QB])
                K.dma(K.sp, KB[:, :, :nkeys], self.kbT[h * 128:(h + 1) * 128, 0:nkeys].rearrange("(c d) q -> d c q", d=64),
                      KB, writes=[KB])
                return QB, KB

            for h in range(4):
                QB, KB = load_qk(h, 0, NMETA, NMETA)
                attend(QB, KB, h, NMETA, [(0, NMETA, vmeta[:NMETA, h * 128:(h + 1) * 128], vmeta, cmeta[:, :], cmeta)], 0)
            for qb in range(NQB):
                nkt = 4 * (qb + 1)
                tq = NMETA + qb * 512
                nxt = load_qk(0, tq, 512, NMETA + nkt * 128)
                for h in range(4):
                    K.maybe_reset()
                    QB, KB = nxt
                    if h + 1 < 4:
                        nxt = load_qk(h + 1, tq, 512, NMETA + nkt * 128)
                    tiles = [(0, NMETA, vmeta[:NMETA, h * 128:(h + 1) * 128], vmeta, None, None)]
                    for kt in range(nkt):
                        dj = kt - 4 * qb
                        tiles.append((NMETA + kt * 128, 128, vb_sb[:, kt, h * 128:(h + 1) * 128], vb_sb.sub(kt // 8),
                                      cmT[:, dj, :] if dj >= 0 else None, cmT if dj >= 0 else None))
                    attend(QB, KB, h, 512, tiles, tq)
            K.end_phase()

    def attn_out_phase(self, src, li):
        K = self.K
        nc = self.nc
        j = li // 2
        NB = 256
        blocks = token_blocks(self.S, NB)
        with ExitStack() as st:
            woA = K.tile(st, "woA", [64, 8, D], BF16)
            woB = K.tile(st, "woB", [128, 4, D], BF16)
            wav = self.attn_w_out[j, 0:512, :].rearrange("(h d) m -> d h m", d=64)
            wbv = self.attn_w_out[j, 512:1024, :].rearrange("(h p) m -> p h m", p=128)
            for q in range(2):
                self.wload(K.swslot(), woA[:, q * 4:(q + 1) * 4, :], wav[:, q * 4:(q + 1) * 4, :], [woA.sub(q)])
                self.wload(K.swslot(), woB[:, q * 2:(q + 1) * 2, :], wbv[:, q * 2:(q + 1) * 2, :], [woB.sub(q)])
            xt = [K.tile(st, f"xt{i}", [128, 8, NB], F32) for i in range(2)]
            mA = [K.tile(st, f"mA{i}", [64, 8, NB], BF16) for i in range(2)]
            mB = [K.tile(st, f"mB{i}", [128, 4, NB], BF16) for i in range(2)]
            po = [K.psum(st, f"po{i}") for i in range(2)]
            srcv = src.rearrange("(c p) t -> p c t", p=128)
            dstv = self.hT.rearrange("(c p) t -> p c t", p=128)
            for b, (t0, n) in enumerate(blocks):
                K.maybe_reset()
                X = xt[b % 2]
                A = mA[b % 2]
                B_ = mB[b % 2]
                K.dma(K.sp, X[:, :, :n], srcv[:, :, t0:t0 + n], X, reads=[K.dres("h", b)], writes=[X])
                K.dma(K.sp, A[:, :, :n], self.mixA[:, t0:t0 + n].rearrange("(h d) t -> d h t", d=64), A, writes=[A])
                K.dma(K.sp, B_[:, :, :n], self.mixB[:, t0:t0 + n].rearrange("(h p) t -> p h t", p=128), B_, writes=[B_])
                for m in range(8):
                    P = po[m % 2]
                    for h in range(8):
                        K.mm(P[:, :n], woA[:, h, m * 128:(m + 1) * 128], A[:, h, :n], h == 0, False, [woA, A], [P])
                    for hb in range(4):
                        K.mm(P[:, :n], woB[:, hb, m * 128:(m + 1) * 128], B_[:, hb, :n], False, hb == 3, [woB, B_], [P])
                    K.op(K.dve, lambda: nc.vector.tensor_tensor(out=X[:, m, :n], in0=P[:, :n], in1=X[:, m, :n], op=ALU.add),
                         reads=[P, X.sub(m)], writes=[X.sub(m)])
                K.dma(K.sp, dstv[:, :, t0:t0 + n], X[:, :, :n], X, reads=[X], writes=[K.dres("h", b)])
            K.end_phase()

    def copy_phase(self, src):
        K = self.K
        NB = 256
        blocks = token_blocks(self.S, NB)
        with ExitStack() as st:
            xt = [K.tile(st, f"xt{i}", [128, 8, NB], F32) for i in range(2)]
            srcv = src.rearrange("(c p) t -> p c t", p=128)
            dstv = self.hT.rearrange("(c p) t -> p c t", p=128)
            for b, (t0, n) in enumerate(blocks):
                K.maybe_reset()
                X = xt[b % 2]
                K.dma(K.sp, X[:, :, :n], srcv[:, :, t0:t0 + n], X, writes=[X])
                K.dma(K.sp, dstv[:, :, t0:t0 + n], X[:, :, :n], X, reads=[X])
            K.end_phase()

    def final_phase(self, src):
        K = self.K
        nc = self.nc
        NB = 256
        blocks = token_blocks(self.S, NB)[1:]
        with ExitStack() as st:
            self.consts(st)
            gcol = self.load_cols(st, "gcol", self.fin_gc[:, :], 8)
            xt = [K.tile(st, f"xt{i}", [128, 8, NB], F32) for i in range(2)]
            sqt = [K.tile(st, f"sq{i}", [128, NB], F32) for i in range(2)]
            rstd = K.tile(st, "rstd", [128, NB], F32)
            ss = K.psum(st, "ss")
            srcv = src.rearrange("(c p) t -> p c t", p=128)
            dstv = self.yT.rearrange("(c p) t -> p c t", p=128)
            for b, (t0, n) in enumerate(blocks):
                K.maybe_reset()
                X = xt[b % 2]
                K.dma(K.sp, X[:, :, :n], srcv[:, :, t0:t0 + n], X, writes=[X])
                self.rms_rstd(None, X, n, sqt, ss, rstd, self.ones_f)
                for c in range(8):
                    K.op(K.dve, lambda c=c, X=X: nc.vector.scalar_tensor_tensor(
                        out=X[:, c, :n], in0=X[:, c, :n], scalar=gcol[:, c:c + 1], in1=rstd[:, :n],
                        op0=ALU.mult, op1=ALU.mult), reads=[X.sub(c), gcol, rstd], writes=[X.sub(c)])
                K.dma(K.sp, dstv[:, :, t0 - NMETA:t0 - NMETA + n], X[:, :, :n], X, reads=[X])
            K.end_phase()


def full_plan():
    plan = []
    for i in range(DEPTH):
        plan.append(("ffn", i, 0))
        plan.append(("attn", i) if i % 2 == 0 else ("rec", i))
        plan.append(("ffn", i, 1))
    return plan


def cols128(v):
    v = np.asarray(v, np.float32)
    return np.ascontiguousarray(v.reshape(8, 128).T)


def host_inputs(inp, S):
    f32 = np.float32
    shared = {}
    ng = np.asarray(inp["norm_g"], f32)
    shared["norm_gc"] = np.ascontiguousarray(
        np.concatenate([cols128(ng[i, k]) for i in range(DEPTH) for k in range(3)], axis=1))
    shared["fin_gc"] = cols128(inp["final_norm_g"])
    for k in ["ffn_w_gu", "ffn_w_down", "attn_w_in", "attn_w_out", "rec_w_in", "rec_w_out", "rec_gate_w"]:
        shared[k] = np.ascontiguousarray(np.asarray(inp[k], f32))
    lg = np.asarray(inp["idx_k_ln_g"], f32)
    lb = np.asarray(inp["idx_k_ln_b"], f32)
    shared["idx_lnc"] = np.ascontiguousarray(np.stack([lg[0], lb[0], lg[1], lb[1]], axis=1))
    dl = np.asarray(inp["diff_lambda"], f32).reshape(1, 2 * 256)
    shared["dlam_bc"] = np.ascontiguousarray(np.broadcast_to(dl, (128, 512)))
    shared["subln_c"] = np.ascontiguousarray(np.asarray(inp["diff_subln_g"], f32).T)
    rc = []
    for j in range(2):
        for w in range(4):
            rc.append(cols128(inp["rec_conv_w"][j][w]))
        rc.append(cols128(inp["rec_conv_b"][j]))
        rc.append(cols128(inp["rec_gate_b"][j][0]))
        rc.append(cols128(inp["rec_gate_b"][j][1]))
        rc.append(cols128(inp["rec_lambda"][j]))
    shared["rec_cols"] = np.ascontiguousarray(np.concatenate(rc, axis=1))
    shared["c_ident_bf"] = np.eye(128, dtype=f32).astype(ml_dtypes.bfloat16)
    q = np.arange(128)[:, None]
    k = np.arange(128)[None, :]
    shared["c_negmask"] = np.where(k <= q, 0.0, NEG).astype(f32)
    kk = np.arange(128)[:, None, None]
    jj = np.arange(4)[None, :, None]
    qq = np.arange(512)[None, None, :]
    shared["c_cmaskT"] = np.ascontiguousarray(
        ((128 * jj + kk) <= qq).astype(f32).reshape(128, 2048)).astype(ml_dtypes.bfloat16)
    k16 = np.arange(16)[:, None]
    q16 = np.arange(16)[None, :]
    shared["c_cmeta"] = (k16 <= q16).astype(f32).astype(ml_dtypes.bfloat16)
    x = np.asarray(inp["x"], f32)
    meta = np.asarray(inp["meta_tokens"], f32)
    maps = []
    for b in range(x.shape[0]):
        m = dict(shared)
        m["xT"] = np.ascontiguousarray(np.concatenate([meta, x[b]], axis=0).T)
        maps.append(m)
    return maps


_CACHE = {}


def run(inp, plan=None):
    x = np.asarray(inp["x"])
    B, S, _ = x.shape
    plan = full_plan() if plan is None else plan
    key = (S, tuple(plan))
    if key not in _CACHE:
        _CACHE[key] = Prog(S, plan)
    prog = _CACHE[key]
    maps = host_inputs(inp, S)
    res = run_bass_kernel_spmd(prog.nc, maps, core_ids=list(range(B)))
    out = np.stack([np.ascontiguousarray(res.results[b]["yT"].T) for b in range(B)], axis=0)
    return out.astype(np.float32)


def kernel(**inputs):
    return run(inputs)
```

```python
import math
from contextlib import ExitStack

import numpy as np
import ml_dtypes
import concourse.bass as bass
import concourse.mybir as mybir
from concourse.bass_utils import run_bass_kernel_spmd

F32 = mybir.dt.float32
BF16 = mybir.dt.bfloat16
AF = mybir.ActivationFunctionType
ALU = mybir.AluOpType
AX = mybir.AxisListType

D = 1024
NMETA = 16
DFF = 2816
EPS = 1e-6
DEPTH = 4
ATTN_IN = 3656
NEG = -1.0e30
RESET_LIMIT = 700


ALL_RES = []


class Res:
    __slots__ = ("last_w", "readers", "parent", "kids")

    def __init__(self, parent=None):
        ALL_RES.append(self)
        self.last_w = None
        self.readers = {}
        self.parent = parent
        self.kids = []
        if parent is not None:
            parent.kids.append(self)

    def related(self):
        out = [self]
        p = self.parent
        while p is not None:
            out.append(p)
            p = p.parent
        stack = list(self.kids)
        while stack:
            k = stack.pop()
            out.append(k)
            stack.extend(k.kids)
        return out


class Tile:
    def __init__(self, K, handle, name):
        self.K = K
        self.h = handle
        self.name = name
        self.res = Res()
        self.subs = {}
        self.slots = {}
        self.dsem = None
        self.dcnt = 0

    def __getitem__(self, idx):
        return self.h[idx]

    def slot(self, key):
        sl = self.slots.get(key)
        if sl is None:
            sl = Tile(self.K, self.h, f"{self.name}_s{key}")
            self.slots[key] = sl
            self.K.phase_tiles.append(sl)
        return sl

    def sub(self, key):
        r = self.subs.get(key)
        if r is None:
            r = Res(self.res)
            self.subs[key] = r
        return r


class Eng:
    def __init__(self, K, name, eng, self_dep):
        self.K = K
        self.name = name
        self.eng = eng
        self.sem = K.nc.alloc_semaphore("es_" + name)
        self.cnt = 0
        self.seen = {}
        self.self_dep = self_dep


class Kb:
    def __init__(self, nc):
        self.nc = nc
        fs = list(nc.free_semaphores)
        nc.gpsimd.sem_clear(range(min(fs), max(fs) + 1))
        self.pe = Eng(self, "pe", nc.tensor, False)
        self.act = Eng(self, "act", nc.scalar, True)
        self.dve = Eng(self, "dve", nc.vector, True)
        self.pool = Eng(self, "pool", nc.gpsimd, True)
        self.sp = Eng(self, "sp", nc.sync, True)
        self.engs = [self.pe, self.act, self.dve, self.pool, self.sp]
        self.sems = {}
        for e in self.engs:
            self.sems[id(e.sem)] = (e.sem, e)
        self.dram_res = {}
        self.phase_tiles = []
        self.n_ops = 0
        self.free_hw = []
        self.sw_holders = []
        self.sw_idx = 0
        self.uid = 0
        for e in self.engs:
            nc.gpsimd.sem_clear(e.sem)
        nc.all_engine_barrier()

    def tile(self, st, name, shape, dtype):
        self.uid += 1
        name = f"{name}_{self.uid}"
        h = st.enter_context(self.nc.sbuf_tensor(name, list(shape), dtype))
        t = Tile(self, h, name)
        self.phase_tiles.append(t)
        return t

    def psum(self, st, name, shape=(128, 512), dtype=F32):
        self.uid += 1
        name = f"{name}_{self.uid}"
        h = st.enter_context(self.nc.psum_tensor(name, list(shape), dtype))
        return Tile(self, h, name)

    def swslot(self):
        if self.sw_idx == len(self.sw_holders):
            hld = Tile(self, None, f"sw{self.sw_idx}")
            hld.dsem = self.nc.alloc_semaphore(f"ds_sw{self.sw_idx}")
            hld.dsem_sw = True
            self.sems[id(hld.dsem)] = (hld.dsem, hld)
            self.sw_holders.append(hld)
        hld = self.sw_holders[self.sw_idx]
        self.sw_idx += 1
        return hld

    def dres(self, *key):
        r = self.dram_res.get(key)
        if r is None:
            r = Res()
            self.dram_res[key] = r
        return r

    def _need(self, reads, writes):
        need = {}

        def add(rec):
            if rec is None:
                return
            k, v = rec
            if need.get(k, 0) < v:
                need[k] = v

        for r in reads:
            for x in r.related():
                add(x.last_w)
        for w in writes:
            for x in w.related():
                add(x.last_w)
                for k, v in x.readers.items():
                    add((k, v))
        return need

    def _wait(self, E, need):
        for k, v in need.items():
            sem, owner = self.sems[k]
            if owner is E and not E.self_dep:
                continue
            if E.seen.get(k, 0) >= v:
                continue
            E.eng.wait_ge(sem, v)
            E.seen[k] = v

    @staticmethod
    def _resl(xs):
        out = []
        for x in xs:
            out.append(x.res if isinstance(x, Tile) else x)
        return out

    def op(self, E, fn, reads=(), writes=(), sig=True):
        reads = self._resl(reads)
        writes = self._resl(writes)
        self._wait(E, self._need(reads, writes))
        ins = fn()
        if sig:
            E.cnt += 1
            ins.then_inc(E.sem, 1)
            rec = (id(E.sem), E.cnt)
        else:
            rec = (id(E.sem), E.cnt + 1)
        for r in reads:
            if r.readers.get(rec[0], 0) < rec[1]:
                r.readers[rec[0]] = rec[1]
        for w in writes:
            w.last_w = rec
            w.readers = {}
        self.n_ops += 1
        return ins

    def mm(self, out, lhsT, rhs, start, stop, reads, writes, sig=None):
        nc = self.nc
        return self.op(self.pe, lambda: nc.tensor.matmul(out=out, lhsT=lhsT, rhs=rhs, start=start, stop=stop),
                       reads=reads, writes=writes, sig=stop if sig is None else sig)

    def dma(self, E, out, in_, stile, reads=(), writes=(), **kw):
        reads = self._resl(reads)
        writes = self._resl(writes)
        if stile.dsem is None:
            assert E is not self.pool, "gpsimd DMAs must use K.swslot() holders"
            stile.dsem = self.free_hw.pop() if self.free_hw else self.nc.alloc_semaphore("ds_" + stile.name)
            stile.dsem_sw = False
            self.sems[id(stile.dsem)] = (stile.dsem, stile)
        assert stile.dsem_sw == (E is self.pool)
        self._wait(E, self._need(reads, writes))
        ins = E.eng.dma_start(out=out, in_=in_, **kw)
        stile.dcnt += 16
        ins.then_inc(stile.dsem, 16)
        rec = (id(stile.dsem), stile.dcnt)
        for r in reads:
            if r.readers.get(rec[0], 0) < rec[1]:
                r.readers[rec[0]] = rec[1]
        for w in writes:
            w.last_w = rec
            w.readers = {}
        self.n_ops += 1
        return ins

    def reset(self):
        nc = self.nc
        sp = self.sp
        for e in self.engs:
            if e is not sp and e.cnt > 0 and sp.seen.get(id(e.sem), 0) < e.cnt:
                sp.eng.wait_ge(e.sem, e.cnt)
        for t in self.phase_tiles + self.sw_holders:
            if t.dsem is not None and t.dcnt > 0:
                sp.eng.wait_ge(t.dsem, t.dcnt)
        nc.all_engine_barrier()
        for e in self.engs:
            nc.gpsimd.sem_clear(e.sem)
            e.cnt = 0
            e.seen = {}
        for t in self.phase_tiles:
            if t.dsem is not None:
                nc.gpsimd.sem_clear(t.dsem)
                t.dcnt = 0
        nc.all_engine_barrier()
        for r in ALL_RES:
            r.last_w = None
            r.readers = {}

    def maybe_reset(self, limit=RESET_LIMIT):
        if max(e.cnt for e in self.engs) > limit:
            self.reset()

    def end_phase(self):
        self.reset()
        dsems = []
        for t in self.phase_tiles:
            if t.dsem is not None:
                dsems.append((t.dsem, t.dsem_sw))
                del self.sems[id(t.dsem)]
                t.dsem = None
        del ALL_RES[:]
        for s, sw in dsems:
            self.free_hw.append(s)
        self.sw_idx = 0
        self.phase_tiles = []
        self.dram_res = {}


def token_blocks(S, NB):
    blocks = [(0, NMETA)]
    for i in range(S // NB):
        blocks.append((NMETA + i * NB, NB))
    return blocks


class Prog:
    def __init__(self, S, plan):
        self.S = S
        self.T = S + NMETA
        self.plan = plan
        nc = bass.Bass("TRN2", target_bir_lowering=False)
        self.nc = nc
        T = self.T

        def din(name, shape, dt=F32):
            return nc.dram_tensor(name, list(shape), dt, kind="ExternalInput").ap()

        self.xin = din("xT", [D, T])
        self.norm_gc = din("norm_gc", [128, DEPTH * 3 * 8])
        self.fin_gc = din("fin_gc", [128, 8])
        self.w_gu = din("ffn_w_gu", [DEPTH, 2, D, 2 * DFF])
        self.w_dn = din("ffn_w_down", [DEPTH, 2, DFF, D])
        self.attn_w_in = din("attn_w_in", [2, D, ATTN_IN])
        self.attn_w_out = din("attn_w_out", [2, D, D])
        self.idx_lnc = din("idx_lnc", [64, 4])
        self.dlam_bc = din("dlam_bc", [128, 2 * 256])
        self.subln_c = din("subln_c", [128, 2])
        self.rec_w_in = din("rec_w_in", [2, D, 2 * D])
        self.rec_w_out = din("rec_w_out", [2, D, D])
        self.rec_gate_w = din("rec_gate_w", [2, 2, 4, 256, 256])
        self.rec_cols = din("rec_cols", [128, 2 * 8 * 8])
        self.c_ident_bf = din("c_ident_bf", [128, 128], BF16)
        self.c_negmask = din("c_negmask", [128, 128])
        self.c_cmaskT = din("c_cmaskT", [128, 4 * 512], BF16)
        self.c_cmeta = din("c_cmeta", [16, 16], BF16)
        self.yT = nc.dram_tensor("yT", [D, S], F32, kind="ExternalOutput").ap()
        self.hT = nc.dram_tensor("hT", [D, T], F32).ap()
        self.TOPK = min(256, S // 4)

        def scr(name, shape, dt=BF16):
            return nc.dram_tensor(name, list(shape), dt).ap()

        self.qaT = scr("qaT", [512, T])
        self.kaT = scr("kaT", [512, T])
        self.va = scr("va", [T, 512])
        self.qiT = scr("qiT", [512, T])
        self.kiT = scr("kiT", [64, T])
        self.wi = scr("wi", [T, 8], F32)
        self.qbT = scr("qbT", [512, T])
        self.kbT = scr("kbT", [512, T])
        self.vb = scr("vb", [T, 512])
        self.mixA = scr("mixA", [512, T])
        self.mixB = scr("mixB", [512, T])
        self.K = Kb(nc)
        self.build()

    def build(self):
        src = self.xin
        for ph in self.plan:
            kind = ph[0]
            if kind == "ffn":
                self.ffn_phase(src, ph[1], ph[2])
                src = self.hT
            elif kind == "copy":
                self.copy_phase(src)
                src = self.hT
            elif kind == "rec":
                self.rec_phase(src, ph[1])
                src = self.hT
            elif kind == "aproj":
                self.attn_proj_phase(src, ph[1])
            elif kind == "dsa":
                self.dsa_phase(ph[1])
            elif kind == "diff":
                self.diff_phase(ph[1])
            elif kind == "aout":
                self.attn_out_phase(src, ph[1])
                src = self.hT
            elif kind == "attn":
                self.attn_proj_phase(src, ph[1])
                self.dsa_phase(ph[1])
                self.diff_phase(ph[1])
                self.attn_out_phase(src, ph[1])
                src = self.hT
            else:
                raise ValueError(kind)
        self.final_phase(src)

    def rms_rstd(self, st_tiles, X, n, sqt, ss, rstd, ones):
        K = self.K
        for c0 in range(0, n, 256):
            c1 = min(n, c0 + 256)
            w = c1 - c0
            for c in range(8):
                s = sqt[c % 2]
                if getattr(self, "dve_square", False):
                    K.op(K.dve, lambda: K.nc.vector.tensor_tensor(out=s[:, :w], in0=X[:, c, c0:c1], in1=X[:, c, c0:c1], op=ALU.mult),
                         reads=[X.sub(c)], writes=[s])
                else:
                    K.op(K.act, lambda: K.nc.scalar.activation(out=s[:, :w], in_=X[:, c, c0:c1], func=AF.Square),
                         reads=[X.sub(c)], writes=[s])
                K.op(K.pe, lambda: K.nc.tensor.matmul(out=ss[:, c0:c1], lhsT=ones[:, :], rhs=s[:, :w],
                                                      start=(c == 0), stop=(c == 7)),
                     reads=[s, ones], writes=[ss])
        K.op(K.act, lambda: K.nc.scalar.activation(out=rstd[:, :n], in_=ss[:, :n], func=AF.Sqrt,
                                                   bias=self.eps_t[:, 0:1], scale=1.0 / D),
             reads=[ss, self.eps_t], writes=[rstd])
        K.op(K.dve, lambda: K.nc.vector.reciprocal(out=rstd[:, :n], in_=rstd[:, :n]), reads=[rstd], writes=[rstd])

    def consts(self, st):
        K = self.K
        nc = self.nc
        self.ones_f = K.tile(st, "ones_f", [128, 128], F32)
        self.ones_b = K.tile(st, "ones_b", [128, 128], BF16)
        self.eps_t = K.tile(st, "eps_t", [128, 1], F32)
        self.one_t = K.tile(st, "one_t", [128, 1], F32)
        K.op(K.dve, lambda: nc.vector.memset(self.one_t[:, :], 1.0), writes=[self.one_t])
        K.op(K.dve, lambda: nc.vector.memset(self.ones_f[:, :], 1.0), writes=[self.ones_f])
        K.op(K.dve, lambda: nc.vector.memset(self.ones_b[:, :], 1.0), writes=[self.ones_b])
        K.op(K.dve, lambda: nc.vector.memset(self.eps_t[:, :], EPS), writes=[self.eps_t])

    def load_cols(self, st, name, src_ap, ncols, parts=128):
        K = self.K
        t = K.tile(st, name, [parts, ncols], F32)
        K.dma(K.sp, t[:, :], src_ap, t, writes=[t])
        return t

    def wload(self, t, dst_ap, src_ap, writes):
        K = self.K
        K.dma(K.pool, dst_ap, src_ap, t, writes=writes, max_dma_last_dim=4096)

    def ffn_phase(self, src, li, fi):
        K = self.K
        nc = self.nc
        NB = 512
        blocks = token_blocks(self.S, NB)
        with ExitStack() as st:
            self.consts(st)
            wgu = K.tile(st, "wgu", [128, 8, 2 * DFF], BF16)
            wd = K.tile(st, "wd", [128, 22, D], BF16)
            ni = li * 3 + (0 if fi == 0 else 2)
            gcol = self.load_cols(st, "gcol", self.norm_gc[:, ni * 8:(ni + 1) * 8], 8)
            for k in range(8):
                self.wload(K.swslot(), wgu[:, k, :], self.w_gu[li, fi, k * 128:(k + 1) * 128, :], [wgu.sub(k)])
            wdv = self.w_dn[li, fi].rearrange("(j p) m -> p j m", p=128)
            for j0 in range(0, 22, 2):
                self.wload(K.swslot(), wd[:, j0:j0 + 2, :], wdv[:, j0:j0 + 2, :], [wd.sub(j0), wd.sub(j0 + 1)])
            xt = [K.tile(st, f"xt{i}", [128, 8, NB], F32) for i in range(2)]
            sqt = [K.tile(st, f"sq{i}", [128, 256], F32) for i in range(2)]
            rstd = K.tile(st, "rstd", [128, NB], F32)
            xn = K.tile(st, "xn", [128, 8, NB], BF16)
            sg = [K.tile(st, f"sg{i}", [128, NB], BF16) for i in range(2)]
            hh = K.tile(st, "hh", [128, 22, NB], BF16)
            ss = K.psum(st, "ss")
            pg = [K.psum(st, f"pg{i}") for i in range(2)]
            pu = [K.psum(st, f"pu{i}") for i in range(2)]
            po = [K.psum(st, f"po{i}") for i in range(2)]
            srcv = src.rearrange("(c p) t -> p c t", p=128)
            dstv = self.hT.rearrange("(c p) t -> p c t", p=128)
            for b, (t0, n) in enumerate(blocks):
                K.maybe_reset()
                X = xt[b % 2]
                K.dma(K.sp, X[:, :, :n], srcv[:, :, t0:t0 + n], X, reads=[K.dres("h", b)], writes=[X])
                self.rms_rstd(None, X, n, sqt, ss, rstd, self.ones_f)
                for c in range(8):
                    K.op(K.dve, lambda c=c: nc.vector.scalar_tensor_tensor(
                        out=xn[:, c, :n], in0=X[:, c, :n], scalar=gcol[:, c:c + 1], in1=rstd[:, :n],
                        op0=ALU.mult, op1=ALU.mult), reads=[X.sub(c), gcol, rstd], writes=[xn.sub(c)])
                for j in range(22):
                    G = pg[j % 2]
                    U = pu[j % 2]
                    for k in range(8):
                        K.op(K.pe, lambda k=k, j=j, G=G: nc.tensor.matmul(
                            out=G[:, :n], lhsT=wgu[:, k, j * 128:(j + 1) * 128], rhs=xn[:, k, :n],
                            start=(k == 0), stop=(k == 7)), reads=[wgu.sub(k), xn.sub(k)], writes=[G], sig=(k == 7))
                    for k in range(8):
                        K.op(K.pe, lambda k=k, j=j, U=U: nc.tensor.matmul(
                            out=U[:, :n], lhsT=wgu[:, k, DFF + j * 128:DFF + (j + 1) * 128], rhs=xn[:, k, :n],
                            start=(k == 0), stop=(k == 7)), reads=[wgu.sub(k), xn.sub(k)], writes=[U], sig=(k == 7))
                    s = sg[j % 2]
                    K.op(K.act, lambda s=s, G=G: nc.scalar.activation(out=s[:, :n], in_=G[:, :n], func=AF.Silu),
                         reads=[G], writes=[s])
                    K.op(K.dve, lambda s=s, U=U, j=j: nc.vector.tensor_tensor(
                        out=hh[:, j, :n], in0=s[:, :n], in1=U[:, :n], op=ALU.mult),
                        reads=[s, U], writes=[hh.sub(j)])
                for m in range(8):
                    P = po[m % 2]
                    for j in range(22):
                        K.op(K.pe, lambda j=j, m=m, P=P: nc.tensor.matmul(
                            out=P[:, :n], lhsT=wd[:, j, m * 128:(m + 1) * 128], rhs=hh[:, j, :n],
                            start=(j == 0), stop=(j == 21)), reads=[wd.sub(j), hh.sub(j)], writes=[P], sig=(j == 21))
                    K.op(K.dve, lambda m=m, P=P, X=X: nc.vector.scalar_tensor_tensor(
                        out=X[:, m, :n], in0=P[:, :n], scalar=0.5, in1=X[:, m, :n],
                        op0=ALU.mult, op1=ALU.add), reads=[P, X.sub(m)], writes=[X.sub(m)])
                K.dma(K.sp, dstv[:, :, t0:t0 + n], X[:, :, :n], X, reads=[X], writes=[K.dres("h", b)])
            K.end_phase()


    def load_norm(self, X, xn, gcol, srcv, t0, n, b, sqt, ss, rstd):
        K = self.K
        nc = self.nc
        K.dma(K.sp, X[:, :, :n], srcv[:, :, t0:t0 + n], X, reads=[K.dres("h", b)], writes=[X])
        self.rms_rstd(None, X, n, sqt, ss, rstd, self.ones_f)
        for c in range(8):
            K.op(K.dve, lambda: nc.vector.scalar_tensor_tensor(
                out=xn[:, c, :n], in0=X[:, c, :n], scalar=gcol[:, c:c + 1], in1=rstd[:, :n],
                op0=ALU.mult, op1=ALU.mult), reads=[X.sub(c), gcol, rstd], writes=[xn.sub(c)])

    def rec_phase(self, src, li):
        K = self.K
        nc = self.nc
        j = li // 2
        NB = 256
        blocks = token_blocks(self.S, NB)
        with ExitStack() as st:
            self.consts(st)
            win = K.tile(st, "rwin", [128, 8, 2 * D], BF16)
            gw = K.tile(st, "rgw", [128, 16, 256], BF16)
            wout = K.tile(st, "rwout", [128, 8, D], BF16)
            gcol = self.load_cols(st, "gcol", self.norm_gc[:, (li * 3 + 1) * 8:(li * 3 + 2) * 8], 8)
            rc = self.load_cols(st, "rc", self.rec_cols[:, j * 64:(j + 1) * 64], 64)
            for k in range(8):
                self.wload(K.swslot(), win[:, k, :], self.rec_w_in[j, k * 128:(k + 1) * 128, :], [win.sub(k)])
            gwv = self.rec_gate_w[j].rearrange("g n (ic p) jj -> p (g n ic) jj", p=128)
            for q in range(2):
                self.wload(K.swslot(), gw[:, q * 8:(q + 1) * 8, :], gwv[:, q * 8:(q + 1) * 8, :], [gw.sub(q)])
            wov = self.rec_w_out[j].rearrange("(k p) m -> p k m", p=128)
            for q in range(2):
                self.wload(K.swslot(), wout[:, q * 4:(q + 1) * 4, :], wov[:, q * 4:(q + 1) * 4, :], [wout.sub(q)])
            clam = K.tile(st, "clam", [128, 8], F32)
            K.op(K.act, lambda: nc.scalar.activation(out=clam[:, :], in_=rc[:, 56:64], func=AF.Exp, scale=-1.0),
                 reads=[rc], writes=[clam])
            K.op(K.dve, lambda: nc.vector.tensor_scalar(out=clam[:, :], in0=clam[:, :], scalar1=1.0, scalar2=None,
                                                        op0=ALU.add), reads=[clam], writes=[clam])
            K.op(K.act, lambda: nc.scalar.activation(out=clam[:, :], in_=clam[:, :], func=AF.Ln),
                 reads=[clam], writes=[clam])
            K.op(K.dve, lambda: nc.vector.tensor_scalar(out=clam[:, :], in0=clam[:, :], scalar1=-8.0, scalar2=None,
                                                        op0=ALU.mult), reads=[clam], writes=[clam])
            xt = [K.tile(st, f"xt{i}", [128, 8, NB], F32) for i in range(2)]
            sqt = [K.tile(st, f"sq{i}", [128, NB], F32) for i in range(2)]
            rstd = K.tile(st, "rstd", [128, NB], F32)
            xn = K.tile(st, "xn", [128, 8, NB], BF16)
            yb = K.tile(st, "yb", [128, 8, NB], BF16)
            xb = [K.tile(st, f"xb{i}", [128, 8, 3 + NB], F32) for i in range(2)]
            xc = K.tile(st, "xc", [128, 8, NB], F32)
            xcb = K.tile(st, "xcb", [128, 8, NB], BF16)
            t1 = [K.tile(st, f"t1{i}", [128, NB], F32) for i in range(2)]
            gx = K.tile(st, "gx", [128, 8, NB], F32)
            at = K.tile(st, "at", [128, 8, NB], F32)
            ga8 = K.tile(st, "ga8", [128, 8, NB], F32)
            mu8 = K.tile(st, "mu8", [128, 8, NB], F32)
            ut = K.tile(st, "ut", [128, 8, NB], F32)
            hs = [K.tile(st, f"hs{i}", [128, 8, NB], F32) for i in range(2)]
            zb = K.tile(st, "zb", [128, 8, NB], BF16)
            ss = K.psum(st, "ss")
            py = [K.psum(st, f"py{i}") for i in range(2)]
            pgt = [K.psum(st, f"pgt{i}") for i in range(2)]
            po = [K.psum(st, f"po{i}") for i in range(2)]
            srcv = src.rearrange("(c p) t -> p c t", p=128)
            dstv = self.hT.rearrange("(c p) t -> p c t", p=128)
            K.op(K.dve, lambda: nc.vector.memset(xb[0][:, :, 0:3], 0.0), writes=[xb[0]])
            nprev = 0
            for b, (t0, n) in enumerate(blocks):
                K.maybe_reset()
                X = xt[b % 2]
                XB = xb[b % 2]
                XBn = xb[(b + 1) % 2]
                HS = hs[b % 2]
                HSp = hs[(b + 1) % 2]
                self.dve_square = True
                self.load_norm(X, xn, gcol, srcv, t0, n, b, sqt, ss, rstd)
                self.dve_square = False
                for m in range(8):
                    P = py[m % 2]
                    T1 = t1[m % 2]
                    for k in range(8):
                        K.op(K.pe, lambda: nc.tensor.matmul(out=P[:, :n], lhsT=win[:, k, m * 128:(m + 1) * 128],
                                                            rhs=xn[:, k, :n], start=(k == 0), stop=(k == 7)),
                             reads=[win.sub(k), xn.sub(k)], writes=[P], sig=(k == 7))
                    K.op(K.dve, lambda: nc.vector.tensor_scalar(out=T1[:, :n], in0=P[:, :n], scalar1=0.044715,
                                                                scalar2=None, op0=ALU.mult), reads=[P], writes=[T1])
                    K.op(K.dve, lambda: nc.vector.tensor_tensor(out=T1[:, :n], in0=T1[:, :n], in1=P[:, :n], op=ALU.mult),
                         reads=[T1, P], writes=[T1])
                    K.op(K.dve, lambda: nc.vector.scalar_tensor_tensor(out=T1[:, :n], in0=T1[:, :n], scalar=1.0, in1=P[:, :n],
                                                                       op0=ALU.add, op1=ALU.mult),
                         reads=[T1, P], writes=[T1])
                    K.op(K.act, lambda: nc.scalar.activation(out=T1[:, :n], in_=T1[:, :n], func=AF.Sigmoid,
                                                             scale=1.5957691216057308), reads=[T1], writes=[T1])
                    K.op(K.dve, lambda: nc.vector.tensor_tensor(out=yb[:, m, :n], in0=T1[:, :n], in1=P[:, :n], op=ALU.mult),
                         reads=[T1, P], writes=[yb.sub(m)])
                for m in range(8):
                    P = py[m % 2]
                    for k in range(8):
                        K.op(K.pe, lambda: nc.tensor.matmul(out=P[:, :n], lhsT=win[:, k, D + m * 128:D + (m + 1) * 128],
                                                            rhs=xn[:, k, :n], start=(k == 0), stop=(k == 7)),
                             reads=[win.sub(k), xn.sub(k)], writes=[P], sig=(k == 7))
                    K.op(K.act, lambda: nc.scalar.activation(out=XB[:, m, 3:3 + n], in_=P[:, :n], func=AF.Copy),
                         reads=[P], writes=[XB.sub(m)])
                for m in range(8):
                    K.op(K.dve, lambda: nc.vector.tensor_scalar(
                        out=xc[:, m, :n], in0=XB[:, m, 0:n], scalar1=rc[:, m:m + 1], scalar2=rc[:, 32 + m:33 + m],
                        op0=ALU.mult, op1=ALU.add), reads=[XB.sub(m), rc], writes=[xc.sub(m)])
                    for w in range(1, 4):
                        K.op(K.dve, lambda: nc.vector.scalar_tensor_tensor(
                            out=xc[:, m, :n], in0=XB[:, m, w:w + n], scalar=rc[:, w * 8 + m:w * 8 + m + 1],
                            in1=xc[:, m, :n], op0=ALU.mult, op1=ALU.add), reads=[XB.sub(m), rc, xc.sub(m)],
                            writes=[xc.sub(m)])
                    K.op(K.pool, lambda: nc.gpsimd.tensor_copy(out=xcb[:, m, :n], in_=xc[:, m, :n]),
                         reads=[xc.sub(m)], writes=[xcb.sub(m)])
                K.op(K.pool, lambda: nc.gpsimd.tensor_copy(out=XBn[:, :, 0:3], in_=XB[:, :, n:n + 3]),
                     reads=[XB], writes=[XBn])
                for oc in range(8):
                    nb_, jc = oc // 2, oc % 2
                    P0 = pgt[0]
                    P1 = pgt[1]
                    for g, P in ((0, P0), (1, P1)):
                        for ic in range(2):
                            K.op(K.pe, lambda: nc.tensor.matmul(
                                out=P[:, :n], lhsT=gw[:, (g * 4 + nb_) * 2 + ic, jc * 128:(jc + 1) * 128],
                                rhs=xcb[:, nb_ * 2 + ic, :n], start=(ic == 0), stop=(ic == 1)),
                                reads=[gw, xcb.sub(nb_ * 2 + ic)], writes=[P], sig=(ic == 1))
                    K.op(K.act, lambda: nc.scalar.activation(out=gx[:, oc, :n], in_=P0[:, :n], func=AF.Sigmoid,
                                                             bias=rc[:, 40 + oc:41 + oc]), reads=[P0, rc], writes=[gx.sub(oc)])
                    K.op(K.act, lambda: nc.scalar.activation(out=ga8[:, oc, :n], in_=P1[:, :n], func=AF.Sigmoid,
                                                             bias=rc[:, 48 + oc:49 + oc]), reads=[P1, rc], writes=[ga8.sub(oc)])
                for oc in range(8):
                    K.op(K.act, lambda: nc.scalar.activation(out=at[:, oc, :n], in_=ga8[:, oc, :n], func=AF.Exp,
                                                             scale=clam[:, oc:oc + 1]), reads=[ga8.sub(oc), clam], writes=[at.sub(oc)])
                    K.op(K.dve, lambda: nc.vector.tensor_tensor(out=mu8[:, oc, :n], in0=at[:, oc, :n], in1=at[:, oc, :n],
                                                                op=ALU.mult), reads=[at.sub(oc)], writes=[mu8.sub(oc)])
                    K.op(K.dve, lambda: nc.vector.tensor_tensor(out=ut[:, oc, :n], in0=gx[:, oc, :n], in1=xc[:, oc, :n],
                                                                op=ALU.mult), reads=[gx.sub(oc), xc.sub(oc)], writes=[ut.sub(oc)])
                for oc in range(8):
                    K.op(K.act, lambda: nc.scalar.activation(out=mu8[:, oc, :n], in_=mu8[:, oc, :n], func=AF.Sqrt,
                                                             bias=self.one_t[:, 0:1], scale=-1.0),
                         reads=[mu8.sub(oc), self.one_t], writes=[mu8.sub(oc)])
                    K.op(K.dve, lambda: nc.vector.tensor_tensor(out=ut[:, oc, :n], in0=ut[:, oc, :n], in1=mu8[:, oc, :n],
                                                                op=ALU.mult), reads=[ut.sub(oc), mu8.sub(oc)], writes=[ut.sub(oc)])
                    init = 0.0 if b == 0 else HSp[:, oc, nprev - 1:nprev]
                    K.op(K.dve, lambda: nc.vector.tensor_tensor_scan(
                        out=HS[:, oc, :n], data0=at[:, oc, :n], data1=ut[:, oc, :n], initial=init,
                        op0=ALU.mult, op1=ALU.add), reads=[at.sub(oc), ut.sub(oc), HSp.sub(oc)], writes=[HS.sub(oc)])
                    K.op(K.dve, lambda: nc.vector.tensor_tensor(out=zb[:, oc, :n], in0=HS[:, oc, :n], in1=yb[:, oc, :n],
                                                                op=ALU.mult), reads=[HS.sub(oc), yb.sub(oc)], writes=[zb.sub(oc)])
                for m in range(8):
                    P = po[m % 2]
                    for k in range(8):
                        K.op(K.pe, lambda: nc.tensor.matmul(out=P[:, :n], lhsT=wout[:, k, m * 128:(m + 1) * 128],
                                                            rhs=zb[:, k, :n], start=(k == 0), stop=(k == 7)),
                             reads=[wout, zb.sub(k)], writes=[P], sig=(k == 7))
                    K.op(K.dve, lambda: nc.vector.tensor_tensor(out=X[:, m, :n], in0=P[:, :n], in1=X[:, m, :n], op=ALU.add),
                         reads=[P, X.sub(m)], writes=[X.sub(m)])
                K.dma(K.sp, dstv[:, :, t0:t0 + n], X[:, :, :n], X, reads=[X], writes=[K.dres("h", b)])
                nprev = n
            K.end_phase()

    def attn_proj_phase(self, src, li):
        K = self.K
        nc = self.nc
        j = li // 2
        NB = 256
        blocks = token_blocks(self.S, NB)
        with ExitStack() as st:
            self.consts(st)
            win = K.tile(st, "awin", [128, 8, ATTN_IN], BF16)
            gcol = self.load_cols(st, "gcol", self.norm_gc[:, (li * 3 + 1) * 8:(li * 3 + 2) * 8], 8)
            lnc = self.load_cols(st, "lnc", self.idx_lnc[:, j * 2:j * 2 + 2], 2, parts=64)
            for k in range(8):
                self.wload(K.swslot(), win[:, k, :], self.attn_w_in[j, k * 128:(k + 1) * 128, :], [win.sub(k)])
            xt = [K.tile(st, f"xt{i}", [128, 8, NB], F32) for i in range(2)]
            sqt = [K.tile(st, f"sq{i}", [128, NB], F32) for i in range(2)]
            rstd = K.tile(st, "rstd", [128, NB], F32)
            xn = K.tile(st, "xn", [128, 8, NB], BF16)
            fo = [K.tile(st, f"fo{i}", [128, 4, NB], BF16) for i in range(2)]
            kf = K.tile(st, "kf", [64, NB], F32)
            kx = K.tile(st, "kx", [64, NB], F32)
            ksq = K.tile(st, "ksq", [64, NB], F32)
            krs = K.tile(st, "krs", [64, NB], F32)
            ko = [K.tile(st, f"ko{i}", [64, NB], BF16) for i in range(2)]
            vo = [K.tile(st, f"vo{i}", [128, 512], BF16) for i in range(4)]
            wo = [K.tile(st, f"wo{i}", [128, 8], F32) for i in range(2)]
            ss = K.psum(st, "ss")
            pf = [K.psum(st, f"pf{i}") for i in range(2)]
            pk = K.psum(st, "pk")
            pk2 = K.psum(st, "pk2")
            pt = [K.psum(st, f"pt{i}") for i in range(2)]
            srcv = src.rearrange("(c p) t -> p c t", p=128)
            groups = [(self.qaT, 0), (self.kaT, 512), (self.qiT, 1536), (self.qbT, 2120), (self.kbT, 2632)]
            gi = 0
            vi = 0
            wi_i = 0
            for b, (t0, n) in enumerate(blocks):
                K.maybe_reset()
                X = xt[b % 2]
                self.load_norm(X, xn, gcol, srcv, t0, n, b, sqt, ss, rstd)
                for (dst, c0) in groups:
                    FO = fo[gi % 2]
                    gi += 1
                    for m in range(4):
                        P = pf[m % 2]
                        for k in range(8):
                            K.mm(P[:, :n], win[:, k, c0 + m * 128:c0 + (m + 1) * 128], xn[:, k, :n], k == 0, k == 7,
                                 [win.sub(k), xn.sub(k)], [P])
                        if m % 2 == 0:
                            K.op(K.act, lambda: nc.scalar.activation(out=FO[:, m, :n], in_=P[:, :n], func=AF.Copy),
                                 reads=[P], writes=[FO.sub(m)])
                        else:
                            K.op(K.dve, lambda: nc.vector.tensor_copy(out=FO[:, m, :n], in_=P[:, :n]),
                                 reads=[P], writes=[FO.sub(m)])
                    K.dma(K.sp, dst.rearrange("(m p) t -> p m t", p=128)[:, :, t0:t0 + n], FO[:, :, :n], FO, reads=[FO])
                for k in range(8):
                    K.mm(pk[:64, :n], win[:, k, 2048:2112], xn[:, k, :n], k == 0, k == 7, [win.sub(k), xn.sub(k)], [pk])
                K.op(K.act, lambda: nc.scalar.activation(out=kf[:, :n], in_=pk[:64, :n], func=AF.Copy), reads=[pk], writes=[kf])
                K.mm(pk2[:64, :n], self.ones_f[:64, :64], kf[:, :n], True, True, [self.ones_f, kf], [pk2])
                K.op(K.dve, lambda: nc.vector.scalar_tensor_tensor(out=kx[:, :n], in0=pk2[:64, :n], scalar=-1.0 / 64,
                                                                   in1=kf[:, :n], op0=ALU.mult, op1=ALU.add),
                     reads=[pk2, kf], writes=[kx])
                K.op(K.act, lambda: nc.scalar.activation(out=ksq[:, :n], in_=kx[:, :n], func=AF.Square), reads=[kx], writes=[ksq])
                K.mm(pk2[:64, :n], self.ones_f[:64, :64], ksq[:, :n], True, True, [self.ones_f, ksq], [pk2])
                K.op(K.act, lambda: nc.scalar.activation(out=krs[:, :n], in_=pk2[:64, :n], func=AF.Sqrt,
                                                         bias=self.eps_t[:64, 0:1], scale=1.0 / 64),
                     reads=[pk2, self.eps_t], writes=[krs])
                K.op(K.dve, lambda: nc.vector.reciprocal(out=krs[:, :n], in_=krs[:, :n]), reads=[krs], writes=[krs])
                K.op(K.dve, lambda: nc.vector.tensor_tensor(out=kx[:, :n], in0=kx[:, :n], in1=krs[:, :n], op=ALU.mult),
                     reads=[kx, krs], writes=[kx])
                KO = ko[b % 2]
                K.op(K.dve, lambda: nc.vector.tensor_scalar(out=KO[:, :n], in0=kx[:, :n], scalar1=lnc[:, 0:1],
                                                            scalar2=lnc[:, 1:2], op0=ALU.mult, op1=ALU.add),
                     reads=[kx, lnc], writes=[KO])
                K.dma(K.sp, self.kiT[:, t0:t0 + n], KO[:, :n], KO, reads=[KO])
                for c_lo in range(0, n, 128):
                    tn = min(128, n - c_lo)
                    for (dst, c0) in ((self.va, 1024), (self.vb, 3144)):
                        P = pt[vi % 2]
                        VO = vo[vi % 4]
                        vi += 1
                        for k in range(8):
                            K.mm(P[:tn, :512], xn[:, k, c_lo:c_lo + tn], win[:, k, c0:c0 + 512], k == 0, k == 7,
                                 [win.sub(k), xn.sub(k)], [P])
                        if vi % 2 == 0:
                            K.op(K.act, lambda: nc.scalar.activation(out=VO[:tn, :], in_=P[:tn, :512], func=AF.Copy),
                                 reads=[P], writes=[VO])
                        else:
                            K.op(K.dve, lambda: nc.vector.tensor_copy(out=VO[:tn, :], in_=P[:tn, :512]), reads=[P], writes=[VO])
                        K.dma(K.sp, dst[t0 + c_lo:t0 + c_lo + tn, :], VO[:tn, :], VO, reads=[VO])
                    WO = wo[wi_i % 2]
                    wi_i += 1
                    for k in range(8):
                        K.mm(pk2[:tn, :8], xn[:, k, c_lo:c_lo + tn], win[:, k, 2112:2120], k == 0, k == 7,
                             [win.sub(k), xn.sub(k)], [pk2])
                    K.op(K.dve, lambda: nc.vector.tensor_scalar(out=WO[:tn, :], in0=pk2[:tn, :8], scalar1=0.044194173824159216,
                                                                scalar2=None, op0=ALU.mult), reads=[pk2], writes=[WO])
                    K.dma(K.sp, self.wi[t0 + c_lo:t0 + c_lo + tn, :], WO[:tn, :], WO, reads=[WO])
            K.end_phase()

    def dsa_phase(self, li):
        K = self.K
        nc = self.nc
        S = self.S
        TOPK = float(self.TOPK)
        NKT = S // 128
        NQB = S // 512
        NIT = 18
        with ExitStack() as st:
            self.consts(st)
            ident = K.tile(st, "ident", [128, 128], BF16)
            K.dma(K.sp, ident[:, :], self.c_ident_bf[:, :], ident, writes=[ident])
            negm = K.tile(st, "negm", [128, 128], F32)
            K.dma(K.sp, negm[:, :], self.c_negmask[:, :], negm, writes=[negm])
            cmeta = K.tile(st, "cmeta", [16, 16], BF16)
            K.dma(K.sp, cmeta[:, :], self.c_cmeta[:, :], cmeta, writes=[cmeta])
            ki_sb = K.tile(st, "ki_sb", [64, S], BF16)
            K.dma(K.sp, ki_sb[:, :], self.kiT[:, NMETA:], ki_sb, writes=[ki_sb])
            va_sb = K.tile(st, "va_sb", [128, NKT, 512], BF16)
            vav = self.va[NMETA:, :].rearrange("(kt p) c -> p kt c", p=128)
            for g0 in range(0, NKT, 8):
                g1 = min(NKT, g0 + 8)
                K.dma(K.sp, va_sb[:, g0:g1, :], vav[:, g0:g1, :], va_sb.slot(g0), writes=[va_sb.sub(g0 // 8)])
            vmeta = K.tile(st, "vmeta", [16, 512], BF16)
            K.dma(K.sp, vmeta[:, :], self.va[0:NMETA, :], vmeta, writes=[vmeta])
            maskT = [K.tile(st, f"maskT{i}", [128, NKT, 512], BF16) for i in range(2)]
            score = K.tile(st, "score", [128, S], F32)
            mask01 = K.tile(st, "mask01", [128, S], BF16)
            rl = [K.tile(st, f"rl{i}", [128, 512], F32) for i in range(2)]
            qi_sb = [K.tile(st, f"qi{i}", [64, 8, 128], BF16) for i in range(2)]
            wi_sb = [K.tile(st, f"wi{i}", [128, 8], F32) for i in range(2)]
            smax = K.tile(st, "smax", [128, 1], F32)
            lo = K.tile(st, "lo", [128, 1], F32)
            w0 = K.tile(st, "w0", [128, 1], F32)
            mid = K.tile(st, "mid", [128, 1], F32)
            cnt = K.tile(st, "cnt", [128, 1], F32)
            gg = K.tile(st, "gg", [128, 1], F32)
            qa_sb = [K.tile(st, f"qa{i}", [64, 512], BF16) for i in range(2)]
            ka_sb = [K.tile(st, f"ka{i}", [64, NMETA + S], BF16) for i in range(2)]
            et = [K.tile(st, f"et{i}", [128, 512], BF16) for i in range(2)]
            ptl = [K.tile(st, f"ptl{i}", [128, 512], BF16) for i in range(2)]
            rs = K.tile(st, "rs", [64, 512], F32)
            oh = [K.tile(st, f"oh{i}", [64, 512], BF16) for i in range(2)]
            pl = [K.psum(st, f"pl{i}") for i in range(2)]
            ptr = K.psum(st, "ptr", (128, 4, 128), BF16)
            pst = [K.psum(st, f"pst{i}") for i in range(2)]
            po = K.psum(st, "po")
            ps = K.psum(st, "ps")
            ei = [0]

            po_sb = K.tile(st, "po_sb", [64, 512], F32)
            ps_sb = K.tile(st, "ps_sb", [64, 512], F32)

            def attend(QA, KA, h, nq, key_tiles, tq):
                nt = len(key_tiles)
                base = ei[0]
                ei[0] += nt

                def emit_score(i):
                    k0, kn = key_tiles[i][0], key_tiles[i][1]
                    PST = pst[(base + i) % 2]
                    K.mm(PST[:kn, :nq], KA[:, k0:k0 + kn], QA[:, :nq], True, True, [KA, QA], [PST])

                emit_score(0)
                if nt > 1:
                    emit_score(1)
                for i, (k0, kn, v_ap, v_res, m_ap, m_res) in enumerate(key_tiles):
                    PST = pst[(base + i) % 2]
                    E_ = et[(base + i) % 2]
                    P_ = ptl[(base + i) % 2]
                    K.op(K.act, lambda: nc.scalar.activation(out=E_[:kn, :nq], in_=PST[:kn, :nq], func=AF.Exp, scale=0.125),
                         reads=[PST], writes=[E_])
                    if m_ap is not None:
                        K.op(K.dve, lambda: nc.vector.tensor_tensor(out=P_[:kn, :nq], in0=E_[:kn, :nq], in1=m_ap, op=ALU.mult),
                             reads=[E_, m_res], writes=[P_])
                        R_ = P_
                    else:
                        R_ = E_
                    if i + 2 < nt:
                        emit_score(i + 2)
                    K.mm(po[:64, :nq], v_ap, R_[:kn, :nq], i == 0, i == nt - 1, [v_res, R_], [po], sig=True)
                    K.mm(ps[:64, :nq], self.ones_b[:kn, :64], R_[:kn, :nq], i == 0, i == nt - 1, [self.ones_b, R_], [ps], sig=True)
                OH = oh[h % 2]
                K.op(K.act, lambda: nc.scalar.activation(out=ps_sb[:, :nq], in_=ps[:64, :nq], func=AF.Copy), reads=[ps], writes=[ps_sb])
                K.op(K.act, lambda: nc.scalar.activation(out=po_sb[:, :nq], in_=po[:64, :nq], func=AF.Copy), reads=[po], writes=[po_sb])
                K.op(K.dve, lambda: nc.vector.reciprocal(out=rs[:, :nq], in_=ps_sb[:, :nq]), reads=[ps_sb], writes=[rs])
                K.op(K.dve, lambda: nc.vector.tensor_tensor(out=OH[:, :nq], in0=po_sb[:, :nq], in1=rs[:, :nq], op=ALU.mult),
                     reads=[po_sb, rs], writes=[OH])
                K.dma(K.sp, self.mixA[h * 64:(h + 1) * 64, tq:tq + nq], OH[:, :nq], OH, reads=[OH])

            for h in range(8):
                QA = qa_sb[h % 2]
                KA = ka_sb[h % 2]
                K.dma(K.sp, QA[:, :NMETA], self.qaT[h * 64:(h + 1) * 64, 0:NMETA], QA, writes=[QA])
                K.dma(K.sp, KA[:, :NMETA], self.kaT[h * 64:(h + 1) * 64, 0:NMETA], KA, writes=[KA])
                attend(QA, KA, h, NMETA, [(0, NMETA, vmeta[:NMETA, h * 64:(h + 1) * 64], vmeta, cmeta[:, :], cmeta)], 0)

            for qb in range(NQB):
                nkt = 4 * (qb + 1)
                MT = maskT[qb % 2]
                K.op(K.dve, lambda: nc.vector.memset(MT[:, 4 * qb:4 * qb + 4, :], 0.0), writes=[MT])
                for jq in range(4):
                    K.maybe_reset()
                    qt = 4 * qb + jq
                    nk = (qt + 1) * 128
                    tq0 = NMETA + qt * 128
                    QI = qi_sb[jq % 2]
                    WI = wi_sb[jq % 2]
                    K.dma(K.sp, QI[:, :, :], self.qiT[:, tq0:tq0 + 128].rearrange("(h d) q -> d h q", d=64), QI, writes=[QI])
                    K.dma(K.sp, WI[:, :], self.wi[tq0:tq0 + 128, :], WI, writes=[WI])
                    nch = (nk + 511) // 512
                    ri = 0
                    for h in range(8):
                        for ch in range(nch):
                            c0 = ch * 512
                            w = min(512, nk - c0)
                            PL = pl[ri % 2]
                            RL = rl[ri % 2]
                            ri += 1
                            K.mm(PL[:, :w], QI[:, h, :], ki_sb[:, c0:c0 + w], True, True, [QI, ki_sb], [PL])
                            K.op(K.act, lambda: nc.scalar.activation(out=RL[:, :w], in_=PL[:, :w], func=AF.Relu),
                                 reads=[PL], writes=[RL])
                            if h == 0:
                                K.op(K.dve, lambda: nc.vector.tensor_scalar(out=score[:, c0:c0 + w], in0=RL[:, :w],
                                                                            scalar1=WI[:, 0:1], scalar2=None, op0=ALU.mult),
                                     reads=[RL, WI], writes=[score.sub(ch)])
                            else:
                                K.op(K.dve, lambda: nc.vector.scalar_tensor_tensor(
                                    out=score[:, c0:c0 + w], in0=RL[:, :w], scalar=WI[:, h:h + 1], in1=score[:, c0:c0 + w],
                                    op0=ALU.mult, op1=ALU.add), reads=[RL, WI, score.sub(ch)], writes=[score.sub(ch)])
                    K.op(K.dve, lambda: nc.vector.tensor_reduce(out=smax[:, :], in_=score[:, :nk], axis=AX.X, op=ALU.max),
                         reads=[score], writes=[smax])
                    K.op(K.dve, lambda: nc.vector.tensor_reduce(out=lo[:, :], in_=score[:, :nk], axis=AX.X, op=ALU.min),
                         reads=[score], writes=[lo])
                    K.op(K.dve, lambda: nc.vector.tensor_tensor(out=score[:, qt * 128:(qt + 1) * 128],
                                                                in0=score[:, qt * 128:(qt + 1) * 128], in1=negm[:, :], op=ALU.add),
                         reads=[score, negm], writes=[score])
                    K.op(K.dve, lambda: nc.vector.tensor_tensor(out=w0[:, :], in0=smax[:, :], in1=lo[:, :], op=ALU.subtract),
                         reads=[smax, lo], writes=[w0])
                    for it in range(NIT):
                        c = 2.0 ** -(it + 1)
                        K.op(K.dve, lambda: nc.vector.scalar_tensor_tensor(out=mid[:, :], in0=w0[:, :], scalar=c, in1=lo[:, :],
                                                                           op0=ALU.mult, op1=ALU.add),
                             reads=[w0, lo], writes=[mid])
                        K.op(K.dve, lambda: nc.vector.tensor_scalar(out=mask01[:, :nk], in0=score[:, :nk], scalar1=mid[:, 0:1],
                                                                    scalar2=None, op0=ALU.is_ge, op1=ALU.add,
                                                                    accum_out=cnt[:, 0:1]),
                             reads=[score, mid], writes=[mask01, cnt])
                        K.op(K.dve, lambda: nc.vector.tensor_scalar(out=gg[:, :], in0=cnt[:, :], scalar1=TOPK, scalar2=c,
                                                                    op0=ALU.is_ge, op1=ALU.mult), reads=[cnt], writes=[gg])
                        K.op(K.dve, lambda: nc.vector.scalar_tensor_tensor(out=lo[:, :], in0=gg[:, :], scalar=w0[:, 0:1],
                                                                           in1=lo[:, :], op0=ALU.mult, op1=ALU.add),
                             reads=[gg, w0, lo], writes=[lo])
                    K.op(K.dve, lambda: nc.vector.tensor_scalar(out=mask01[:, :nk], in0=score[:, :nk], scalar1=lo[:, 0:1],
                                                                scalar2=None, op0=ALU.is_ge), reads=[score, lo], writes=[mask01])
                    for kt0 in range(0, qt + 1, 4):
                        g_ = min(4, qt + 1 - kt0)
                        for i in range(g_):
                            K.op(K.pe, lambda: nc.tensor.transpose(out=ptr[:, i, :], in_=mask01[:, (kt0 + i) * 128:(kt0 + i + 1) * 128],
                                                                   identity=ident[:, :]),
                                 reads=[mask01, ident], writes=[ptr], sig=(i == g_ - 1))
                        K.op(K.act, lambda: nc.scalar.activation(out=MT[:, kt0:kt0 + g_, jq * 128:(jq + 1) * 128],
                                                                 in_=ptr[:, 0:g_, :], func=AF.Copy), reads=[ptr], writes=[MT])
                tq = NMETA + qb * 512

                def load_head(h):
                    QA = qa_sb[h % 2]
                    KA = ka_sb[h % 2]
                    K.dma(K.sp, QA[:, :], self.qaT[h * 64:(h + 1) * 64, tq:tq + 512], QA, writes=[QA])
                    K.dma(K.sp, KA[:, :NMETA + nkt * 128], self.kaT[h * 64:(h + 1) * 64, 0:NMETA + nkt * 128], KA, writes=[KA])
                    return QA, KA

                nxt = load_head(0)
                for h in range(8):
                    K.maybe_reset()
                    QA, KA = nxt
                    if h + 1 < 8:
                        nxt = load_head(h + 1)
                    tiles = [(0, NMETA, vmeta[:NMETA, h * 64:(h + 1) * 64], vmeta, None, None)]
                    for kt in range(nkt):
                        tiles.append((NMETA + kt * 128, 128, va_sb[:, kt, h * 64:(h + 1) * 64], va_sb.sub(kt // 8),
                                      MT[:, kt, :], MT))
                    attend(QA, KA, h, 512, tiles, tq)
            K.end_phase()

    def diff_phase(self, li):
        K = self.K
        nc = self.nc
        S = self.S
        j = li // 2
        lam_init = 0.8 - 0.6 * math.exp(-0.3 * li)
        NKT = S // 128
        NQB = S // 512
        with ExitStack() as st:
            self.consts(st)
            cmT = K.tile(st, "cmT", [128, 4, 512], BF16)
            K.dma(K.sp, cmT[:, :, :], self.c_cmaskT.rearrange("p (j q) -> p j q", q=512), cmT, writes=[cmT])
            cmeta = K.tile(st, "cmeta", [16, 16], BF16)
            K.dma(K.sp, cmeta[:, :], self.c_cmeta[:, :], cmeta, writes=[cmeta])
            dl = self.load_cols(st, "dl", self.dlam_bc[:, j * 256:(j + 1) * 256], 256)
            sgc2 = self.load_cols(st, "sgc", self.subln_c[:, :], 2)
            pr = K.tile(st, "pr", [128, 64], F32)
            s12 = K.tile(st, "s12", [128, 2], F32)
            neglam = K.tile(st, "neglam", [128, 1], F32)
            sgs = K.tile(st, "sgs", [128, 1], F32)
            for r in range(2):
                K.op(K.dve, lambda: nc.vector.tensor_tensor(out=pr[:, :], in0=dl[:, r * 128:r * 128 + 64],
                                                            in1=dl[:, r * 128 + 64:r * 128 + 128], op=ALU.mult),
                     reads=[dl], writes=[pr])
                K.op(K.dve, lambda: nc.vector.tensor_reduce(out=s12[:, r:r + 1], in_=pr[:, :], axis=AX.X, op=ALU.add),
                     reads=[pr], writes=[s12])
            K.op(K.act, lambda: nc.scalar.activation(out=s12[:, :], in_=s12[:, :], func=AF.Exp), reads=[s12], writes=[s12])
            K.op(K.dve, lambda: nc.vector.tensor_tensor(out=neglam[:, :], in0=s12[:, 1:2], in1=s12[:, 0:1], op=ALU.subtract),
                 reads=[s12], writes=[neglam])
            K.op(K.dve, lambda: nc.vector.tensor_scalar(out=neglam[:, :], in0=neglam[:, :], scalar1=-lam_init, scalar2=None,
                                                        op0=ALU.add), reads=[neglam], writes=[neglam])
            K.op(K.dve, lambda: nc.vector.tensor_scalar(out=sgs[:, :], in0=sgc2[:, j:j + 1], scalar1=1.0 - lam_init, scalar2=None,
                                                        op0=ALU.mult), reads=[sgc2], writes=[sgs])
            vb_sb = K.tile(st, "vb_sb", [128, NKT, 512], BF16)
            vbv = self.vb[NMETA:, :].rearrange("(kt p) c -> p kt c", p=128)
            for g0 in range(0, NKT, 8):
                g1 = min(NKT, g0 + 8)
                K.dma(K.sp, vb_sb[:, g0:g1, :], vbv[:, g0:g1, :], vb_sb.slot(g0), writes=[vb_sb.sub(g0 // 8)])
            vmeta = K.tile(st, "vbmeta", [16, 512], BF16)
            K.dma(K.sp, vmeta[:, :], self.vb[0:NMETA, :], vmeta, writes=[vmeta])
            qb_sb = [K.tile(st, f"qb{i}", [64, 2, 512], BF16) for i in range(2)]
            kb_sb = [K.tile(st, f"kb{i}", [64, 2, NMETA + S], BF16) for i in range(2)]
            et = [K.tile(st, f"et{i}", [128, 512], BF16) for i in range(4)]
            r1 = K.tile(st, "r1", [128, 512], F32)
            a1 = K.tile(st, "a1", [128, 512], F32)
            a2 = K.tile(st, "a2", [128, 512], F32)
            sq = K.tile(st, "sqd", [128, 512], F32)
            yt = [K.tile(st, f"yt{i}", [128, 512], BF16) for i in range(2)]
            pst = [K.psum(st, f"pst{i}") for i in range(2)]
            oc_ = [K.psum(st, f"o{i}") for i in range(2)]
            sc_ = [K.psum(st, f"s{i}") for i in range(2)]
            pss = K.psum(st, "pss")
            ei = [0]

            def attend(QB, KB, h, nq, key_tiles, tq):
                steps = [(i, c) for i in range(len(key_tiles)) for c in range(2)]
                nt = len(key_tiles)
                ns = len(steps)
                base = ei[0]
                ei[0] += ns

                def emit_score(si):
                    i, c = steps[si]
                    k0, kn = key_tiles[i][0], key_tiles[i][1]
                    PST = pst[(base + si) % 2]
                    K.mm(PST[:kn, :nq], KB[:, c, k0:k0 + kn], QB[:, c, :nq], True, True, [KB, QB], [PST])

                emit_score(0)
                emit_score(1)
                for si, (i, c) in enumerate(steps):
                    (k0, kn, v_ap, v_res, m_ap, m_res) = key_tiles[i]
                    PST = pst[(base + si) % 2]
                    E_ = et[(base + si) % 4]
                    K.op(K.act, lambda: nc.scalar.activation(out=E_[:kn, :nq], in_=PST[:kn, :nq], func=AF.Exp, scale=0.125),
                         reads=[PST], writes=[E_])
                    if m_ap is not None:
                        K.op(K.dve, lambda: nc.vector.tensor_tensor(out=E_[:kn, :nq], in0=E_[:kn, :nq], in1=m_ap, op=ALU.mult),
                             reads=[E_, m_res], writes=[E_])
                    if si + 2 < ns:
                        emit_score(si + 2)
                    K.mm(oc_[c][:, :nq], v_ap, E_[:kn, :nq], i == 0, i == nt - 1, [v_res, E_], [oc_[c]], sig=True)
                    K.mm(sc_[c][:, :nq], self.ones_b[:kn, :], E_[:kn, :nq], i == 0, i == nt - 1, [self.ones_b, E_], [sc_[c]], sig=True)
                Y = yt[h % 2]
                K.op(K.dve, lambda: nc.vector.reciprocal(out=r1[:, :nq], in_=sc_[0][:, :nq]), reads=[sc_[0]], writes=[r1])
                K.op(K.dve, lambda: nc.vector.tensor_tensor(out=a1[:, :nq], in0=oc_[0][:, :nq], in1=r1[:, :nq], op=ALU.mult),
                     reads=[oc_[0], r1], writes=[a1])
                K.op(K.dve, lambda: nc.vector.reciprocal(out=r1[:, :nq], in_=sc_[1][:, :nq]), reads=[sc_[1]], writes=[r1])
                K.op(K.dve, lambda: nc.vector.tensor_tensor(out=a2[:, :nq], in0=oc_[1][:, :nq], in1=r1[:, :nq], op=ALU.mult),
                     reads=[oc_[1], r1], writes=[a2])
                K.op(K.dve, lambda: nc.vector.scalar_tensor_tensor(out=a1[:, :nq], in0=a2[:, :nq], scalar=neglam[:, 0:1],
                                                                   in1=a1[:, :nq], op0=ALU.mult, op1=ALU.add),
                     reads=[a2, neglam, a1], writes=[a1])
                K.op(K.act, lambda: nc.scalar.activation(out=sq[:, :nq], in_=a1[:, :nq], func=AF.Square), reads=[a1], writes=[sq])
                for c0 in range(0, nq, 256):
                    c1 = min(nq, c0 + 256)
                    K.op(K.pe, lambda: nc.tensor.matmul(out=pss[:, c0:c1], lhsT=self.ones_f[:, :], rhs=sq[:, c0:c1],
                                                        start=True, stop=True), reads=[self.ones_f, sq], writes=[pss])
                K.op(K.act, lambda: nc.scalar.activation(out=r1[:, :nq], in_=pss[:, :nq], func=AF.Sqrt,
                                                         bias=self.eps_t[:, 0:1], scale=1.0 / 128),
                     reads=[pss, self.eps_t], writes=[r1])
                K.op(K.dve, lambda: nc.vector.reciprocal(out=r1[:, :nq], in_=r1[:, :nq]), reads=[r1], writes=[r1])
                K.op(K.dve, lambda: nc.vector.scalar_tensor_tensor(out=Y[:, :nq], in0=a1[:, :nq], scalar=sgs[:, 0:1],
                                                                   in1=r1[:, :nq], op0=ALU.mult, op1=ALU.mult),
                     reads=[a1, sgs, r1], writes=[Y])
                K.dma(K.sp, self.mixB[h * 128:(h + 1) * 128, tq:tq + nq], Y[:, :nq], Y, reads=[Y])

            def load_qk(h, tq, nq, nkeys):
                QB = qb_sb[h % 2]
                KB = kb_sb[h % 2]
                K.dma(K.sp, QB[:, :, :nq], self.qbT[h * 128:(h + 1) * 128, tq:tq + nq].rearrange("(c d) q -> d c q", d=64),
                      QB, writes=[QB])
                K.dma(K.sp, KB[:, :, :nkeys], self.kbT[h * 128:(h + 1) * 128, 0:nkeys].rearrange("(c d) q -> d c q", d=64),
                      KB, writes=[KB])
                return QB, KB

            for h in range(4):
                QB, KB = load_qk(h, 0, NMETA, NMETA)
                attend(QB, KB, h, NMETA, [(0, NMETA, vmeta[:NMETA, h * 128:(h + 1) * 128], vmeta, cmeta[:, :], cmeta)], 0)
            for qb in range(NQB):
                nkt = 4 * (qb + 1)
                tq = NMETA + qb * 512
                nxt = load_qk(0, tq, 512, NMETA + nkt * 128)
                for h in range(4):
                    K.maybe_reset()
                    QB, KB = nxt
                    if h + 1 < 4:
                        nxt = load_qk(h + 1, tq, 512, NMETA + nkt * 128)
                    tiles = [(0, NMETA, vmeta[:NMETA, h * 128:(h + 1) * 128], vmeta, None, None)]
                    for kt in range(nkt):
                        dj = kt - 4 * qb
                        tiles.append((NMETA + kt * 128, 128, vb_sb[:, kt, h * 128:(h + 1) * 128], vb_sb.sub(kt // 8),
                                      cmT[:, dj, :] if dj >= 0 else None, cmT if dj >= 0 else None))
                    attend(QB, KB, h, 512, tiles, tq)
            K.end_phase()

    def attn_out_phase(self, src, li):
        K = self.K
        nc = self.nc
        j = li // 2
        NB = 256
        blocks = token_blocks(self.S, NB)
        with ExitStack() as st:
            woA = K.tile(st, "woA", [64, 8, D], BF16)
            woB = K.tile(st, "woB", [128, 4, D], BF16)
            wav = self.attn_w_out[j, 0:512, :].rearrange("(h d) m -> d h m", d=64)
            wbv = self.attn_w_out[j, 512:1024, :].rearrange("(h p) m -> p h m", p=128)
            for q in range(2):
                self.wload(K.swslot(), woA[:, q * 4:(q + 1) * 4, :], wav[:, q * 4:(q + 1) * 4, :], [woA.sub(q)])
                self.wload(K.swslot(), woB[:, q * 2:(q + 1) * 2, :], wbv[:, q * 2:(q + 1) * 2, :], [woB.sub(q)])
            xt = [K.tile(st, f"xt{i}", [128, 8, NB], F32) for i in range(2)]
            mA = [K.tile(st, f"mA{i}", [64, 8, NB], BF16) for i in range(2)]
            mB = [K.tile(st, f"mB{i}", [128, 4, NB], BF16) for i in range(2)]
            po = [K.psum(st, f"po{i}") for i in range(2)]
            srcv = src.rearrange("(c p) t -> p c t", p=128)
            dstv = self.hT.rearrange("(c p) t -> p c t", p=128)
            for b, (t0, n) in enumerate(blocks):
                K.maybe_reset()
                X = xt[b % 2]
                A = mA[b % 2]
                B_ = mB[b % 2]
                K.dma(K.sp, X[:, :, :n], srcv[:, :, t0:t0 + n], X, reads=[K.dres("h", b)], writes=[X])
                K.dma(K.sp, A[:, :, :n], self.mixA[:, t0:t0 + n].rearrange("(h d) t -> d h t", d=64), A, writes=[A])
                K.dma(K.sp, B_[:, :, :n], self.mixB[:, t0:t0 + n].rearrange("(h p) t -> p h t", p=128), B_, writes=[B_])
                for m in range(8):
                    P = po[m % 2]
                    for h in range(8):
                        K.mm(P[:, :n], woA[:, h, m * 128:(m + 1) * 128], A[:, h, :n], h == 0, False, [woA, A], [P])
                    for hb in range(4):
                        K.mm(P[:, :n], woB[:, hb, m * 128:(m + 1) * 128], B_[:, hb, :n], False, hb == 3, [woB, B_], [P])
                    K.op(K.dve, lambda: nc.vector.tensor_tensor(out=X[:, m, :n], in0=P[:, :n], in1=X[:, m, :n], op=ALU.add),
                         reads=[P, X.sub(m)], writes=[X.sub(m)])
                K.dma(K.sp, dstv[:, :, t0:t0 + n], X[:, :, :n], X, reads=[X], writes=[K.dres("h", b)])
            K.end_phase()

    def copy_phase(self, src):
        K = self.K
        NB = 256
        blocks = token_blocks(self.S, NB)
        with ExitStack() as st:
            xt = [K.tile(st, f"xt{i}", [128, 8, NB], F32) for i in range(2)]
            srcv = src.rearrange("(c p) t -> p c t", p=128)
            dstv = self.hT.rearrange("(c p) t -> p c t", p=128)
            for b, (t0, n) in enumerate(blocks):
                K.maybe_reset()
                X = xt[b % 2]
                K.dma(K.sp, X[:, :, :n], srcv[:, :, t0:t0 + n], X, writes=[X])
                K.dma(K.sp, dstv[:, :, t0:t0 + n], X[:, :, :n], X, reads=[X])
            K.end_phase()

    def final_phase(self, src):
        K = self.K
        nc = self.nc
        NB = 256
        blocks = token_blocks(self.S, NB)[1:]
        with ExitStack() as st:
            self.consts(st)
            gcol = self.load_cols(st, "gcol", self.fin_gc[:, :], 8)
            xt = [K.tile(st, f"xt{i}", [128, 8, NB], F32) for i in range(2)]
            sqt = [K.tile(st, f"sq{i}", [128, NB], F32) for i in range(2)]
            rstd = K.tile(st, "rstd", [128, NB], F32)
            ss = K.psum(st, "ss")
            srcv = src.rearrange("(c p) t -> p c t", p=128)
            dstv = self.yT.rearrange("(c p) t -> p c t", p=128)
            for b, (t0, n) in enumerate(blocks):
                K.maybe_reset()
                X = xt[b % 2]
                K.dma(K.sp, X[:, :, :n], srcv[:, :, t0:t0 + n], X, writes=[X])
                self.rms_rstd(None, X, n, sqt, ss, rstd, self.ones_f)
                for c in range(8):
                    K.op(K.dve, lambda c=c, X=X: nc.vector.scalar_tensor_tensor(
                        out=X[:, c, :n], in0=X[:, c, :n], scalar=gcol[:, c:c + 1], in1=rstd[:, :n],
                        op0=ALU.mult, op1=ALU.mult), reads=[X.sub(c), gcol, rstd], writes=[X.sub(c)])
                K.dma(K.sp, dstv[:, :, t0 - NMETA:t0 - NMETA + n], X[:, :, :n], X, reads=[X])
            K.end_phase()


def full_plan():
    plan = []
    for i in range(DEPTH):
        plan.append(("ffn", i, 0))
        plan.append(("attn", i) if i % 2 == 0 else ("rec", i))
        plan.append(("ffn", i, 1))
    return plan


def cols128(v):
    v = np.asarray(v, np.float32)
    return np.ascontiguousarray(v.reshape(8, 128).T)


def host_inputs(inp, S):
    f32 = np.float32
    shared = {}
    ng = np.asarray(inp["norm_g"], f32)
    shared["norm_gc"] = np.ascontiguousarray(
        np.concatenate([cols128(ng[i, k]) for i in range(DEPTH) for k in range(3)], axis=1))
    shared["fin_gc"] = cols128(inp["final_norm_g"])
    for k in ["ffn_w_gu", "ffn_w_down", "attn_w_in", "attn_w_out", "rec_w_in", "rec_w_out", "rec_gate_w"]:
        shared[k] = np.ascontiguousarray(np.asarray(inp[k], f32))
    lg = np.asarray(inp["idx_k_ln_g"], f32)
    lb = np.asarray(inp["idx_k_ln_b"], f32)
    shared["idx_lnc"] = np.ascontiguousarray(np.stack([lg[0], lb[0], lg[1], lb[1]], axis=1))
    dl = np.asarray(inp["diff_lambda"], f32).reshape(1, 2 * 256)
    shared["dlam_bc"] = np.ascontiguousarray(np.broadcast_to(dl, (128, 512)))
    shared["subln_c"] = np.ascontiguousarray(np.asarray(inp["diff_subln_g"], f32).T)
    rc = []
    for j in range(2):
        for w in range(4):
            rc.append(cols128(inp["rec_conv_w"][j][w]))
        rc.append(cols128(inp["rec_conv_b"][j]))
        rc.append(cols128(inp["rec_gate_b"][j][0]))
        rc.append(cols128(inp["rec_gate_b"][j][1]))
        rc.append(cols128(inp["rec_lambda"][j]))
    shared["rec_cols"] = np.ascontiguousarray(np.concatenate(rc, axis=1))
    shared["c_ident_bf"] = np.eye(128, dtype=f32).astype(ml_dtypes.bfloat16)
    q = np.arange(128)[:, None]
    k = np.arange(128)[None, :]
    shared["c_negmask"] = np.where(k <= q, 0.0, NEG).astype(f32)
    kk = np.arange(128)[:, None, None]
    jj = np.arange(4)[None, :, None]
    qq = np.arange(512)[None, None, :]
    shared["c_cmaskT"] = np.ascontiguousarray(
        ((128 * jj + kk) <= qq).astype(f32).reshape(128, 2048)).astype(ml_dtypes.bfloat16)
    k16 = np.arange(16)[:, None]
    q16 = np.arange(16)[None, :]
    shared["c_cmeta"] = (k16 <= q16).astype(f32).astype(ml_dtypes.bfloat16)
    x = np.asarray(inp["x"], f32)
    meta = np.asarray(inp["meta_tokens"], f32)
    maps = []
    for b in range(x.shape[0]):
        m = dict(shared)
        m["xT"] = np.ascontiguousarray(np.concatenate([meta, x[b]], axis=0).T)
        maps.append(m)
    return maps


_CACHE = {}


def run(inp, plan=None):
    x = np.asarray(inp["x"])
    B, S, _ = x.shape
    plan = full_plan() if plan is None else plan
    key = (S, tuple(plan))
    if key not in _CACHE:
        _CACHE[key] = Prog(S, plan)
    prog = _CACHE[key]
    maps = host_inputs(inp, S)
    res = run_bass_kernel_spmd(prog.nc, maps, core_ids=list(range(B)))
    out = np.stack([np.ascontiguousarray(res.results[b]["yT"].T) for b in range(B)], axis=0)
    return out.astype(np.float32)


def kernel(**inputs):
    return run(inputs)
```

```python
import math
from contextlib import ExitStack

import numpy as np
import ml_dtypes
import concourse.bass as bass
import concourse.mybir as mybir
from concourse.bass_utils import run_bass_kernel_spmd

F32 = mybir.dt.float32
BF16 = mybir.dt.bfloat16
AF = mybir.ActivationFunctionType
ALU = mybir.AluOpType
AX = mybir.AxisListType

D = 1024
NMETA = 16
DFF = 2816
EPS = 1e-6
DEPTH = 4
ATTN_IN = 3656
NEG = -1.0e30
RESET_LIMIT = 700


ALL_RES = []


class Res:
    __slots__ = ("last_w", "readers", "parent", "kids")

    def __init__(self, parent=None):
        ALL_RES.append(self)
        self.last_w = None
        self.readers = {}
        self.parent = parent
        self.kids = []
        if parent is not None:
            parent.kids.append(self)

    def related(self):
        out = [self]
        p = self.parent
        while p is not None:
            out.append(p)
            p = p.parent
        stack = list(self.kids)
        while stack:
            k = stack.pop()
            out.append(k)
            stack.extend(k.kids)
        return out


class Tile:
    def __init__(self, K, handle, name):
        self.K = K
        self.h = handle
        self.name = name
        self.res = Res()
        self.subs = {}
        self.slots = {}
        self.dsem = None
        self.dcnt = 0

    def __getitem__(self, idx):
        return self.h[idx]

    def slot(self, key):
        sl = self.slots.get(key)
        if sl is None:
            sl = Tile(self.K, self.h, f"{self.name}_s{key}")
            self.slots[key] = sl
            self.K.phase_tiles.append(sl)
        return sl

    def sub(self, key):
        r = self.subs.get(key)
        if r is None:
            r = Res(self.res)
            self.subs[key] = r
        return r


class Eng:
    def __init__(self, K, name, eng, self_dep):
        self.K = K
        self.name = name
        self.eng = eng
        self.sem = K.nc.alloc_semaphore("es_" + name)
        self.cnt = 0
        self.seen = {}
        self.self_dep = self_dep


class Kb:
    def __init__(self, nc):
        self.nc = nc
        fs = list(nc.free_semaphores)
        nc.gpsimd.sem_clear(range(min(fs), max(fs) + 1))
        self.pe = Eng(self, "pe", nc.tensor, False)
        self.act = Eng(self, "act", nc.scalar, True)
        self.dve = Eng(self, "dve", nc.vector, True)
        self.pool = Eng(self, "pool", nc.gpsimd, True)
        self.sp = Eng(self, "sp", nc.sync, True)
        self.engs = [self.pe, self.act, self.dve, self.pool, self.sp]
        self.sems = {}
        for e in self.engs:
            self.sems[id(e.sem)] = (e.sem, e)
        self.dram_res = {}
        self.phase_tiles = []
        self.n_ops = 0
        self.free_hw = []
        self.sw_holders = []
        self.sw_idx = 0
        self.uid = 0
        for e in self.engs:
            nc.gpsimd.sem_clear(e.sem)
        nc.all_engine_barrier()

    def tile(self, st, name, shape, dtype):
        self.uid += 1
        name = f"{name}_{self.uid}"
        h = st.enter_context(self.nc.sbuf_tensor(name, list(shape), dtype))
        t = Tile(self, h, name)
        self.phase_tiles.append(t)
        return t

    def psum(self, st, name, shape=(128, 512), dtype=F32):
        self.uid += 1
        name = f"{name}_{self.uid}"
        h = st.enter_context(self.nc.psum_tensor(name, list(shape), dtype))
        return Tile(self, h, name)

    def swslot(self):
        if self.sw_idx == len(self.sw_holders):
            hld = Tile(self, None, f"sw{self.sw_idx}")
            hld.dsem = self.nc.alloc_semaphore(f"ds_sw{self.sw_idx}")
            hld.dsem_sw = True
            self.sems[id(hld.dsem)] = (hld.dsem, hld)
            self.sw_holders.append(hld)
        hld = self.sw_holders[self.sw_idx]
        self.sw_idx += 1
        return hld

    def dres(self, *key):
        r = self.dram_res.get(key)
        if r is None:
            r = Res()
            self.dram_res[key] = r
        return r

    def _need(self, reads, writes):
        need = {}

        def add(rec):
            if rec is None:
                return
            k, v = rec
            if need.get(k, 0) < v:
                need[k] = v

        for r in reads:
            for x in r.related():
                add(x.last_w)
        for w in writes:
            for x in w.related():
                add(x.last_w)
                for k, v in x.readers.items():
                    add((k, v))
        return need

    def _wait(self, E, need):
        for k, v in need.items():
            sem, owner = self.sems[k]
            if owner is E and not E.self_dep:
                continue
            if E.seen.get(k, 0) >= v:
                continue
            E.eng.wait_ge(sem, v)
            E.seen[k] = v

    @staticmethod
    def _resl(xs):
        out = []
        for x in xs:
            out.append(x.res if isinstance(x, Tile) else x)
        return out

    def op(self, E, fn, reads=(), writes=(), sig=True):
        reads = self._resl(reads)
        writes = self._resl(writes)
        self._wait(E, self._need(reads, writes))
        ins = fn()
        if sig:
            E.cnt += 1
            ins.then_inc(E.sem, 1)
            rec = (id(E.sem), E.cnt)
        else:
            rec = (id(E.sem), E.cnt + 1)
        for r in reads:
            if r.readers.get(rec[0], 0) < rec[1]:
                r.readers[rec[0]] = rec[1]
        for w in writes:
            w.last_w = rec
            w.readers = {}
        self.n_ops += 1
        return ins

    def mm(self, out, lhsT, rhs, start, stop, reads, writes, sig=None):
        nc = self.nc
        return self.op(self.pe, lambda: nc.tensor.matmul(out=out, lhsT=lhsT, rhs=rhs, start=start, stop=stop),
                       reads=reads, writes=writes, sig=stop if sig is None else sig)

    def dma(self, E, out, in_, stile, reads=(), writes=(), **kw):
        reads = self._resl(reads)
        writes = self._resl(writes)
        if stile.dsem is None:
            assert E is not self.pool, "gpsimd DMAs must use K.swslot() holders"
            stile.dsem = self.free_hw.pop() if self.free_hw else self.nc.alloc_semaphore("ds_" + stile.name)
            stile.dsem_sw = False
            self.sems[id(stile.dsem)] = (stile.dsem, stile)
        assert stile.dsem_sw == (E is self.pool)
        self._wait(E, self._need(reads, writes))
        ins = E.eng.dma_start(out=out, in_=in_, **kw)
        stile.dcnt += 16
        ins.then_inc(stile.dsem, 16)
        rec = (id(stile.dsem), stile.dcnt)
        for r in reads:
            if r.readers.get(rec[0], 0) < rec[1]:
                r.readers[rec[0]] = rec[1]
        for w in writes:
            w.last_w = rec
            w.readers = {}
        self.n_ops += 1
        return ins

    def reset(self):
        nc = self.nc
        sp = self.sp
        for e in self.engs:
            if e is not sp and e.cnt > 0 and sp.seen.get(id(e.sem), 0) < e.cnt:
                sp.eng.wait_ge(e.sem, e.cnt)
        for t in self.phase_tiles + self.sw_holders:
            if t.dsem is not None and t.dcnt > 0:
                sp.eng.wait_ge(t.dsem, t.dcnt)
        nc.all_engine_barrier()
        for e in self.engs:
            nc.gpsimd.sem_clear(e.sem)
            e.cnt = 0
            e.seen = {}
        for t in self.phase_tiles:
            if t.dsem is not None:
                nc.gpsimd.sem_clear(t.dsem)
                t.dcnt = 0
        nc.all_engine_barrier()
        for r in ALL_RES:
            r.last_w = None
            r.readers = {}

    def maybe_reset(self, limit=RESET_LIMIT):
        if max(e.cnt for e in self.engs) > limit:
            self.reset()

    def end_phase(self):
        self.reset()
        dsems = []
        for t in self.phase_tiles:
            if t.dsem is not None:
                dsems.append((t.dsem, t.dsem_sw))
                del self.sems[id(t.dsem)]
                t.dsem = None
        del ALL_RES[:]
        for s, sw in dsems:
            self.free_hw.append(s)
        self.sw_idx = 0
        self.phase_tiles = []
        self.dram_res = {}


def token_blocks(S, NB):
    blocks = [(0, NMETA)]
    for i in range(S // NB):
        blocks.append((NMETA + i * NB, NB))
    return blocks


class Prog:
    def __init__(self, S, plan):
        self.S = S
        self.T = S + NMETA
        self.plan = plan
        nc = bass.Bass("TRN2", target_bir_lowering=False)
        self.nc = nc
        T = self.T

        def din(name, shape, dt=F32):
            return nc.dram_tensor(name, list(shape), dt, kind="ExternalInput").ap()

        self.xin = din("xT", [D, T])
        self.norm_gc = din("norm_gc", [128, DEPTH * 3 * 8])
        self.fin_gc = din("fin_gc", [128, 8])
        self.w_gu = din("ffn_w_gu", [DEPTH, 2, D, 2 * DFF])
        self.w_dn = din("ffn_w_down", [DEPTH, 2, DFF, D])
        self.attn_w_in = din("attn_w_in", [2, D, ATTN_IN])
        self.attn_w_out = din("attn_w_out", [2, D, D])
        self.idx_lnc = din("idx_lnc", [64, 4])
        self.dlam_bc = din("dlam_bc", [128, 2 * 256])
        self.subln_c = din("subln_c", [128, 2])
        self.rec_w_in = din("rec_w_in", [2, D, 2 * D])
        self.rec_w_out = din("rec_w_out", [2, D, D])
        self.rec_gate_w = din("rec_gate_w", [2, 2, 4, 256, 256])
        self.rec_cols = din("rec_cols", [128, 2 * 8 * 8])
        self.c_ident_bf = din("c_ident_bf", [128, 128], BF16)
        self.c_negmask = din("c_negmask", [128, 128])
        self.c_cmaskT = din("c_cmaskT", [128, 4 * 512], BF16)
        self.c_cmeta = din("c_cmeta", [16, 16], BF16)
        self.yT = nc.dram_tensor("yT", [D, S], F32, kind="ExternalOutput").ap()
        self.hT = nc.dram_tensor("hT", [D, T], F32).ap()
        self.TOPK = min(256, S // 4)

        def scr(name, shape, dt=BF16):
            return nc.dram_tensor(name, list(shape), dt).ap()

        self.qaT = scr("qaT", [512, T])
        self.kaT = scr("kaT", [512, T])
        self.va = scr("va", [T, 512])
        self.qiT = scr("qiT", [512, T])
        self.kiT = scr("kiT", [64, T])
        self.wi = scr("wi", [T, 8], F32)
        self.qbT = scr("qbT", [512, T])
        self.kbT = scr("kbT", [512, T])
        self.vb = scr("vb", [T, 512])
        self.mixA = scr("mixA", [512, T])
        self.mixB = scr("mixB", [512, T])
        self.K = Kb(nc)
        self.build()

    def build(self):
        src = self.xin
        for ph in self.plan:
            kind = ph[0]
            if kind == "ffn":
                self.ffn_phase(src, ph[1], ph[2])
                src = self.hT
            elif kind == "copy":
                self.copy_phase(src)
                src = self.hT
            elif kind == "rec":
                self.rec_phase(src, ph[1])
                src = self.hT
            elif kind == "aproj":
                self.attn_proj_phase(src, ph[1])
            elif kind == "dsa":
                self.dsa_phase(ph[1])
            elif kind == "diff":
                self.diff_phase(ph[1])
            elif kind == "aout":
                self.attn_out_phase(src, ph[1])
                src = self.hT
            elif kind == "attn":
                self.attn_proj_phase(src, ph[1])
                self.dsa_phase(ph[1])
                self.diff_phase(ph[1])
                self.attn_out_phase(src, ph[1])
                src = self.hT
            else:
                raise ValueError(kind)
        self.final_phase(src)

    def rms_rstd(self, st_tiles, X, n, sqt, ss, rstd, ones):
        K = self.K
        for c0 in range(0, n, 256):
            c1 = min(n, c0 + 256)
            w = c1 - c0
            for c in range(8):
                s = sqt[c % 2]
                if getattr(self, "dve_square", False):
                    K.op(K.dve, lambda: K.nc.vector.tensor_tensor(out=s[:, :w], in0=X[:, c, c0:c1], in1=X[:, c, c0:c1], op=ALU.mult),
                         reads=[X.sub(c)], writes=[s])
                else:
                    K.op(K.act, lambda: K.nc.scalar.activation(out=s[:, :w], in_=X[:, c, c0:c1], func=AF.Square),
                         reads=[X.sub(c)], writes=[s])
                K.op(K.pe, lambda: K.nc.tensor.matmul(out=ss[:, c0:c1], lhsT=ones[:, :], rhs=s[:, :w],
                                                      start=(c == 0), stop=(c == 7)),
                     reads=[s, ones], writes=[ss])
        K.op(K.act, lambda: K.nc.scalar.activation(out=rstd[:, :n], in_=ss[:, :n], func=AF.Sqrt,
                                                   bias=self.eps_t[:, 0:1], scale=1.0 / D),
             reads=[ss, self.eps_t], writes=[rstd])
        K.op(K.dve, lambda: K.nc.vector.reciprocal(out=rstd[:, :n], in_=rstd[:, :n]), reads=[rstd], writes=[rstd])

    def consts(self, st):
        K = self.K
        nc = self.nc
        self.ones_f = K.tile(st, "ones_f", [128, 128], F32)
        self.ones_b = K.tile(st, "ones_b", [128, 128], BF16)
        self.eps_t = K.tile(st, "eps_t", [128, 1], F32)
        self.one_t = K.tile(st, "one_t", [128, 1], F32)
        K.op(K.dve, lambda: nc.vector.memset(self.one_t[:, :], 1.0), writes=[self.one_t])
        K.op(K.dve, lambda: nc.vector.memset(self.ones_f[:, :], 1.0), writes=[self.ones_f])
        K.op(K.dve, lambda: nc.vector.memset(self.ones_b[:, :], 1.0), writes=[self.ones_b])
        K.op(K.dve, lambda: nc.vector.memset(self.eps_t[:, :], EPS), writes=[self.eps_t])

    def load_cols(self, st, name, src_ap, ncols, parts=128):
        K = self.K
        t = K.tile(st, name, [parts, ncols], F32)
        K.dma(K.sp, t[:, :], src_ap, t, writes=[t])
        return t

    def wload(self, t, dst_ap, src_ap, writes):
        K = self.K
        K.dma(K.pool, dst_ap, src_ap, t, writes=writes, max_dma_last_dim=4096)

    def ffn_phase(self, src, li, fi):
        K = self.K
        nc = self.nc
        NB = 512
        blocks = token_blocks(self.S, NB)
        with ExitStack() as st:
            self.consts(st)
            wgu = K.tile(st, "wgu", [128, 8, 2 * DFF], BF16)
            wd = K.tile(st, "wd", [128, 22, D], BF16)
            ni = li * 3 + (0 if fi == 0 else 2)
            gcol = self.load_cols(st, "gcol", self.norm_gc[:, ni * 8:(ni + 1) * 8], 8)
            for k in range(8):
                self.wload(K.swslot(), wgu[:, k, :], self.w_gu[li, fi, k * 128:(k + 1) * 128, :], [wgu.sub(k)])
            wdv = self.w_dn[li, fi].rearrange("(j p) m -> p j m", p=128)
            for j0 in range(0, 22, 2):
                self.wload(K.swslot(), wd[:, j0:j0 + 2, :], wdv[:, j0:j0 + 2, :], [wd.sub(j0), wd.sub(j0 + 1)])
            xt = [K.tile(st, f"xt{i}", [128, 8, NB], F32) for i in range(2)]
            sqt = [K.tile(st, f"sq{i}", [128, 256], F32) for i in range(2)]
            rstd = K.tile(st, "rstd", [128, NB], F32)
            xn = K.tile(st, "xn", [128, 8, NB], BF16)
            sg = [K.tile(st, f"sg{i}", [128, NB], BF16) for i in range(2)]
            hh = K.tile(st, "hh", [128, 22, NB], BF16)
            ss = K.psum(st, "ss")
            pg = [K.psum(st, f"pg{i}") for i in range(2)]
            pu = [K.psum(st, f"pu{i}") for i in range(2)]
            po = [K.psum(st, f"po{i}") for i in range(2)]
            srcv = src.rearrange("(c p) t -> p c t", p=128)
            dstv = self.hT.rearrange("(c p) t -> p c t", p=128)
            for b, (t0, n) in enumerate(blocks):
                K.maybe_reset()
                X = xt[b % 2]
                K.dma(K.sp, X[:, :, :n], srcv[:, :, t0:t0 + n], X, reads=[K.dres("h", b)], writes=[X])
                self.rms_rstd(None, X, n, sqt, ss, rstd, self.ones_f)
                for c in range(8):
                    K.op(K.dve, lambda c=c: nc.vector.scalar_tensor_tensor(
                        out=xn[:, c, :n], in0=X[:, c, :n], scalar=gcol[:, c:c + 1], in1=rstd[:, :n],
                        op0=ALU.mult, op1=ALU.mult), reads=[X.sub(c), gcol, rstd], writes=[xn.sub(c)])
                for j in range(22):
                    G = pg[j % 2]
                    U = pu[j % 2]
                    for k in range(8):
                        K.op(K.pe, lambda k=k, j=j, G=G: nc.tensor.matmul(
                            out=G[:, :n], lhsT=wgu[:, k, j * 128:(j + 1) * 128], rhs=xn[:, k, :n],
                            start=(k == 0), stop=(k == 7)), reads=[wgu.sub(k), xn.sub(k)], writes=[G], sig=(k == 7))
                    for k in range(8):
                        K.op(K.pe, lambda k=k, j=j, U=U: nc.tensor.matmul(
                            out=U[:, :n], lhsT=wgu[:, k, DFF + j * 128:DFF + (j + 1) * 128], rhs=xn[:, k, :n],
                            start=(k == 0), stop=(k == 7)), reads=[wgu.sub(k), xn.sub(k)], writes=[U], sig=(k == 7))
                    s = sg[j % 2]
                    K.op(K.act, lambda s=s, G=G: nc.scalar.activation(out=s[:, :n], in_=G[:, :n], func=AF.Silu),
                         reads=[G], writes=[s])
                    K.op(K.dve, lambda s=s, U=U, j=j: nc.vector.tensor_tensor(
                        out=hh[:, j, :n], in0=s[:, :n], in1=U[:, :n], op=ALU.mult),
                        reads=[s, U], writes=[hh.sub(j)])
                for m in range(8):
                    P = po[m % 2]
                    for j in range(22):
                        K.op(K.pe, lambda j=j, m=m, P=P: nc.tensor.matmul(
                            out=P[:, :n], lhsT=wd[:, j, m * 128:(m + 1) * 128], rhs=hh[:, j, :n],
                            start=(j == 0), stop=(j == 21)), reads=[wd.sub(j), hh.sub(j)], writes=[P], sig=(j == 21))
                    K.op(K.dve, lambda m=m, P=P, X=X: nc.vector.scalar_tensor_tensor(
                        out=X[:, m, :n], in0=P[:, :n], scalar=0.5, in1=X[:, m, :n],
                        op0=ALU.mult, op1=ALU.add), reads=[P, X.sub(m)], writes=[X.sub(m)])
                K.dma(K.sp, dstv[:, :, t0:t0 + n], X[:, :, :n], X, reads=[X], writes=[K.dres("h", b)])
            K.end_phase()


    def load_norm(self, X, xn, gcol, srcv, t0, n, b, sqt, ss, rstd):
        K = self.K
        nc = self.nc
        K.dma(K.sp, X[:, :, :n], srcv[:, :, t0:t0 + n], X, reads=[K.dres("h", b)], writes=[X])
        self.rms_rstd(None, X, n, sqt, ss, rstd, self.ones_f)
        for c in range(8):
            K.op(K.dve, lambda: nc.vector.scalar_tensor_tensor(
                out=xn[:, c, :n], in0=X[:, c, :n], scalar=gcol[:, c:c + 1], in1=rstd[:, :n],
                op0=ALU.mult, op1=ALU.mult), reads=[X.sub(c), gcol, rstd], writes=[xn.sub(c)])

    def rec_phase(self, src, li):
        K = self.K
        nc = self.nc
        j = li // 2
        NB = 256
        blocks = token_blocks(self.S, NB)
        with ExitStack() as st:
            self.consts(st)
            win = K.tile(st, "rwin", [128, 8, 2 * D], BF16)
            gw = K.tile(st, "rgw", [128, 16, 256], BF16)
            wout = K.tile(st, "rwout", [128, 8, D], BF16)
            gcol = self.load_cols(st, "gcol", self.norm_gc[:, (li * 3 + 1) * 8:(li * 3 + 2) * 8], 8)
            rc = self.load_cols(st, "rc", self.rec_cols[:, j * 64:(j + 1) * 64], 64)
            for k in range(8):
                self.wload(K.swslot(), win[:, k, :], self.rec_w_in[j, k * 128:(k + 1) * 128, :], [win.sub(k)])
            gwv = self.rec_gate_w[j].rearrange("g n (ic p) jj -> p (g n ic) jj", p=128)
            for q in range(2):
                self.wload(K.swslot(), gw[:, q * 8:(q + 1) * 8, :], gwv[:, q * 8:(q + 1) * 8, :], [gw.sub(q)])
            wov = self.rec_w_out[j].rearrange("(k p) m -> p k m", p=128)
            for q in range(2):
                self.wload(K.swslot(), wout[:, q * 4:(q + 1) * 4, :], wov[:, q * 4:(q + 1) * 4, :], [wout.sub(q)])
            clam = K.tile(st, "clam", [128, 8], F32)
            K.op(K.act, lambda: nc.scalar.activation(out=clam[:, :], in_=rc[:, 56:64], func=AF.Exp, scale=-1.0),
                 reads=[rc], writes=[clam])
            K.op(K.dve, lambda: nc.vector.tensor_scalar(out=clam[:, :], in0=clam[:, :], scalar1=1.0, scalar2=None,
                                                        op0=ALU.add), reads=[clam], writes=[clam])
            K.op(K.act, lambda: nc.scalar.activation(out=clam[:, :], in_=clam[:, :], func=AF.Ln),
                 reads=[clam], writes=[clam])
            K.op(K.dve, lambda: nc.vector.tensor_scalar(out=clam[:, :], in0=clam[:, :], scalar1=-8.0, scalar2=None,
                                                        op0=ALU.mult), reads=[clam], writes=[clam])
            xt = [K.tile(st, f"xt{i}", [128, 8, NB], F32) for i in range(2)]
            sqt = [K.tile(st, f"sq{i}", [128, NB], F32) for i in range(2)]
            rstd = K.tile(st, "rstd", [128, NB], F32)
            xn = K.tile(st, "xn", [128, 8, NB], BF16)
            yb = K.tile(st, "yb", [128, 8, NB], BF16)
            xb = [K.tile(st, f"xb{i}", [128, 8, 3 + NB], F32) for i in range(2)]
            xc = K.tile(st, "xc", [128, 8, NB], F32)
            xcb = K.tile(st, "xcb", [128, 8, NB], BF16)
            t1 = [K.tile(st, f"t1{i}", [128, NB], F32) for i in range(2)]
            gx = K.tile(st, "gx", [128, 8, NB], F32)
            at = K.tile(st, "at", [128, 8, NB], F32)
            ga8 = K.tile(st, "ga8", [128, 8, NB], F32)
            mu8 = K.tile(st, "mu8", [128, 8, NB], F32)
            ut = K.tile(st, "ut", [128, 8, NB], F32)
            hs = [K.tile(st, f"hs{i}", [128, 8, NB], F32) for i in range(2)]
            zb = K.tile(st, "zb", [128, 8, NB], BF16)
            ss = K.psum(st, "ss")
            py = [K.psum(st, f"py{i}") for i in range(2)]
            pgt = [K.psum(st, f"pgt{i}") for i in range(2)]
            po = [K.psum(st, f"po{i}") for i in range(2)]
            srcv = src.rearrange("(c p) t -> p c t", p=128)
            dstv = self.hT.rearrange("(c p) t -> p c t", p=128)
            K.op(K.dve, lambda: nc.vector.memset(xb[0][:, :, 0:3], 0.0), writes=[xb[0]])
            nprev = 0
            for b, (t0, n) in enumerate(blocks):
                K.maybe_reset()
                X = xt[b % 2]
                XB = xb[b % 2]
                XBn = xb[(b + 1) % 2]
                HS = hs[b % 2]
                HSp = hs[(b + 1) % 2]
                self.dve_square = True
                self.load_norm(X, xn, gcol, srcv, t0, n, b, sqt, ss, rstd)
                self.dve_square = False
                for m in range(8):
                    P = py[m % 2]
                    T1 = t1[m % 2]
                    for k in range(8):
                        K.op(K.pe, lambda: nc.tensor.matmul(out=P[:, :n], lhsT=win[:, k, m * 128:(m + 1) * 128],
                                                            rhs=xn[:, k, :n], start=(k == 0), stop=(k == 7)),
                             reads=[win.sub(k), xn.sub(k)], writes=[P], sig=(k == 7))
                    K.op(K.dve, lambda: nc.vector.tensor_scalar(out=T1[:, :n], in0=P[:, :n], scalar1=0.044715,
                                                                scalar2=None, op0=ALU.mult), reads=[P], writes=[T1])
                    K.op(K.dve, lambda: nc.vector.tensor_tensor(out=T1[:, :n], in0=T1[:, :n], in1=P[:, :n], op=ALU.mult),
                         reads=[T1, P], writes=[T1])
                    K.op(K.dve, lambda: nc.vector.scalar_tensor_tensor(out=T1[:, :n], in0=T1[:, :n], scalar=1.0, in1=P[:, :n],
                                                                       op0=ALU.add, op1=ALU.mult),
                         reads=[T1, P], writes=[T1])
                    K.op(K.act, lambda: nc.scalar.activation(out=T1[:, :n], in_=T1[:, :n], func=AF.Sigmoid,
                                                             scale=1.5957691216057308), reads=[T1], writes=[T1])
                    K.op(K.dve, lambda: nc.vector.tensor_tensor(out=yb[:, m, :n], in0=T1[:, :n], in1=P[:, :n], op=ALU.mult),
                         reads=[T1, P], writes=[yb.sub(m)])
                for m in range(8):
                    P = py[m % 2]
                    for k in range(8):
                        K.op(K.pe, lambda: nc.tensor.matmul(out=P[:, :n], lhsT=win[:, k, D + m * 128:D + (m + 1) * 128],
                                                            rhs=xn[:, k, :n], start=(k == 0), stop=(k == 7)),
                             reads=[win.sub(k), xn.sub(k)], writes=[P], sig=(k == 7))
                    K.op(K.act, lambda: nc.scalar.activation(out=XB[:, m, 3:3 + n], in_=P[:, :n], func=AF.Copy),
                         reads=[P], writes=[XB.sub(m)])
                for m in range(8):
                    K.op(K.dve, lambda: nc.vector.tensor_scalar(
                        out=xc[:, m, :n], in0=XB[:, m, 0:n], scalar1=rc[:, m:m + 1], scalar2=rc[:, 32 + m:33 + m],
                        op0=ALU.mult, op1=ALU.add), reads=[XB.sub(m), rc], writes=[xc.sub(m)])
                    for w in range(1, 4):
                        K.op(K.dve, lambda: nc.vector.scalar_tensor_tensor(
                            out=xc[:, m, :n], in0=XB[:, m, w:w + n], scalar=rc[:, w * 8 + m:w * 8 + m + 1],
                            in1=xc[:, m, :n], op0=ALU.mult, op1=ALU.add), reads=[XB.sub(m), rc, xc.sub(m)],
                            writes=[xc.sub(m)])
                    K.op(K.pool, lambda: nc.gpsimd.tensor_copy(out=xcb[:, m, :n], in_=xc[:, m, :n]),
                         reads=[xc.sub(m)], writes=[xcb.sub(m)])
                K.op(K.pool, lambda: nc.gpsimd.tensor_copy(out=XBn[:, :, 0:3], in_=XB[:, :, n:n + 3]),
                     reads=[XB], writes=[XBn])
                for oc in range(8):
                    nb_, jc = oc // 2, oc % 2
                    P0 = pgt[0]
                    P1 = pgt[1]
                    for g, P in ((0, P0), (1, P1)):
                        for ic in range(2):
                            K.op(K.pe, lambda: nc.tensor.matmul(
                                out=P[:, :n], lhsT=gw[:, (g * 4 + nb_) * 2 + ic, jc * 128:(jc + 1) * 128],
                                rhs=xcb[:, nb_ * 2 + ic, :n], start=(ic == 0), stop=(ic == 1)),
                                reads=[gw, xcb.sub(nb_ * 2 + ic)], writes=[P], sig=(ic == 1))
                    K.op(K.act, lambda: nc.scalar.activation(out=gx[:, oc, :n], in_=P0[:, :n], func=AF.Sigmoid,
                                                             bias=rc[:, 40 + oc:41 + oc]), reads=[P0, rc], writes=[gx.sub(oc)])
                    K.op(K.act, lambda: nc.scalar.activation(out=ga8[:, oc, :n], in_=P1[:, :n], func=AF.Sigmoid,
                                                             bias=rc[:, 48 + oc:49 + oc]), reads=[P1, rc], writes=[ga8.sub(oc)])
                for oc in range(8):
                    K.op(K.act, lambda: nc.scalar.activation(out=at[:, oc, :n], in_=ga8[:, oc, :n], func=AF.Exp,
                                                             scale=clam[:, oc:oc + 1]), reads=[ga8.sub(oc), clam], writes=[at.sub(oc)])
                    K.op(K.dve, lambda: nc.vector.tensor_tensor(out=mu8[:, oc, :n], in0=at[:, oc, :n], in1=at[:, oc, :n],
                                                                op=ALU.mult), reads=[at.sub(oc)], writes=[mu8.sub(oc)])
                    K.op(K.dve, lambda: nc.vector.tensor_tensor(out=ut[:, oc, :n], in0=gx[:, oc, :n], in1=xc[:, oc, :n],
                                                                op=ALU.mult), reads=[gx.sub(oc), xc.sub(oc)], writes=[ut.sub(oc)])
                for oc in range(8):
                    K.op(K.act, lambda: nc.scalar.activation(out=mu8[:, oc, :n], in_=mu8[:, oc, :n], func=AF.Sqrt,
                                                             bias=self.one_t[:, 0:1], scale=-1.0),
                         reads=[mu8.sub(oc), self.one_t], writes=[mu8.sub(oc)])
                    K.op(K.dve, lambda: nc.vector.tensor_tensor(out=ut[:, oc, :n], in0=ut[:, oc, :n], in1=mu8[:, oc, :n],
                                                                op=ALU.mult), reads=[ut.sub(oc), mu8.sub(oc)], writes=[ut.sub(oc)])
                    init = 0.0 if b == 0 else HSp[:, oc, nprev - 1:nprev]
                    K.op(K.dve, lambda: nc.vector.tensor_tensor_scan(
                        out=HS[:, oc, :n], data0=at[:, oc, :n], data1=ut[:, oc, :n], initial=init,
                        op0=ALU.mult, op1=ALU.add), reads=[at.sub(oc), ut.sub(oc), HSp.sub(oc)], writes=[HS.sub(oc)])
                    K.op(K.dve, lambda: nc.vector.tensor_tensor(out=zb[:, oc, :n], in0=HS[:, oc, :n], in1=yb[:, oc, :n],
                                                                op=ALU.mult), reads=[HS.sub(oc), yb.sub(oc)], writes=[zb.sub(oc)])
                for m in range(8):
                    P = po[m % 2]
                    for k in range(8):
                        K.op(K.pe, lambda: nc.tensor.matmul(out=P[:, :n], lhsT=wout[:, k, m * 128:(m + 1) * 128],
                                                            rhs=zb[:, k, :n], start=(k == 0), stop=(k == 7)),
                             reads=[wout, zb.sub(k)], writes=[P], sig=(k == 7))
                    K.op(K.dve, lambda: nc.vector.tensor_tensor(out=X[:, m, :n], in0=P[:, :n], in1=X[:, m, :n], op=ALU.add),
                         reads=[P, X.sub(m)], writes=[X.sub(m)])
                K.dma(K.sp, dstv[:, :, t0:t0 + n], X[:, :, :n], X, reads=[X], writes=[K.dres("h", b)])
                nprev = n
            K.end_phase()

    def attn_proj_phase(self, src, li):
        K = self.K
        nc = self.nc
        j = li // 2
        NB = 256
        blocks = token_blocks(self.S, NB)
        with ExitStack() as st:
            self.consts(st)
            win = K.tile(st, "awin", [128, 8, ATTN_IN], BF16)
            gcol = self.load_cols(st, "gcol", self.norm_gc[:, (li * 3 + 1) * 8:(li * 3 + 2) * 8], 8)
            lnc = self.load_cols(st, "lnc", self.idx_lnc[:, j * 2:j * 2 + 2], 2, parts=64)
            for k in range(8):
                self.wload(K.swslot(), win[:, k, :], self.attn_w_in[j, k * 128:(k + 1) * 128, :], [win.sub(k)])
            xt = [K.tile(st, f"xt{i}", [128, 8, NB], F32) for i in range(2)]
            sqt = [K.tile(st, f"sq{i}", [128, NB], F32) for i in range(2)]
            rstd = K.tile(st, "rstd", [128, NB], F32)
            xn = K.tile(st, "xn", [128, 8, NB], BF16)
            fo = [K.tile(st, f"fo{i}", [128, 4, NB], BF16) for i in range(2)]
            kf = K.tile(st, "kf", [64, NB], F32)
            kx = K.tile(st, "kx", [64, NB], F32)
            ksq = K.tile(st, "ksq", [64, NB], F32)
            krs = K.tile(st, "krs", [64, NB], F32)
            ko = [K.tile(st, f"ko{i}", [64, NB], BF16) for i in range(2)]
            vo = [K.tile(st, f"vo{i}", [128, 512], BF16) for i in range(4)]
            wo = [K.tile(st, f"wo{i}", [128, 8], F32) for i in range(2)]
            ss = K.psum(st, "ss")
            pf = [K.psum(st, f"pf{i}") for i in range(2)]
            pk = K.psum(st, "pk")
            pk2 = K.psum(st, "pk2")
            pt = [K.psum(st, f"pt{i}") for i in range(2)]
            srcv = src.rearrange("(c p) t -> p c t", p=128)
            groups = [(self.qaT, 0), (self.kaT, 512), (self.qiT, 1536), (self.qbT, 2120), (self.kbT, 2632)]
            gi = 0
            vi = 0
            wi_i = 0
            for b, (t0, n) in enumerate(blocks):
                K.maybe_reset()
                X = xt[b % 2]
                self.load_norm(X, xn, gcol, srcv, t0, n, b, sqt, ss, rstd)
                for (dst, c0) in groups:
                    FO = fo[gi % 2]
                    gi += 1
                    for m in range(4):
                        P = pf[m % 2]
                        for k in range(8):
                            K.mm(P[:, :n], win[:, k, c0 + m * 128:c0 + (m + 1) * 128], xn[:, k, :n], k == 0, k == 7,
                                 [win.sub(k), xn.sub(k)], [P])
                        if m % 2 == 0:
                            K.op(K.act, lambda: nc.scalar.activation(out=FO[:, m, :n], in_=P[:, :n], func=AF.Copy),
                                 reads=[P], writes=[FO.sub(m)])
                        else:
                            K.op(K.dve, lambda: nc.vector.tensor_copy(out=FO[:, m, :n], in_=P[:, :n]),
                                 reads=[P], writes=[FO.sub(m)])
                    K.dma(K.sp, dst.rearrange("(m p) t -> p m t", p=128)[:, :, t0:t0 + n], FO[:, :, :n], FO, reads=[FO])
                for k in range(8):
                    K.mm(pk[:64, :n], win[:, k, 2048:2112], xn[:, k, :n], k == 0, k == 7, [win.sub(k), xn.sub(k)], [pk])
                K.op(K.act, lambda: nc.scalar.activation(out=kf[:, :n], in_=pk[:64, :n], func=AF.Copy), reads=[pk], writes=[kf])
                K.mm(pk2[:64, :n], self.ones_f[:64, :64], kf[:, :n], True, True, [self.ones_f, kf], [pk2])
                K.op(K.dve, lambda: nc.vector.scalar_tensor_tensor(out=kx[:, :n], in0=pk2[:64, :n], scalar=-1.0 / 64,
                                                                   in1=kf[:, :n], op0=ALU.mult, op1=ALU.add),
                     reads=[pk2, kf], writes=[kx])
                K.op(K.act, lambda: nc.scalar.activation(out=ksq[:, :n], in_=kx[:, :n], func=AF.Square), reads=[kx], writes=[ksq])
                K.mm(pk2[:64, :n], self.ones_f[:64, :64], ksq[:, :n], True, True, [self.ones_f, ksq], [pk2])
                K.op(K.act, lambda: nc.scalar.activation(out=krs[:, :n], in_=pk2[:64, :n], func=AF.Sqrt,
                                                         bias=self.eps_t[:64, 0:1], scale=1.0 / 64),
                     reads=[pk2, self.eps_t], writes=[krs])
                K.op(K.dve, lambda: nc.vector.reciprocal(out=krs[:, :n], in_=krs[:, :n]), reads=[krs], writes=[krs])
                K.op(K.dve, lambda: nc.vector.tensor_tensor(out=kx[:, :n], in0=kx[:, :n], in1=krs[:, :n], op=ALU.mult),
                     reads=[kx, krs], writes=[kx])
                KO = ko[b % 2]
                K.op(K.dve, lambda: nc.vector.tensor_scalar(out=KO[:, :n], in0=kx[:, :n], scalar1=lnc[:, 0:1],
                                                            scalar2=lnc[:, 1:2], op0=ALU.mult, op1=ALU.add),
                     reads=[kx, lnc], writes=[KO])
                K.dma(K.sp, self.kiT[:, t0:t0 + n], KO[:, :n], KO, reads=[KO])
                for c_lo in range(0, n, 128):
                    tn = min(128, n - c_lo)
                    for (dst, c0) in ((self.va, 1024), (self.vb, 3144)):
                        P = pt[vi % 2]
                        VO = vo[vi % 4]
                        vi += 1
                        for k in range(8):
                            K.mm(P[:tn, :512], xn[:, k, c_lo:c_lo + tn], win[:, k, c0:c0 + 512], k == 0, k == 7,
                                 [win.sub(k), xn.sub(k)], [P])
                        if vi % 2 == 0:
                            K.op(K.act, lambda: nc.scalar.activation(out=VO[:tn, :], in_=P[:tn, :512], func=AF.Copy),
                                 reads=[P], writes=[VO])
                        else:
                            K.op(K.dve, lambda: nc.vector.tensor_copy(out=VO[:tn, :], in_=P[:tn, :512]), reads=[P], writes=[VO])
                        K.dma(K.sp, dst[t0 + c_lo:t0 + c_lo + tn, :], VO[:tn, :], VO, reads=[VO])
                    WO = wo[wi_i % 2]
                    wi_i += 1
                    for k in range(8):
                        K.mm(pk2[:tn, :8], xn[:, k, c_lo:c_lo + tn], win[:, k, 2112:2120], k == 0, k == 7,
                             [win.sub(k), xn.sub(k)], [pk2])
                    K.op(K.dve, lambda: nc.vector.tensor_scalar(out=WO[:tn, :], in0=pk2[:tn, :8], scalar1=0.044194173824159216,
                                                                scalar2=None, op0=ALU.mult), reads=[pk2], writes=[WO])
                    K.dma(K.sp, self.wi[t0 + c_lo:t0 + c_lo + tn, :], WO[:tn, :], WO, reads=[WO])
            K.end_phase()

    def dsa_phase(self, li):
        K = self.K
        nc = self.nc
        S = self.S
        TOPK = float(self.TOPK)
        NKT = S // 128
        NQB = S // 512
        NIT = 18
        with ExitStack() as st:
            self.consts(st)
            ident = K.tile(st, "ident", [128, 128], BF16)
            K.dma(K.sp, ident[:, :], self.c_ident_bf[:, :], ident, writes=[ident])
            negm = K.tile(st, "negm", [128, 128], F32)
            K.dma(K.sp, negm[:, :], self.c_negmask[:, :], negm, writes=[negm])
            cmeta = K.tile(st, "cmeta", [16, 16], BF16)
            K.dma(K.sp, cmeta[:, :], self.c_cmeta[:, :], cmeta, writes=[cmeta])
            ki_sb = K.tile(st, "ki_sb", [64, S], BF16)
            K.dma(K.sp, ki_sb[:, :], self.kiT[:, NMETA:], ki_sb, writes=[ki_sb])
            va_sb = K.tile(st, "va_sb", [128, NKT, 512], BF16)
            vav = self.va[NMETA:, :].rearrange("(kt p) c -> p kt c", p=128)
            for g0 in range(0, NKT, 8):
                g1 = min(NKT, g0 + 8)
                K.dma(K.sp, va_sb[:, g0:g1, :], vav[:, g0:g1, :], va_sb.slot(g0), writes=[va_sb.sub(g0 // 8)])
            vmeta = K.tile(st, "vmeta", [16, 512], BF16)
            K.dma(K.sp, vmeta[:, :], self.va[0:NMETA, :], vmeta, writes=[vmeta])
            maskT = [K.tile(st, f"maskT{i}", [128, NKT, 512], BF16) for i in range(2)]
            score = K.tile(st, "score", [128, S], F32)
            mask01 = K.tile(st, "mask01", [128, S], BF16)
            rl = [K.tile(st, f"rl{i}", [128, 512], F32) for i in range(2)]
            qi_sb = [K.tile(st, f"qi{i}", [64, 8, 128], BF16) for i in range(2)]
            wi_sb = [K.tile(st, f"wi{i}", [128, 8], F32) for i in range(2)]
            smax = K.tile(st, "smax", [128, 1], F32)
            lo = K.tile(st, "lo", [128, 1], F32)
            w0 = K.tile(st, "w0", [128, 1], F32)
            mid = K.tile(st, "mid", [128, 1], F32)
            cnt = K.tile(st, "cnt", [128, 1], F32)
            gg = K.tile(st, "gg", [128, 1], F32)
            qa_sb = [K.tile(st, f"qa{i}", [64, 512], BF16) for i in range(2)]
            ka_sb = [K.tile(st, f"ka{i}", [64, NMETA + S], BF16) for i in range(2)]
            et = [K.tile(st, f"et{i}", [128, 512], BF16) for i in range(2)]
            ptl = [K.tile(st, f"ptl{i}", [128, 512], BF16) for i in range(2)]
            rs = K.tile(st, "rs", [64, 512], F32)
            oh = [K.tile(st, f"oh{i}", [64, 512], BF16) for i in range(2)]
            pl = [K.psum(st, f"pl{i}") for i in range(2)]
            ptr = K.psum(st, "ptr", (128, 4, 128), BF16)
            pst = [K.psum(st, f"pst{i}") for i in range(2)]
            po = K.psum(st, "po")
            ps = K.psum(st, "ps")
            ei = [0]

            po_sb = K.tile(st, "po_sb", [64, 512], F32)
            ps_sb = K.tile(st, "ps_sb", [64, 512], F32)

            def attend(QA, KA, h, nq, key_tiles, tq):
                nt = len(key_tiles)
                base = ei[0]
                ei[0] += nt

                def emit_score(i):
                    k0, kn = key_tiles[i][0], key_tiles[i][1]
                    PST = pst[(base + i) % 2]
                    K.mm(PST[:kn, :nq], KA[:, k0:k0 + kn], QA[:, :nq], True, True, [KA, QA], [PST])

                emit_score(0)
                if nt > 1:
                    emit_score(1)
                for i, (k0, kn, v_ap, v_res, m_ap, m_res) in enumerate(key_tiles):
                    PST = pst[(base + i) % 2]
                    E_ = et[(base + i) % 2]
                    P_ = ptl[(base + i) % 2]
                    K.op(K.act, lambda: nc.scalar.activation(out=E_[:kn, :nq], in_=PST[:kn, :nq], func=AF.Exp, scale=0.125),
                         reads=[PST], writes=[E_])
                    if m_ap is not None:
                        K.op(K.dve, lambda: nc.vector.tensor_tensor(out=P_[:kn, :nq], in0=E_[:kn, :nq], in1=m_ap, op=ALU.mult),
                             reads=[E_, m_res], writes=[P_])
                        R_ = P_
                    else:
                        R_ = E_
                    if i + 2 < nt:
                        emit_score(i + 2)
                    K.mm(po[:64, :nq], v_ap, R_[:kn, :nq], i == 0, i == nt - 1, [v_res, R_], [po], sig=True)
                    K.mm(ps[:64, :nq], self.ones_b[:kn, :64], R_[:kn, :nq], i == 0, i == nt - 1, [self.ones_b, R_], [ps], sig=True)
                OH = oh[h % 2]
                K.op(K.act, lambda: nc.scalar.activation(out=ps_sb[:, :nq], in_=ps[:64, :nq], func=AF.Copy), reads=[ps], writes=[ps_sb])
                K.op(K.act, lambda: nc.scalar.activation(out=po_sb[:, :nq], in_=po[:64, :nq], func=AF.Copy), reads=[po], writes=[po_sb])
                K.op(K.dve, lambda: nc.vector.reciprocal(out=rs[:, :nq], in_=ps_sb[:, :nq]), reads=[ps_sb], writes=[rs])
                K.op(K.dve, lambda: nc.vector.tensor_tensor(out=OH[:, :nq], in0=po_sb[:, :nq], in1=rs[:, :nq], op=ALU.mult),
                     reads=[po_sb, rs], writes=[OH])
                K.dma(K.sp, self.mixA[h * 64:(h + 1) * 64, tq:tq + nq], OH[:, :nq], OH, reads=[OH])

            for h in range(8):
                QA = qa_sb[h % 2]
                KA = ka_sb[h % 2]
                K.dma(K.sp, QA[:, :NMETA], self.qaT[h * 64:(h + 1) * 64, 0:NMETA], QA, writes=[QA])
                K.dma(K.sp, KA[:, :NMETA], self.kaT[h * 64:(h + 1) * 64, 0:NMETA], KA, writes=[KA])
                attend(QA, KA, h, NMETA, [(0, NMETA, vmeta[:NMETA, h * 64:(h + 1) * 64], vmeta, cmeta[:, :], cmeta)], 0)

            for qb in range(NQB):
                nkt = 4 * (qb + 1)
                MT = maskT[qb % 2]
                K.op(K.dve, lambda: nc.vector.memset(MT[:, 4 * qb:4 * qb + 4, :], 0.0), writes=[MT])
                for jq in range(4):
                    K.maybe_reset()
                    qt = 4 * qb + jq
                    nk = (qt + 1) * 128
                    tq0 = NMETA + qt * 128
                    QI = qi_sb[jq % 2]
                    WI = wi_sb[jq % 2]
                    K.dma(K.sp, QI[:, :, :], self.qiT[:, tq0:tq0 + 128].rearrange("(h d) q -> d h q", d=64), QI, writes=[QI])
                    K.dma(K.sp, WI[:, :], self.wi[tq0:tq0 + 128, :], WI, writes=[WI])
                    nch = (nk + 511) // 512
                    ri = 0
                    for h in range(8):
                        for ch in range(nch):
                            c0 = ch * 512
                            w = min(512, nk - c0)
                            PL = pl[ri % 2]
                            RL = rl[ri % 2]
                            ri += 1
                            K.mm(PL[:, :w], QI[:, h, :], ki_sb[:, c0:c0 + w], True, True, [QI, ki_sb], [PL])
                            K.op(K.act, lambda: nc.scalar.activation(out=RL[:, :w], in_=PL[:, :w], func=AF.Relu),
                                 reads=[PL], writes=[RL])
                            if h == 0:
                                K.op(K.dve, lambda: nc.vector.tensor_scalar(out=score[:, c0:c0 + w], in0=RL[:, :w],
                                                                            scalar1=WI[:, 0:1], scalar2=None, op0=ALU.mult),
                                     reads=[RL, WI], writes=[score.sub(ch)])
                            else:
                                K.op(K.dve, lambda: nc.vector.scalar_tensor_tensor(
                                    out=score[:, c0:c0 + w], in0=RL[:, :w], scalar=WI[:, h:h + 1], in1=score[:, c0:c0 + w],
                                    op0=ALU.mult, op1=ALU.add), reads=[RL, WI, score.sub(ch)], writes=[score.sub(ch)])
                    K.op(K.dve, lambda: nc.vector.tensor_reduce(out=smax[:, :], in_=score[:, :nk], axis=AX.X, op=ALU.max),
                         reads=[score], writes=[smax])
                    K.op(K.dve, lambda: nc.vector.tensor_reduce(out=lo[:, :], in_=score[:, :nk], axis=AX.X, op=ALU.min),
                         reads=[score], writes=[lo])
                    K.op(K.dve, lambda: nc.vector.tensor_tensor(out=score[:, qt * 128:(qt + 1) * 128],
                                                                in0=score[:, qt * 128:(qt + 1) * 128], in1=negm[:, :], op=ALU.add),
                         reads=[score, negm], writes=[score])
                    K.op(K.dve, lambda: nc.vector.tensor_tensor(out=w0[:, :], in0=smax[:, :], in1=lo[:, :], op=ALU.subtract),
                         reads=[smax, lo], writes=[w0])
                    for it in range(NIT):
                        c = 2.0 ** -(it + 1)
                        K.op(K.dve, lambda: nc.vector.scalar_tensor_tensor(out=mid[:, :], in0=w0[:, :], scalar=c, in1=lo[:, :],
                                                                           op0=ALU.mult, op1=ALU.add),
                             reads=[w0, lo], writes=[mid])
                        K.op(K.dve, lambda: nc.vector.tensor_scalar(out=mask01[:, :nk], in0=score[:, :nk], scalar1=mid[:, 0:1],
                                                                    scalar2=None, op0=ALU.is_ge, op1=ALU.add,
                                                                    accum_out=cnt[:, 0:1]),
                             reads=[score, mid], writes=[mask01, cnt])
                        K.op(K.dve, lambda: nc.vector.tensor_scalar(out=gg[:, :], in0=cnt[:, :], scalar1=TOPK, scalar2=c,
                                                                    op0=ALU.is_ge, op1=ALU.mult), reads=[cnt], writes=[gg])
                        K.op(K.dve, lambda: nc.vector.scalar_tensor_tensor(out=lo[:, :], in0=gg[:, :], scalar=w0[:, 0:1],
                                                                           in1=lo[:, :], op0=ALU.mult, op1=ALU.add),
                             reads=[gg, w0, lo], writes=[lo])
                    K.op(K.dve, lambda: nc.vector.tensor_scalar(out=mask01[:, :nk], in0=score[:, :nk], scalar1=lo[:, 0:1],
                                                                scalar2=None, op0=ALU.is_ge), reads=[score, lo], writes=[mask01])
                    for kt0 in range(0, qt + 1, 4):
                        g_ = min(4, qt + 1 - kt0)
                        for i in range(g_):
                            K.op(K.pe, lambda: nc.tensor.transpose(out=ptr[:, i, :], in_=mask01[:, (kt0 + i) * 128:(kt0 + i + 1) * 128],
                                                                   identity=ident[:, :]),
                                 reads=[mask01, ident], writes=[ptr], sig=(i == g_ - 1))
                        K.op(K.act, lambda: nc.scalar.activation(out=MT[:, kt0:kt0 + g_, jq * 128:(jq + 1) * 128],
                                                                 in_=ptr[:, 0:g_, :], func=AF.Copy), reads=[ptr], writes=[MT])
                tq = NMETA + qb * 512

                def load_head(h):
                    QA = qa_sb[h % 2]
                    KA = ka_sb[h % 2]
                    K.dma(K.sp, QA[:, :], self.qaT[h * 64:(h + 1) * 64, tq:tq + 512], QA, writes=[QA])
                    K.dma(K.sp, KA[:, :NMETA + nkt * 128], self.kaT[h * 64:(h + 1) * 64, 0:NMETA + nkt * 128], KA, writes=[KA])
                    return QA, KA

                nxt = load_head(0)
                for h in range(8):
                    K.maybe_reset()
                    QA, KA = nxt
                    if h + 1 < 8:
                        nxt = load_head(h + 1)
                    tiles = [(0, NMETA, vmeta[:NMETA, h * 64:(h + 1) * 64], vmeta, None, None)]
                    for kt in range(nkt):
                        tiles.append((NMETA + kt * 128, 128, va_sb[:, kt, h * 64:(h + 1) * 64], va_sb.sub(kt // 8),
                                      MT[:, kt, :], MT))
                    attend(QA, KA, h, 512, tiles, tq)
            K.end_phase()

    def diff_phase(self, li):
        K = self.K
        nc = self.nc
        S = self.S
        j = li // 2
        lam_init = 0.8 - 0.6 * math.exp(-0.3 * li)
        NKT = S // 128
        NQB = S // 512
        with ExitStack() as st:
            self.consts(st)
            cmT = K.tile(st, "cmT", [128, 4, 512], BF16)
            K.dma(K.sp, cmT[:, :, :], self.c_cmaskT.rearrange("p (j q) -> p j q", q=512), cmT, writes=[cmT])
            cmeta = K.tile(st, "cmeta", [16, 16], BF16)
            K.dma(K.sp, cmeta[:, :], self.c_cmeta[:, :], cmeta, writes=[cmeta])
            dl = self.load_cols(st, "dl", self.dlam_bc[:, j * 256:(j + 1) * 256], 256)
            sgc2 = self.load_cols(st, "sgc", self.subln_c[:, :], 2)
            pr = K.tile(st, "pr", [128, 64], F32)
            s12 = K.tile(st, "s12", [128, 2], F32)
            neglam = K.tile(st, "neglam", [128, 1], F32)
            sgs = K.tile(st, "sgs", [128, 1], F32)
            for r in range(2):
                K.op(K.dve, lambda: nc.vector.tensor_tensor(out=pr[:, :], in0=dl[:, r * 128:r * 128 + 64],
                                                            in1=dl[:, r * 128 + 64:r * 128 + 128], op=ALU.mult),
                     reads=[dl], writes=[pr])
                K.op(K.dve, lambda: nc.vector.tensor_reduce(out=s12[:, r:r + 1], in_=pr[:, :], axis=AX.X, op=ALU.add),
                     reads=[pr], writes=[s12])
            K.op(K.act, lambda: nc.scalar.activation(out=s12[:, :], in_=s12[:, :], func=AF.Exp), reads=[s12], writes=[s12])
            K.op(K.dve, lambda: nc.vector.tensor_tensor(out=neglam[:, :], in0=s12[:, 1:2], in1=s12[:, 0:1], op=ALU.subtract),
                 reads=[s12], writes=[neglam])
            K.op(K.dve, lambda: nc.vector.tensor_scalar(out=neglam[:, :], in0=neglam[:, :], scalar1=-lam_init, scalar2=None,
                                                        op0=ALU.add), reads=[neglam], writes=[neglam])
            K.op(K.dve, lambda: nc.vector.tensor_scalar(out=sgs[:, :], in0=sgc2[:, j:j + 1], scalar1=1.0 - lam_init, scalar2=None,
                                                        op0=ALU.mult), reads=[sgc2], writes=[sgs])
            vb_sb = K.tile(st, "vb_sb", [128, NKT, 512], BF16)
            vbv = self.vb[NMETA:, :].rearrange("(kt p) c -> p kt c", p=128)
            for g0 in range(0, NKT, 8):
                g1 = min(NKT, g0 + 8)
                K.dma(K.sp, vb_sb[:, g0:g1, :], vbv[:, g0:g1, :], vb_sb.slot(g0), writes=[vb_sb.sub(g0 // 8)])
            vmeta = K.tile(st, "vbmeta", [16, 512], BF16)
            K.dma(K.sp, vmeta[:, :], self.vb[0:NMETA, :], vmeta, writes=[vmeta])
            qb_sb = [K.tile(st, f"qb{i}", [64, 2, 512], BF16) for i in range(2)]
            kb_sb = [K.tile(st, f"kb{i}", [64, 2, NMETA + S], BF16) for i in range(2)]
            et = [K.tile(st, f"et{i}", [128, 512], BF16) for i in range(4)]
            r1 = K.tile(st, "r1", [128, 512], F32)
            a1 = K.tile(st, "a1", [128, 512], F32)
            a2 = K.tile(st, "a2", [128, 512], F32)
            sq = K.tile(st, "sqd", [128, 512], F32)
            yt = [K.tile(st, f"yt{i}", [128, 512], BF16) for i in range(2)]
            pst = [K.psum(st, f"pst{i}") for i in range(2)]
            oc_ = [K.psum(st, f"o{i}") for i in range(2)]
            sc_ = [K.psum(st, f"s{i}") for i in range(2)]
            pss = K.psum(st, "pss")
            ei = [0]

            def attend(QB, KB, h, nq, key_tiles, tq):
                steps = [(i, c) for i in range(len(key_tiles)) for c in range(2)]
                nt = len(key_tiles)
                ns = len(steps)
                base = ei[0]
                ei[0] += ns

                def emit_score(si):
                    i, c = steps[si]
                    k0, kn = key_tiles[i][0], key_tiles[i][1]
                    PST = pst[(base + si) % 2]
                    K.mm(PST[:kn, :nq], KB[:, c, k0:k0 + kn], QB[:, c, :nq], True, True, [KB, QB], [PST])

                emit_score(0)
                emit_score(1)
                for si, (i, c) in enumerate(steps):
                    (k0, kn, v_ap, v_res, m_ap, m_res) = key_tiles[i]
                    PST = pst[(base + si) % 2]
                    E_ = et[(base + si) % 4]
                    K.op(K.act, lambda: nc.scalar.activation(out=E_[:kn, :nq], in_=PST[:kn, :nq], func=AF.Exp, scale=0.125),
                         reads=[PST], writes=[E_])
                    if m_ap is not None:
                        K.op(K.dve, lambda: nc.vector.tensor_tensor(out=E_[:kn, :nq], in0=E_[:kn, :nq], in1=m_ap, op=ALU.mult),
                             reads=[E_, m_res], writes=[E_])
                    if si + 2 < ns:
                        emit_score(si + 2)
                    K.mm(oc_[c][:, :nq], v_ap, E_[:kn, :nq], i == 0, i == nt - 1, [v_res, E_], [oc_[c]], sig=True)
                    K.mm(sc_[c][:, :nq], self.ones_b[:kn, :], E_[:kn, :nq], i == 0, i == nt - 1, [self.ones_b, E_], [sc_[c]], sig=True)
                Y = yt[h % 2]
                K.op(K.dve, lambda: nc.vector.reciprocal(out=r1[:, :nq], in_=sc_[0][:, :nq]), reads=[sc_[0]], writes=[r1])
                K.op(K.dve, lambda: nc.vector.tensor_tensor(out=a1[:, :nq], in0=oc_[0][:, :nq], in1=r1[:, :nq], op=ALU.mult),
                     reads=[oc_[0], r1], writes=[a1])
                K.op(K.dve, lambda: nc.vector.reciprocal(out=r1[:, :nq], in_=sc_[1][:, :nq]), reads=[sc_[1]], writes=[r1])
                K.op(K.dve, lambda: nc.vector.tensor_tensor(out=a2[:, :nq], in0=oc_[1][:, :nq], in1=r1[:, :nq], op=ALU.mult),
                     reads=[oc_[1], r1], writes=[a2])
                K.op(K.dve, lambda: nc.vector.scalar_tensor_tensor(out=a1[:, :nq], in0=a2[:, :nq], scalar=neglam[:, 0:1],
                                                                   in1=a1[:, :nq], op0=ALU.mult, op1=ALU.add),
                     reads=[a2, neglam, a1], writes=[a1])
                K.op(K.act, lambda: nc.scalar.activation(out=sq[:, :nq], in_=a1[:, :nq], func=AF.Square), reads=[a1], writes=[sq])
                for c0 in range(0, nq, 256):
                    c1 = min(nq, c0 + 256)
                    K.op(K.pe, lambda: nc.tensor.matmul(out=pss[:, c0:c1], lhsT=self.ones_f[:, :], rhs=sq[:, c0:c1],
                                                        start=True, stop=True), reads=[self.ones_f, sq], writes=[pss])
                K.op(K.act, lambda: nc.scalar.activation(out=r1[:, :nq], in_=pss[:, :nq], func=AF.Sqrt,
                                                         bias=self.eps_t[:, 0:1], scale=1.0 / 128),
                     reads=[pss, self.eps_t], writes=[r1])
                K.op(K.dve, lambda: nc.vector.reciprocal(out=r1[:, :nq], in_=r1[:, :nq]), reads=[r1], writes=[r1])
                K.op(K.dve, lambda: nc.vector.scalar_tensor_tensor(out=Y[:, :nq], in0=a1[:, :nq], scalar=sgs[:, 0:1],
                                                                   in1=r1[:, :nq], op0=ALU.mult, op1=ALU.mult),
                     reads=[a1, sgs, r1], writes=[Y])
                K.dma(K.sp, self.mixB[h * 128:(h + 1) * 128, tq:tq + nq], Y[:, :nq], Y, reads=[Y])

            def load_qk(h, tq, nq, nkeys):
                QB = qb_sb[h % 2]
                KB = kb_sb[h % 2]
                K.dma(K.sp, QB[:, :, :nq], self.qbT[h * 128:(h + 1) * 128, tq:tq + nq].rearrange("(c d) q -> d c q", d=64),
                      QB, writes=[QB])
                K.dma(K.sp, KB[:, :, :nkeys], self.kbT[h * 128:(h + 1) * 128, 0:nkeys].rearrange("(c d) q -> d c q", d=64),
                      KB, writes=[KB])
                return QB, KB

            for h in range(4):
                QB, KB = load_qk(h, 0, NMETA, NMETA)
                attend(QB, KB, h, NMETA, [(0, NMETA, vmeta[:NMETA, h * 128:(h + 1) * 128], vmeta, cmeta[:, :], cmeta)], 0)
            for qb in range(NQB):
                nkt = 4 * (qb + 1)
                tq = NMETA + qb * 512
                nxt = load_qk(0, tq, 512, NMETA + nkt * 128)
                for h in range(4):
                    K.maybe_reset()
                    QB, KB = nxt
                    if h + 1 < 4:
                        nxt = load_qk(h + 1, tq, 512, NMETA + nkt * 128)
                    tiles = [(0, NMETA, vmeta[:NMETA, h * 128:(h + 1) * 128], vmeta, None, None)]
                    for kt in range(nkt):
                        dj = kt - 4 * qb
                        tiles.append((NMETA + kt * 128, 128, vb_sb[:, kt, h * 128:(h + 1) * 128], vb_sb.sub(kt // 8),
                                      cmT[:, dj, :] if dj >= 0 else None, cmT if dj >= 0 else None))
                    attend(QB, KB, h, 512, tiles, tq)
            K.end_phase()

    def attn_out_phase(self, src, li):
        K = self.K
        nc = self.nc
        j = li // 2
        NB = 512
        blocks = token_blocks(self.S, NB)
        with ExitStack() as st:
            woA = K.tile(st, "woA", [64, 8, D], BF16)
            woB = K.tile(st, "woB", [128, 4, D], BF16)
            wav = self.attn_w_out[j, 0:512, :].rearrange("(h d) m -> d h m", d=64)
            wbv = self.attn_w_out[j, 512:1024, :].rearrange("(h p) m -> p h m", p=128)
            for q in range(2):
                self.wload(K.swslot(), woA[:, q * 4:(q + 1) * 4, :], wav[:, q * 4:(q + 1) * 4, :], [woA.sub(q)])
                self.wload(K.swslot(), woB[:, q * 2:(q + 1) * 2, :], wbv[:, q * 2:(q + 1) * 2, :], [woB.sub(q)])
            xt = [K.tile(st, f"xt{i}", [128, 8, NB], F32) for i in range(2)]
            mA = [K.tile(st, f"mA{i}", [64, 8, NB], BF16) for i in range(2)]
            mB = [K.tile(st, f"mB{i}", [128, 4, NB], BF16) for i in range(2)]
            po = [K.psum(st, f"po{i}") for i in range(2)]
            srcv = src.rearrange("(c p) t -> p c t", p=128)
            dstv = self.hT.rearrange("(c p) t -> p c t", p=128)
            for b, (t0, n) in enumerate(blocks):
                K.maybe_reset()
                X = xt[b % 2]
                A = mA[b % 2]
                B_ = mB[b % 2]
                K.dma(K.sp, X[:, :, :n], srcv[:, :, t0:t0 + n], X, reads=[K.dres("h", b)], writes=[X])
                K.dma(K.sp, A[:, :, :n], self.mixA[:, t0:t0 + n].rearrange("(h d) t -> d h t", d=64), A, writes=[A])
                K.dma(K.sp, B_[:, :, :n], self.mixB[:, t0:t0 + n].rearrange("(h p) t -> p h t", p=128), B_, writes=[B_])
                for m in range(8):
                    P = po[m % 2]
                    for h in range(8):
                        K.mm(P[:, :n], woA[:, h, m * 128:(m + 1) * 128], A[:, h, :n], h == 0, False, [woA, A], [P])
                    for hb in range(4):
                        K.mm(P[:, :n], woB[:, hb, m * 128:(m + 1) * 128], B_[:, hb, :n], False, hb == 3, [woB, B_], [P])
                    K.op(K.dve, lambda: nc.vector.tensor_tensor(out=X[:, m, :n], in0=P[:, :n], in1=X[:, m, :n], op=ALU.add),
                         reads=[P, X.sub(m)], writes=[X.sub(m)])
                K.dma(K.sp, dstv[:, :, t0:t0 + n], X[:, :, :n], X, reads=[X], writes=[K.dres("h", b)])
            K.end_phase()

    def copy_phase(self, src):
        K = self.K
        NB = 256
        blocks = token_blocks(self.S, NB)
        with ExitStack() as st:
            xt = [K.tile(st, f"xt{i}", [128, 8, NB], F32) for i in range(2)]
            srcv = src.rearrange("(c p) t -> p c t", p=128)
            dstv = self.hT.rearrange("(c p) t -> p c t", p=128)
            for b, (t0, n) in enumerate(blocks):
                K.maybe_reset()
                X = xt[b % 2]
                K.dma(K.sp, X[:, :, :n], srcv[:, :, t0:t0 + n], X, writes=[X])
                K.dma(K.sp, dstv[:, :, t0:t0 + n], X[:, :, :n], X, reads=[X])
            K.end_phase()

    def final_phase(self, src):
        K = self.K
        nc = self.nc
        NB = 512
        blocks = token_blocks(self.S, NB)[1:]
        with ExitStack() as st:
            self.consts(st)
            gcol = self.load_cols(st, "gcol", self.fin_gc[:, :], 8)
            xt = [K.tile(st, f"xt{i}", [128, 8, NB], F32) for i in range(2)]
            sqt = [K.tile(st, f"sq{i}", [128, 256], F32) for i in range(2)]
            rstd = K.tile(st, "rstd", [128, NB], F32)
            ss = K.psum(st, "ss")
            srcv = src.rearrange("(c p) t -> p c t", p=128)
            dstv = self.yT.rearrange("(c p) t -> p c t", p=128)
            for b, (t0, n) in enumerate(blocks):
                K.maybe_reset()
                X = xt[b % 2]
                K.dma(K.sp, X[:, :, :n], srcv[:, :, t0:t0 + n], X, writes=[X])
                self.rms_rstd(None, X, n, sqt, ss, rstd, self.ones_f)
                for c in range(8):
                    K.op(K.dve, lambda c=c, X=X: nc.vector.scalar_tensor_tensor(
                        out=X[:, c, :n], in0=X[:, c, :n], scalar=gcol[:, c:c + 1], in1=rstd[:, :n],
                        op0=ALU.mult, op1=ALU.mult), reads=[X.sub(c), gcol, rstd], writes=[X.sub(c)])
                K.dma(K.sp, dstv[:, :, t0 - NMETA:t0 - NMETA + n], X[:, :, :n], X, reads=[X])
            K.end_phase()


def full_plan():
    plan = []
    for i in range(DEPTH):
        plan.append(("ffn", i, 0))
        plan.append(("attn", i) if i % 2 == 0 else ("rec", i))
        plan.append(("ffn", i, 1))
    return plan


def cols128(v):
    v = np.asarray(v, np.float32)
    return np.ascontiguousarray(v.reshape(8, 128).T)


def host_inputs(inp, S):
    f32 = np.float32
    shared = {}
    ng = np.asarray(inp["norm_g"], f32)
    shared["norm_gc"] = np.ascontiguousarray(
        np.concatenate([cols128(ng[i, k]) for i in range(DEPTH) for k in range(3)], axis=1))
    shared["fin_gc"] = cols128(inp["final_norm_g"])
    for k in ["ffn_w_gu", "ffn_w_down", "attn_w_in", "attn_w_out", "rec_w_in", "rec_w_out", "rec_gate_w"]:
        shared[k] = np.ascontiguousarray(np.asarray(inp[k], f32))
    lg = np.asarray(inp["idx_k_ln_g"], f32)
    lb = np.asarray(inp["idx_k_ln_b"], f32)
    shared["idx_lnc"] = np.ascontiguousarray(np.stack([lg[0], lb[0], lg[1], lb[1]], axis=1))
    dl = np.asarray(inp["diff_lambda"], f32).reshape(1, 2 * 256)
    shared["dlam_bc"] = np.ascontiguousarray(np.broadcast_to(dl, (128, 512)))
    shared["subln_c"] = np.ascontiguousarray(np.asarray(inp["diff_subln_g"], f32).T)
    rc = []
    for j in range(2):
        for w in range(4):
            rc.append(cols128(inp["rec_conv_w"][j][w]))
        rc.append(cols128(inp["rec_conv_b"][j]))
        rc.append(cols128(inp["rec_gate_b"][j][0]))
        rc.append(cols128(inp["rec_gate_b"][j][1]))
        rc.append(cols128(inp["rec_lambda"][j]))
    shared["rec_cols"] = np.ascontiguousarray(np.concatenate(rc, axis=1))
    shared["c_ident_bf"] = np.eye(128, dtype=f32).astype(ml_dtypes.bfloat16)
    q = np.arange(128)[:, None]
    k = np.arange(128)[None, :]
    shared["c_negmask"] = np.where(k <= q, 0.0, NEG).astype(f32)
    kk = np.arange(128)[:, None, None]
    jj = np.arange(4)[None, :, None]
    qq = np.arange(512)[None, None, :]
    shared["c_cmaskT"] = np.ascontiguousarray(
        ((128 * jj + kk) <= qq).astype(f32).reshape(128, 2048)).astype(ml_dtypes.bfloat16)
    k16 = np.arange(16)[:, None]
    q16 = np.arange(16)[None, :]
    shared["c_cmeta"] = (k16 <= q16).astype(f32).astype(ml_dtypes.bfloat16)
    x = np.asarray(inp["x"], f32)
    meta = np.asarray(inp["meta_tokens"], f32)
    maps = []
    for b in range(x.shape[0]):
        m = dict(shared)
        m["xT"] = np.ascontiguousarray(np.concatenate([meta, x[b]], axis=0).T)
        maps.append(m)
    return maps


_CACHE = {}


def run(inp, plan=None):
    x = np.asarray(inp["x"])
    B, S, _ = x.shape
    plan = full_plan() if plan is None else plan
    key = (S, tuple(plan))
    if key not in _CACHE:
        _CACHE[key] = Prog(S, plan)
    prog = _CACHE[key]
    maps = host_inputs(inp, S)
    res = run_bass_kernel_spmd(prog.nc, maps, core_ids=list(range(B)))
    out = np.stack([np.ascontiguousarray(res.results[b]["yT"].T) for b in range(B)], axis=0)
    return out.astype(np.float32)


def kernel(**inputs):
    return run(inputs)
```
